# Optimizing a Trainium2 kernel written in Bass

```python
import jax
import jax.numpy as jnp
from jax import lax
import numpy as np

D_MODEL = 1024
BATCH = 16
SEQ = 2048
DEPTH = 1

D_MIX = D_MODEL
ATT_HEADS = 8
ATT_KV_HEADS = 2
ATT_HEAD_DIM = D_MIX // 16
ATT_WIDTH = ATT_HEADS * ATT_HEAD_DIM
ATT_KV_WIDTH = ATT_KV_HEADS * ATT_HEAD_DIM
WINDOW = 128
MLSTM_HEADS = 4
MLSTM_WIDTH = D_MIX - ATT_WIDTH
MLSTM_HEAD_DIM = MLSTM_WIDTH // MLSTM_HEADS
MLSTM_CHUNK = 64
QK_CONV_WIDTH = 4
IN_COLS = ATT_WIDTH + 2 * ATT_KV_WIDTH + 4 * MLSTM_WIDTH + 2 * MLSTM_HEADS
X_HEADS = 4
X_HEAD_DIM = D_MODEL // X_HEADS
MEM_LEN = 256
D_FF = 2816
FFN_CONV_WIDTH = 3
RMS_EPS = 1e-6
NEG_BIG = -1e30

kernel_name = 'hybrid_swa_mlstm_convffn_block'


def rmsnorm(x, g):
    xf = x.astype(jnp.float32)
    y = xf * lax.rsqrt(jnp.mean(xf * xf, axis=-1, keepdims=True) + RMS_EPS)
    return (y * g.astype(jnp.float32)).astype(x.dtype)


def causal_depthwise_conv(x, w, b):
    K, C = w.shape
    y = lax.conv_general_dilated(
        x, w.astype(x.dtype)[:, None, :], window_strides=(1,),
        padding=[(K - 1, 0)], dimension_numbers=('NWC', 'WIO', 'NWC'),
        feature_group_count=C)
    return y + b.astype(x.dtype)


def alibi_slopes(n_heads):
    return 2.0 ** (-8.0 * jnp.arange(1, n_heads + 1, dtype=jnp.float32) / n_heads)


def sliding_window_attention(q, k, v, sinks, slopes):
    B, S, _, D = q.shape
    W = WINDOW
    nb = S // W
    G = ATT_HEADS // ATT_KV_HEADS
    qb = q.reshape(B, nb, W, ATT_KV_HEADS, G, D)
    kb = k.reshape(B, nb, W, ATT_KV_HEADS, D)
    vb = v.reshape(B, nb, W, ATT_KV_HEADS, D)
    pad = ((0, 0), (1, 0), (0, 0), (0, 0), (0, 0))
    kk = jnp.concatenate([jnp.pad(kb, pad)[:, :-1], kb], axis=2)
    vv = jnp.concatenate([jnp.pad(vb, pad)[:, :-1], vb], axis=2)
    s = jnp.einsum('bnqhgd,bnkhd->bnhgqk', qb, kk,
                   preferred_element_type=jnp.float32) * (D ** -0.5)
    qi = jnp.arange(W)[:, None]
    kj = jnp.arange(2 * W)[None, :]
    dist = qi + W - kj
    in_band = (dist >= 0) & (dist < W)
    valid = in_band[None] & ((jnp.arange(nb)[:, None, None] > 0) | (kj >= W)[None])
    bias = -slopes.reshape(ATT_KV_HEADS, G)[:, :, None, None] * dist.astype(jnp.float32)
    s = jnp.where(valid[None, :, None, None], s + bias[None, None], NEG_BIG)
    sink = sinks.astype(jnp.float32).reshape(ATT_KV_HEADS, G)[None, None, :, :, None, None]
    m = jnp.maximum(jnp.max(s, axis=-1, keepdims=True), sink)
    p = jnp.exp(s - m)
    p = p / (jnp.sum(p, axis=-1, keepdims=True) + jnp.exp(sink - m))
    o = jnp.einsum('bnhgqk,bnkhd->bnqhgd', p.astype(vv.dtype), vv)
    return o.reshape(B, S, ATT_HEADS * D)


def mlstm_chunkwise(q, k, v, i_pre, f_pre):
    B, S, H, D = q.shape
    L = MLSTM_CHUNK
    nc = S // L
    q = q * (D ** -0.5)

    def to_chunks(a):
        return a.reshape(B, nc, L, H, D).transpose(1, 0, 3, 2, 4)

    def gate_chunks(a):
        return a.reshape(B, nc, L, H).transpose(1, 0, 3, 2)

    causal = jnp.tril(jnp.ones((L, L), dtype=bool))

    def step(carry, inp):
        C, n, m = carry
        qc, kc, vc, igc, lfc = inp
        b = jnp.cumsum(lfc, axis=-1)
        dmat = jnp.where(causal, b[..., :, None] - b[..., None, :] + igc[..., None, :], NEG_BIG)
        inter = b + m[..., None]
        m_t = jnp.maximum(inter, jnp.max(dmat, axis=-1))
        w_inter = jnp.exp(inter - m_t)
        sc = jnp.einsum('bhtd,bhsd->bhts', qc, kc) * jnp.exp(dmat - m_t[..., None])
        num = (w_inter[..., None] * jnp.einsum('bhtd,bhde->bhte', qc, C)
               + jnp.einsum('bhts,bhse->bhte', sc, vc))
        den = w_inter * jnp.einsum('bhtd,bhd->bht', qc, n) + jnp.sum(sc, axis=-1)
        h = num / jnp.maximum(jnp.abs(den), jnp.exp(-m_t))[..., None]
        b_end = b[..., -1]
        g = b_end[..., None] - b + igc
        m_new = jnp.maximum(b_end + m, jnp.max(g, axis=-1))
        decay = jnp.exp(b_end + m - m_new)
        ws = jnp.exp(g - m_new[..., None])
        C_new = decay[..., None, None] * C + jnp.einsum('bhs,bhsd,bhse->bhde', ws, kc, vc)
        n_new = decay[..., None] * n + jnp.einsum('bhs,bhsd->bhd', ws, kc)
        return (C_new, n_new, m_new), h

    init = (jnp.zeros((B, H, D, D), jnp.float32),
            jnp.zeros((B, H, D), jnp.float32),
            jnp.zeros((B, H), jnp.float32))
    xs = (to_chunks(q), to_chunks(k), to_chunks(v),
          gate_chunks(i_pre), gate_chunks(jax.nn.log_sigmoid(f_pre)))
    _, h = lax.scan(step, init, xs)
    return h.transpose(1, 0, 3, 2, 4).reshape(B, S, H * D)


def cross_attention(hq, hm, wq, wkv, wo):
    B, S, _ = hq.shape
    M = hm.shape[1]
    q = (hq @ wq).reshape(B, S, X_HEADS, X_HEAD_DIM)
    kv = hm @ wkv
    k = kv[..., :D_MODEL].reshape(B, M, X_HEADS, X_HEAD_DIM)
    v = kv[..., D_MODEL:].reshape(B, M, X_HEADS, X_HEAD_DIM)
    s = jnp.einsum('bshd,bmhd->bhsm', q, k,
                   preferred_element_type=jnp.float32) * (X_HEAD_DIM ** -0.5)
    a = jax.nn.softmax(s, axis=-1).astype(v.dtype)
    o = jnp.einsum('bhsm,bmhd->bshd', a, v).reshape(B, S, X_HEADS * X_HEAD_DIM)
    return o @ wo


def setup_inputs(seed: int = 0) -> dict:
    key = jax.random.key(seed)
    ks = jax.random.split(key, 22)
    f32 = jnp.float32

    def nrm(k, shape, scale):
        return jax.random.normal(k, shape, f32) * scale

    def gain(k, shape):
        return 1.0 + 0.02 * jax.random.normal(k, shape, f32)

    L = DEPTH
    f_bias = jnp.linspace(3.0, 6.0, MLSTM_HEADS, dtype=f32)
    return {
        'x': nrm(ks[0], (BATCH, SEQ, D_MODEL), 1.0),
        'mem': nrm(ks[1], (BATCH, MEM_LEN, D_MODEL), 1.0),
        'norm_mix_g': gain(ks[2], (L, D_MODEL)),
        'w_in': nrm(ks[3], (L, D_MODEL, IN_COLS), D_MODEL ** -0.5),
        'b_gate_if': jnp.concatenate(
            [nrm(ks[4], (L, MLSTM_HEADS), 0.1),
             f_bias[None] + nrm(ks[5], (L, MLSTM_HEADS), 0.1)], axis=-1),
        'conv_qk_w': nrm(ks[6], (L, QK_CONV_WIDTH, 2 * MLSTM_WIDTH), QK_CONV_WIDTH ** -0.5),
        'conv_qk_b': nrm(ks[7], (L, 2 * MLSTM_WIDTH), 0.02),
        'attn_sinks': nrm(ks[8], (L, ATT_HEADS), 0.5),
        'w_out': nrm(ks[9], (L, D_MIX, D_MODEL), D_MIX ** -0.5),
        'norm_xattn_g': gain(ks[10], (L, D_MODEL)),
        'norm_mem_g': gain(ks[11], (L, D_MODEL)),
        'wq_x': nrm(ks[12], (L, D_MODEL, X_HEADS * X_HEAD_DIM), D_MODEL ** -0.5),
        'wkv_x': nrm(ks[13], (L, D_MODEL, 2 * X_HEADS * X_HEAD_DIM), D_MODEL ** -0.5),
        'wo_x': nrm(ks[14], (L, X_HEADS * X_HEAD_DIM, D_MODEL), D_MODEL ** -0.5),
        'norm_ffn_g': gain(ks[15], (L, D_MODEL)),
        'w_up': nrm(ks[16], (L, D_MODEL, 2 * D_FF), D_MODEL ** -0.5),
        'conv_ffn_w': nrm(ks[17], (L, FFN_CONV_WIDTH, D_FF), FFN_CONV_WIDTH ** -0.5),
        'conv_ffn_b': nrm(ks[18], (L, D_FF), 0.02),
        'w_down': nrm(ks[19], (L, D_FF, D_MODEL), D_FF ** -0.5),
        'norm_final_g': gain(ks[20], (D_MODEL,)),
    }


def reference(x, mem, norm_mix_g, w_in, b_gate_if, conv_qk_w, conv_qk_b, attn_sinks,
              w_out, norm_xattn_g, norm_mem_g, wq_x, wkv_x, wo_x, norm_ffn_g, w_up,
              conv_ffn_w, conv_ffn_b, w_down, norm_final_g):
    B, S, _ = x.shape
    H = MLSTM_HEADS
    slopes = alibi_slopes(ATT_HEADS)
    o0 = ATT_WIDTH
    o1 = o0 + ATT_KV_WIDTH
    o2 = o1 + ATT_KV_WIDTH
    o3 = o2 + 2 * MLSTM_WIDTH
    o4 = o3 + MLSTM_WIDTH
    o5 = o4 + MLSTM_WIDTH
    for l in range(DEPTH):
        h = rmsnorm(x, norm_mix_g[l])
        p = h @ w_in[l]
        aq = p[..., :o0].reshape(B, S, ATT_HEADS, ATT_HEAD_DIM)
        ak = p[..., o0:o1].reshape(B, S, ATT_KV_HEADS, ATT_HEAD_DIM)
        av = p[..., o1:o2].reshape(B, S, ATT_KV_HEADS, ATT_HEAD_DIM)
        attn_out = sliding_window_attention(aq, ak, av, attn_sinks[l], slopes)

        mqk = jax.nn.silu(causal_depthwise_conv(p[..., o2:o3], conv_qk_w[l], conv_qk_b[l]))
        mq = mqk[..., :MLSTM_WIDTH].reshape(B, S, H, MLSTM_HEAD_DIM).astype(jnp.float32)
        mk = mqk[..., MLSTM_WIDTH:].reshape(B, S, H, MLSTM_HEAD_DIM).astype(jnp.float32)
        mv = p[..., o3:o4].reshape(B, S, H, MLSTM_HEAD_DIM).astype(jnp.float32)
        mo = p[..., o4:o5].astype(jnp.float32)
        gif = p[..., o5:].astype(jnp.float32) + b_gate_if[l].astype(jnp.float32)
        m_h = mlstm_chunkwise(mq, mk, mv, gif[..., :H], gif[..., H:])
        mlstm_out = (jax.nn.sigmoid(mo) * m_h).astype(x.dtype)

        x = x + jnp.concatenate([attn_out, mlstm_out], axis=-1) @ w_out[l]

        x = x + cross_attention(rmsnorm(x, norm_xattn_g[l]), rmsnorm(mem, norm_mem_g[l]),
                                wq_x[l], wkv_x[l], wo_x[l])

        hf = rmsnorm(x, norm_ffn_g[l])
        u = hf @ w_up[l]
        g = causal_depthwise_conv(u[..., :D_FF], conv_ffn_w[l], conv_ffn_b[l])
        x = x + (jax.nn.silu(g) * u[..., D_FF:]) @ w_down[l]
    return rmsnorm(x, norm_final_g)
```

```python
import os
import numpy as np
import ml_dtypes
from contextlib import ExitStack
import concourse.bass as bass
import concourse.mybir as mybir
from concourse.bass_utils import run_bass_kernel_spmd

F32 = mybir.dt.float32
BF16 = mybir.dt.bfloat16
ALU = mybir.AluOpType
AF = mybir.ActivationFunctionType

D = 1024
SEQ = 2048
BATCH = 16
NCORES = 8
T = 512
NB = 4
DFF = 2816
NJ = 22
MEM = 256
EPS = 1e-6
NSLAB = 3
SLAB_ELEMS = 4096


class StopBuild(Exception):
    pass


class Sched:
    def __init__(self, nc, es):
        self.nc = nc
        self.es = es
        self.engs = {"pe": nc.tensor, "act": nc.scalar, "dve": nc.vector, "pool": nc.gpsimd, "sp": nc.sync}
        self.sems = {}
        self.cnt = {}
        self.seen = {e: {} for e in self.engs}
        self.w = {}
        self.r = {}
        for e in ("pe", "act", "dve", "pool"):
            self.new_sem(e)
        self.nwaits = 0
        self.nops = 0
        self.marks = []
        self._pending = None

    def new_sem(self, name):
        self.sems[name] = self.es.enter_context(self.nc.semaphore("s_" + name))
        self.cnt[name] = 0

    def _deps(self, eng, reads, writes):
        deps = []
        pe = (eng == "pe")
        for k in reads:
            t = self.w.get(k)
            if t is not None and not (pe and t[2] == "pe"):
                deps.append(t)
        for k in writes:
            t = self.w.get(k)
            if t is not None and not (pe and t[2] == "pe"):
                deps.append(t)
            for t in self.r.get(k, ()):
                if not (pe and t[2] == "pe"):
                    deps.append(t)
        return deps

    def _wait(self, eng, deps):
        need = {}
        for (s, v, _) in deps:
            if v > need.get(s, 0):
                need[s] = v
        for s, v in need.items():
            if self.seen[eng].get(s, 0) < v:
                assert v <= self.cnt[s], ("wait on unsignalled", eng, s, v, self.cnt[s])
                self.engs[eng].wait_ge(self.sems[s], v)
                self.seen[eng][s] = v
                self.nwaits += 1

    def _record(self, tok, reads, writes):
        for k in reads:
            self.r.setdefault(k, []).append(tok)
        for k in writes:
            self.w[k] = tok
            self.r[k] = []

    def op(self, eng, fn, reads=(), writes=(), signal=True):
        self._wait(eng, self._deps(eng, reads, writes))
        ins = fn(self.engs[eng])
        self.nops += 1
        if self._pending is not None:
            self.marks.append((self._pending, ins.ins.name))
            self._pending = None
        if signal:
            self.cnt[eng] += 1
            ins.then_inc(self.sems[eng], 1)
            tok = (eng, self.cnt[eng], eng)
        else:
            tok = (eng, self.cnt[eng] + 1, eng)
        self._record(tok, reads, writes)
        return ins

    def mark(self, label):
        if os.environ.get('STOP_AT') == label:
            raise StopBuild(label)
        self._pending = label

    def dma(self, issuer, sem, out, in_, reads=(), writes=()):
        if sem not in self.sems:
            self.new_sem(sem)
        self._wait(issuer, self._deps("dma:" + sem, reads, writes))
        ins = self.engs[issuer].dma_start(out=out, in_=in_)
        self.cnt[sem] += 16
        ins.then_inc(self.sems[sem], 16)
        tok = (sem, self.cnt[sem], "dma:" + sem)
        self._record(tok, reads, writes)
        return ins


def build_program(nseq, ntps, dbg=False, wplan=None):
    NT = nseq * ntps
    NTOK = NT * T
    nc = bass.Bass("TRN2", target_bir_lowering=False)

    def din(name, shape):
        return nc.dram_tensor(name, list(shape), F32, kind="ExternalInput").ap()

    xT_d = din("xT", [128, 8, NTOK])
    memT_d = din("memT", [nseq, 128, 8, MEM])
    winF_d = din("w_inF", [128, 8, 1664])
    winT_d = din("w_inT", [128, 8, 1160])
    wout_d = din("w_out", [128, 8, 1024])
    wq_d = din("wq", [128, 8, 1024])
    wkv_d = din("wkv", [128, 8, 2048])
    wo_d = din("wo", [128, 8, 1024])
    wup_d = din("w_up", [128, 8, 2 * DFF])
    wdn_d = din("w_down", [128, NJ, 1024])
    gains_d = din("gains", [128, 5, 8])
    cqw_d = din("cqw", [128, 8, 4])
    cqb_d = din("cqb", [128, 8])
    cfw_d = din("cfw", [128, NJ, 3])
    cfb_d = din("cfb", [128, NJ])
    bgate_d = din("bgate", [8])
    sinks_d = din("sinks", [8])
    swab_d = din("swab", [4, 128, 512])
    cmat_d = din("cmat", [5, 128, 128])
    outT_d = nc.dram_tensor("outT", [128, 8, NTOK], F32, kind="ExternalOutput").ap()
    if dbg:
        dbg_d = nc.dram_tensor("dbg", [128, 8, NTOK], F32, kind="ExternalOutput").ap()

    with ExitStack() as es:
        S = Sched(nc, es)

        def sb(name, shape, dt):
            return es.enter_context(nc.sbuf_tensor("sb_" + name, list(shape), dt))

        xT = [sb(f"xT{i}", [128, 8, T], F32) for i in range(2)]
        hT = sb("hT", [128, 8, T], BF16)
        mixT = sb("mixT", [128, 8, T], BF16)
        lnv = sb("lnv", [128, T], F32)
        rstd = sb("rstd", [128, T], F32)
        lnv2 = sb("lnv2", [128, T], F32)
        rstd2 = sb("rstd2", [128, T], F32)
        sqb = sb("sqb", [128, 8, T], BF16)
        qTa = sb("qTa", [128, 4, T], BF16)
        kTa = [sb(f"kTa{i}", [128, 128 + T], BF16) for i in range(2)]
        pre = sb("pre", [128, 8, T + 3], F32)
        ct = [sb(f"ct{i}", [128, T], F32) for i in range(2)]
        qkT = sb("qkT", [128, 8, T], BF16)
        vaug = [sb(f"vaug{i}", [128, 2, 65], BF16) for i in range(NB + 1)]
        vpr = [sb(f"vpr{i}", [128, 4, 129], BF16) for i in range(NB)]
        sgo = [sb(f"sgo{i}", [128, T], BF16) for i in range(NB)]
        G = sb("G", [128, NB, 8], F32)
        GE = sb("GE", [128, NB, 4], F32)
        SPV = sb("SPV", [128, NB, 4], F32)
        GA = sb("GA", [128, NB, 4], F32)
        WS = sb("WS", [128, NB * 4], F32)
        CL = sb("CL", [128, NB * 4], F32)
        DEC = [sb(f"DEC{i}", [128, NB * 4], F32) for i in range(2)]
        DECQ = [sb(f"DECQ{i}", [128, NB * 4], F32) for i in range(2)]
        ktok = sb("ktok", [128, 4, 128], BF16)
        scb = sb("scb", [128, 4, 128], BF16)
        Q = sb("Q", [128, 4, 129], F32)
        Pq = sb("Pq", [128, 4, 129], BF16)
        dn = sb("dn", [128, 16], F32)
        rr = sb("rr", [128, 16], F32)
        mlo = [sb(f"mlo{i}", [128, T], BF16) for i in range(2)]
        att = [sb(f"att{i}", [128, T], BF16) for i in range(2)]
        pT = [sb(f"pT{i}", [128, T], BF16) for i in range(4)]
        KTx = sb("KTx", [128, 8, MEM], BF16)
        Vx = [sb(f"Vx{i}", [128, 1024], BF16) for i in range(2)]
        rs = [sb(f"rs{i}", [128, T], F32) for i in range(2)]
        aT = sb("aT", [128, NJ, T], BF16)
        gbuf = [sb(f"gbuf{i}", [128, T + 4], F32) for i in range(3)]
        sgt = [sb(f"sgt{i}", [128, T], F32) for i in range(2)]
        ghalo = sb("ghalo", [128, NJ, 4], F32)
        slab = [sb(f"slab{i}", [128, SLAB_ELEMS], BF16) for i in range(NSLAB)]
        swab = sb("swab", [128, 4, 512], BF16)
        cmatf = sb("cmatf", [128, 5, 128], F32)
        ident = sb("ident", [128, 128], BF16)
        ones = sb("ones", [128, 128], BF16)
        onesdiv = sb("onesdiv", [128, 128], BF16)
        gains = sb("gains", [128, 5, 8], F32)
        cqw = sb("cqw", [128, 8, 4], F32)
        cqb = sb("cqb", [128, 8], F32)
        cfw = sb("cfw", [128, NJ, 3], F32)
        cfb = sb("cfb", [128, NJ], F32)
        bgate = sb("bgate", [128, 8], F32)
        esink = sb("esink", [128, 8], F32)
        mmask = cmatf[:, 0, :]
        tri = cmatf[:, 1, :]
        negones = cmatf[:, 2, :]

        ps = [es.enter_context(nc.psum_tensor(f"ps{i}", [128, 512], F32)) for i in range(8)]
        psb = [p[:].bitcast(BF16) for p in ps]
        bank_ctr = [0]

        def newbank():
            b = bank_ctr[0] % 7
            bank_ctr[0] += 1
            return b

        def pk(bank, lo=0, hi=512):
            return [("ps", bank)]

        def ACT(out, in_, func, reads, writes, bias=None, scale=None):
            kw = {}
            if bias is not None:
                kw["bias"] = bias
            if scale is not None:
                kw["scale"] = scale
            return S.op("act", lambda e: e.activation(out=out, in_=in_, func=func, **kw), reads, writes)

        def TT(out, in0, in1, op, reads, writes, eng="dve"):
            return S.op(eng, lambda e: e.tensor_tensor(out=out, in0=in0, in1=in1, op=op), reads, writes)

        def TS(out, in0, s1, s2, op0, op1, reads, writes, eng="dve"):
            if s2 is None:
                return S.op(eng, lambda e: e.tensor_scalar(out=out, in0=in0, scalar1=s1, scalar2=None, op0=op0), reads, writes)
            return S.op(eng, lambda e: e.tensor_scalar(out=out, in0=in0, scalar1=s1, scalar2=s2, op0=op0, op1=op1), reads, writes)

        def STT(out, in0, scalar, in1, op0, op1, reads, writes):
            return S.op("dve", lambda e: e.scalar_tensor_tensor(out=out, in0=in0, scalar=scalar, in1=in1, op0=op0, op1=op1), reads, writes)

        def CP(out, in_, reads, writes, eng="dve"):
            return S.op(eng, lambda e: e.tensor_copy(out=out, in_=in_), reads, writes)

        def MS(ap, val, writes, eng="dve"):
            return S.op(eng, lambda e: e.memset(ap, val), (), writes)

        def MM(out, lhsT, rhs, start, stop, reads, writes, signal):
            return S.op("pe", lambda e: e.matmul(out, lhsT=lhsT, rhs=rhs, start=start, stop=stop), reads, writes, signal=signal)

        def TR(out, in_, reads, writes, signal):
            return S.op("pe", lambda e: e.transpose(out, in_, ident[:]), list(reads) + ["ident"], writes, signal=signal)

        S.dma("sp", "c0", cmatf[:], cmat_d.rearrange("c p f -> p c f"), (), ["cmatf"])
        S.dma("pool", "c1", swab[:], swab_d.rearrange("c p f -> p c f"), (), ["swab"])
        S.dma("sp", "c2", gains[:], gains_d, (), ["gains"])
        S.dma("sp", "c3", cqw[:], cqw_d, (), ["cqw"])
        S.dma("sp", "c4", cqb[:], cqb_d, (), ["cqb"])
        S.dma("sp", "c5", cfw[:], cfw_d, (), ["cfw"])
        S.dma("sp", "c6", cfb[:], cfb_d, (), ["cfb"])
        S.dma("sp", "c7", bgate[:], bgate_d.partition_broadcast(128), (), ["bgate"])
        S.dma("sp", "c8", esink[:], sinks_d.partition_broadcast(128), (), ["esink"])
        CP(ident[:], cmatf[:, 3, :], ["cmatf"], ["ident"])
        CP(ones[:], cmatf[:, 4, :], ["cmatf"], ["ones"])
        TS(onesdiv[:], cmatf[:, 4, :], 1.0 / D, None, ALU.mult, None, ["cmatf"], ["onesdiv"])
        ACT(esink[:], esink[:], AF.Exp, ["esink"], ["esink"])
        for i in range(NB + 1):
            MS(vaug[i][:], 1.0, [("vaug", i)])
        for i in range(2):
            MS(kTa[i][:], 0.0, [("kTa", i)])
            MS(DEC[i][:], 0.0, [("DEC", i)])
            MS(DECQ[i][:], 0.0, [("DECQ", i)])

        class WStream:
            def __init__(self, plan):
                self.plan = plan
                self.req = []
                self.rec = []
                self.issued = 0
                self.cur = 0

            def view(self, i, kdim, ncols):
                return slab[i % NSLAB][:, 0:kdim * ncols].rearrange("p (k n) -> p k n", k=kdim)

            def _issue(self, i, spec):
                name, c0, kdim, ncols = spec
                b = i % NSLAB
                S.dma("pool", f"ws{b}", self.view(i, kdim, ncols), WSRC[name][:, :, c0:c0 + ncols], (), [("slab", b)])

            def get(self, name, c0, kdim, ncols):
                spec = (name, c0, kdim, ncols)
                i = self.cur
                self.rec.append(spec)
                if self.plan is None:
                    self._issue(i, spec)
                else:
                    assert self.plan[i] == spec, (i, self.plan[i], spec)
                    while self.issued < min(len(self.plan), i + NSLAB):
                        self._issue(self.issued, self.plan[self.issued])
                        self.issued += 1
                self.cur += 1
                return self.view(i, kdim, ncols), ("slab", i % NSLAB)

        WSRC = {"wkv": wkv_d, "winF": winF_d, "winT": winT_d, "wout": wout_d, "wq": wq_d, "wo": wo_d,
                "wup": wup_d, "wdn": wdn_d}
        W = WStream(wplan)

        def xk(xb, c):
            return ("x", xb, c)

        SB7 = [("ps", 7)]

        def stats_finish(n, which=0):
            lv, rv = (lnv, rstd) if which == 0 else (lnv2, rstd2)
            lk, rk = ("lnv", "rstd") if which == 0 else ("lnv2", "rstd2")
            ACT(lv[:, 0:n], ps[7][:, 0:n], AF.Ln, SB7 + ["epsb"], [lk], bias=epsb[:, 0:1])
            ACT(rv[:, 0:n], lv[:, 0:n], AF.Exp, [lk], [rk], scale=-0.5)

        def stats_full(src_ap, src_keys, n):
            S.op("act", lambda e: e.activation(out=sqb[:, :, 0:n], in_=src_ap, func=AF.Square),
                 src_keys, [("sqb", c) for c in range(8)])
            for c in range(8):
                MM(ps[7][:, 0:n], onesdiv[:], sqb[:, c, 0:n], c == 0, c == 7, [("sqb", c), "onesdiv"], SB7, c == 7)

        def norm_stats(src_ap_fn, src_keys_fn, n):
            stats_full(src_ap_fn(None), [src_keys_fn(c) for c in range(8)], n)
            stats_finish(n)

        class FusedStats:
            def __init__(self, xb):
                self.xb = xb
                self.pending = []

            def add(self, mm):
                if os.environ.get('NO_FUSED'):
                    return
                ACT(sqb[:, mm, :], xT[self.xb][:, mm, :], AF.Square, [xk(self.xb, mm)], [("sqb", mm)])
                self.pending.append(mm)

            def flush(self, keep):
                if os.environ.get('NO_FUSED'):
                    return
                while len(self.pending) > keep:
                    c = self.pending.pop(0)
                    MM(ps[7][:], onesdiv[:], sqb[:, c, :], c == 0, c == 7, [("sqb", c), "onesdiv"], SB7, c == 7)

        def norm_apply(xb, gi, which=0):
            rv, rk = (rstd, "rstd") if which == 0 else (rstd2, "rstd2")
            for c in range(8):
                STT(hT[:, c, :], xT[xb][:, c, :], gains[:, gi, c:c + 1], rv[:], ALU.mult, ALU.mult,
                    [xk(xb, c), rk, "gains"], [("hT", c)])

        def proj_group(b, n, lhs_fn, rhs_fn, nk, reads_fn, width=None):
            for k in range(nk):
                MM(ps[b][:, 0:n], lhs_fn(k), rhs_fn(k), k == 0, k == nk - 1, reads_fn(k), pk(b, 0, n), k == nk - 1)

        epsb = sb("epsb", [128, 8], F32)
        MS(epsb[:], EPS, ["epsb"])

        def load_x(t):
            xb = t % 2
            S.dma("sp", f"x{xb}", xT[xb][:], xT_d[:, :, t * T:(t + 1) * T], (), [xk(xb, c) for c in range(8)])

        def mem_stage(s):
            S.dma("sp", "mem", pre[:, :, 0:MEM], memT_d[s], (), [("pre", c) for c in range(8)])
            norm_stats(lambda _: pre[:, :, 0:MEM], lambda c: ("pre", c), MEM)
            for c in range(8):
                STT(hT[:, c, 0:MEM], pre[:, c, 0:MEM], gains[:, 2, c:c + 1], rstd[:, 0:MEM], ALU.mult, ALU.mult,
                    [("pre", c), "rstd", "gains"], [("hT", c)])
            for s4 in range(4):
                wv, wkey = W.get("wkv", s4 * 512, 8, 512)
                if s4 < 2:
                    for m in range(4):
                        b = newbank()
                        proj_group(b, MEM, lambda k: wv[:, k, m * 128:(m + 1) * 128], lambda k: hT[:, k, 0:MEM], 8,
                                   lambda k: [wkey, ("hT", k)])
                        ACT(KTx[:, s4 * 4 + m, :], ps[b][:, 0:MEM], AF.Copy, pk(b, 0, MEM), ["KTx"])
                else:
                    for mb in range(2):
                        b = newbank()
                        proj_group(b, 512, lambda k: hT[:, k, mb * 128:(mb + 1) * 128], lambda k: wv[:, k, :], 8,
                                   lambda k: [wkey, ("hT", k)])
                        ACT(Vx[mb][:, (s4 - 2) * 512:(s4 - 1) * 512], ps[b][:], AF.Copy, pk(b), [("Vx", mb)])

        def mixer_front(t):
            xb = t % 2
            j = t % ntps
            first = (j == 0)
            tb = t % 2
            if first:
                MS(pre[:, :, 0:3], 0.0, [("pre", c) for c in range(8)])
                MS(Q[:], 0.0, ["Q"])
                MS(ghalo[:], 0.0, [("ghalo", jx_) for jx_ in range(NJ)])
            else:
                CP(pre[:, :, 0:3], pre[:, :, T:T + 3], [("pre", c) for c in range(8)], [("pre", c) for c in range(8)])
                CP(kTa[0][0:64, 0:128], kTa[0][0:64, T:T + 128], [("kTa", 0)], [("kTa", 0)])
                CP(kTa[1][64:128, 0:128], kTa[1][64:128, T:T + 128], [("kTa", 1)], [("kTa", 1)])
                CP(vaug[0][:], vaug[NB][:], [("vaug", NB)], [("vaug", 0)])
            if (not first) and t > 0 and (not os.environ.get('NO_EARLY_STATS')) and not os.environ.get('USE_EARLY'):
                norm_apply(xb, 0, which=1)
            elif first or t == 0 or not os.environ.get('USE_EARLY'):
                norm_stats(lambda _: xT[xb][:], lambda c: xk(xb, c), T)
                norm_apply(xb, 0)
            S.mark(f"m{t}.F0")
            wv, wkey = W.get("winF", 0, 8, 512)
            for m in range(4):
                b = newbank()
                proj_group(b, T, lambda k: wv[:, k, m * 128:(m + 1) * 128], lambda k: hT[:, k, :], 8,
                           lambda k: [wkey, ("hT", k)])
                ACT(qTa[:, m, :], ps[b][:], AF.Copy, pk(b), ["qTa"], scale=0.125)
            yield
            S.mark(f"m{t}.F12")
            for f in range(2):
                wv, wkey = W.get("winF", 512 + f * 512, 8, 512)
                for m in range(4):
                    c = f * 4 + m
                    b = newbank()
                    proj_group(b, T, lambda k: wv[:, k, m * 128:(m + 1) * 128], lambda k: hT[:, k, :], 8,
                               lambda k: [wkey, ("hT", k)])
                    ACT(pre[:, c, 3:T + 3], ps[b][:], AF.Copy, pk(b), [("pre", c)])
                    tt = ct[c % 2]
                    tk = ("ct", c % 2)
                    TS(tt[:], pre[:, c, 0:T], cqw[:, c, 0:1], None, ALU.mult, None, [("pre", c), "cqw"], [tk])
                    STT(tt[:], pre[:, c, 1:T + 1], cqw[:, c, 1:2], tt[:], ALU.mult, ALU.add, [("pre", c), "cqw", tk], [tk])
                    STT(tt[:], pre[:, c, 2:T + 2], cqw[:, c, 2:3], tt[:], ALU.mult, ALU.add, [("pre", c), "cqw", tk], [tk])
                    STT(tt[:], pre[:, c, 3:T + 3], cqw[:, c, 3:4], tt[:], ALU.mult, ALU.add, [("pre", c), "cqw", tk], [tk])
                    ACT(qkT[:, c, :], tt[:], AF.Silu, [tk, "cqb"], [("qkT", c)], bias=cqb[:, c:c + 1])
                yield
            S.mark(f"m{t}.F3")
            wv, wkey = W.get("winF", 1536, 8, 128)
            b = newbank()
            proj_group(b, T, lambda k: wv[:, k, 0:128], lambda k: hT[:, k, :], 8, lambda k: [wkey, ("hT", k)])
            ACT(kTa[0][0:64, 128:128 + T], ps[b][0:64, :], AF.Copy, pk(b), [("kTa", 0)])
            ACT(kTa[1][64:128, 128:128 + T], ps[b][64:128, :], AF.Copy, pk(b), [("kTa", 1)])
            yield
            S.mark(f"m{t}.T2")
            wv, wkey = W.get("winT", 1024, 8, 136)
            for bi in range(NB):
                b = newbank()
                proj_group(b, 136, lambda k: hT[:, k, bi * 128:(bi + 1) * 128], lambda k: wv[:, k, :], 8,
                           lambda k: [wkey, ("hT", k)])
                ACT(vaug[1 + bi][:, :, 0:64], ps[b][:, 0:128].rearrange("p (a e) -> p a e", a=2), AF.Copy,
                    pk(b, 0, 136), [("vaug", 1 + bi)])
                TT(G[:, bi, :], ps[b][:, 128:136], bgate[:], ALU.add, pk(b, 0, 136) + ["bgate"], ["G"])
            yield
            S.mark(f"m{t}.gates")
            ACT(GE[:], G[:, :, 4:8], AF.Exp, ["G"], ["GE"], scale=-1.0)
            ACT(SPV[:], GE[:], AF.Ln, ["GE"], ["SPV"], bias=1.0)
            bg = newbank()
            for bi in range(NB):
                MM(ps[bg][:, bi * 4:(bi + 1) * 4], tri, SPV[:, bi, :], True, True, ["SPV", "cmatf"], pk(bg, 0, 32), False)
            for bi in range(NB):
                MM(ps[bg][:, 16 + bi * 4:16 + (bi + 1) * 4], negones, SPV[:, bi, :], True, True, ["SPV", "cmatf"],
                   pk(bg, 0, 32), bi == NB - 1)
            TT(GA[:], G[:, :, 0:4], ps[bg][:, 0:16].rearrange("p (a e) -> p a e", a=NB), ALU.subtract,
               ["G"] + pk(bg, 0, 32), ["GA"])
            ACT(WS[:], GA[:].rearrange("p a e -> p (a e)"), AF.Exp, ["GA"], ["WS"])
            ACT(CL[:], ps[bg][:, 0:16], AF.Exp, pk(bg, 0, 32), ["CL"], scale=-1.0)
            ACT(DEC[tb][:], ps[bg][:, 16:32], AF.Exp, pk(bg, 0, 32), [("DEC", tb)])
            ACT(DECQ[tb][:], DEC[tb][:], AF.Copy, [("DEC", tb)], [("DECQ", tb)], scale=float(128 ** -0.5))
            yield
            S.mark(f"m{t}.T0")
            wv, wkey = W.get("winT", 0, 8, 512)
            for bi in range(NB):
                b = newbank()
                proj_group(b, 512, lambda k: hT[:, k, bi * 128:(bi + 1) * 128], lambda k: wv[:, k, :], 8,
                           lambda k: [wkey, ("hT", k)])
                TT(vpr[bi][:, :, 0:128], ps[b][:].rearrange("p (a e) -> p a e", a=4),
                   WS[:, bi * 4:(bi + 1) * 4].unsqueeze(2).to_broadcast([128, 4, 128]), ALU.mult,
                   pk(b) + ["WS"], [("vpr", bi)])
                CP(vpr[bi][:, :, 128], WS[:, bi * 4:(bi + 1) * 4], ["WS"], [("vpr", bi)])
            yield
            S.mark(f"m{t}.T1")
            wv, wkey = W.get("winT", 512, 8, 512)
            for bi in range(NB):
                b = newbank()
                proj_group(b, 512, lambda k: hT[:, k, bi * 128:(bi + 1) * 128], lambda k: wv[:, k, :], 8,
                           lambda k: [wkey, ("hT", k)])
                ACT(sgo[bi][:], ps[b][:], AF.Tanh, pk(b), [("sgo", bi)], scale=0.5)
                TS(sgo[bi][:], sgo[bi][:], 0.5, 0.5, ALU.mult, ALU.add, [("sgo", bi)], [("sgo", bi)])
            yield

        def swa_block(t, bi):
            j = t % ntps
            blk = slice(bi * 128, (bi + 1) * 128)
            nb = j * NB + bi
            ai = bi % 2
            kbs = [("cur", 1 + bi, slice(128 + bi * 128, 256 + bi * 128), 1)]
            if nb > 0:
                kbs.append(("prev", bi, slice(bi * 128, 128 + bi * 128), 0))
            for kv in range(2):
                for (_, slot, kc, kbi) in kbs:
                    b = newbank()
                    MM(ps[b][:], kTa[kv][:, kc], qTa[:, :, blk], True, False, [("kTa", kv), "qTa"], pk(b), False)
                    MM(ps[b][:], ident[:], swab[:, kv * 2 + kbi, :], False, True, ["ident", "swab"], pk(b), True)
                    ACT(pT[kv * 2 + kbi][:], ps[b][:], AF.Exp, pk(b), [("pT", kv * 2 + kbi)])
                yield
            bos = []
            for kv in range(2):
                bo = newbank()
                bos.append(bo)
                for g in range(4):
                    for ii, (_, slot, kc, kbi) in enumerate(kbs):
                        MM(ps[bo][:, g * 65:(g + 1) * 65], pT[kv * 2 + kbi][:, g * 128:(g + 1) * 128],
                           vaug[slot][:, kv, :], ii == 0, ii == len(kbs) - 1,
                           [("pT", kv * 2 + kbi), ("vaug", slot)], pk(bo, 0, 260),
                           g == 3 and ii == len(kbs) - 1)
                yield
            for kv in range(2):
                bo = bos[kv]
                o3 = ps[bo][:, 0:260].rearrange("p (g e) -> p g e", g=4)
                TT(dn[:, kv * 4:(kv + 1) * 4], o3[:, :, 64], esink[:, kv * 4:(kv + 1) * 4], ALU.add,
                   pk(bo, 0, 260) + ["esink"], [("dn", kv)])
                S.op("dve", lambda e: e.reciprocal(out=rr[:, kv * 4:(kv + 1) * 4], in_=dn[:, kv * 4:(kv + 1) * 4]),
                     [("dn", kv)], [("rr", kv)])
                TT(att[ai][:, kv * 256:(kv + 1) * 256].rearrange("p (g e) -> p g e", g=4), o3[:, :, 0:64],
                   rr[:, kv * 4:(kv + 1) * 4].unsqueeze(2).to_broadcast([128, 4, 64]), ALU.mult,
                   pk(bo, 0, 260) + [("rr", kv)], [("att", ai)])
                yield
            bt = newbank()
            for c in range(4):
                TR(psb[bt][:, c * 128:(c + 1) * 128], att[ai][:, c * 128:(c + 1) * 128], [("att", ai)],
                   pk(bt, 0, 256), c == 3)
            ACT(mixT[:, 0:4, blk], psb[bt][:, 0:512].rearrange("p (c n) -> p c n", c=4), AF.Copy,
                pk(bt, 0, 256), [("mixT", c) for c in range(4)])
            yield

        def ml_block(t, bi):
            tb = t % 2
            blk = slice(bi * 128, (bi + 1) * 128)
            if bi == 0:
                dprev, dqprev = DEC[1 - tb][:, 12:16], DECQ[1 - tb][:, 12:16]
                dkey, dqkey = ("DEC", 1 - tb), ("DECQ", 1 - tb)
            else:
                dprev, dqprev = DEC[tb][:, (bi - 1) * 4:bi * 4], DECQ[tb][:, (bi - 1) * 4:bi * 4]
                dkey, dqkey = ("DEC", tb), ("DECQ", tb)
            mi = bi % 2
            bA = newbank()
            for h in range(4):
                TR(psb[bA][:, h * 128:(h + 1) * 128], qkT[:, 4 + h, blk], [("qkT", 4 + h)], pk(bA), h == 3)
            ACT(ktok[:].rearrange("p h d -> p (h d)"), psb[bA][:, 0:512], AF.Copy, pk(bA), ["ktok"])
            bB = newbank()
            for h in range(4):
                MM(ps[bB][:, h * 128:(h + 1) * 128], qkT[:, 4 + h, blk], qkT[:, h, blk], True, True,
                   [("qkT", 4 + h), ("qkT", h)], pk(bB), h == 3)
            TT(scb[:], ps[bB][:].rearrange("p (h t) -> p h t", h=4),
               cmatf[:, 0:1, :].to_broadcast([128, 4, 128]), ALU.mult, pk(bB) + ["cmatf"], ["scb"])
            TT(Pq[:], Q[:], dqprev.unsqueeze(2).to_broadcast([128, 4, 129]), ALU.mult, ["Q", dqkey], ["Pq"])
            yield
            bC, bD = newbank(), newbank()
            for h in range(4):
                bk = bC if h < 2 else bD
                off = (h % 2) * 129
                MM(ps[bk][:, off:off + 129], qkT[:, h, blk], Pq[:, h, :], True, False, [("qkT", h), "Pq"], pk(bk), False)
                MM(ps[bk][:, off:off + 129], scb[:, h, :], vpr[bi][:, h, :], False, True, ["scb", ("vpr", bi)], pk(bk),
                   h == 1)
            for h in range(4):
                MM(ps[bD][:, 258 + 2 * h:260 + 2 * h], qkT[:, h, blk], Pq[:, h, 127:129], True, False,
                   [("qkT", h), "Pq"], pk(bD), False)
                MM(ps[bD][:, 258 + 2 * h:260 + 2 * h], scb[:, h, :], vpr[bi][:, h, 127:129], False, True,
                   ["scb", ("vpr", bi)], pk(bD), h == 3)
            bE, bF = newbank(), newbank()
            for h in range(4):
                bk = bE if h < 2 else bF
                off = (h % 2) * 129
                MM(ps[bk][:, off:off + 129], ktok[:, h, :], vpr[bi][:, h, :], True, True, ["ktok", ("vpr", bi)], pk(bk),
                   h % 2 == 1)
            yield
            ACT(dn[:, 8:12], ps[bD][:, 258:266].rearrange("p (h two) -> p h two", two=2)[:, :, 1], AF.Abs,
                pk(bD), ["dnm"])
            TT(dn[:, 8:12], dn[:, 8:12], CL[:, bi * 4:(bi + 1) * 4], ALU.max, ["dnm", "CL"], ["dnm"])
            S.op("dve", lambda e: e.reciprocal(out=rr[:, 8:12], in_=dn[:, 8:12]), ["dnm"], ["rrm"])
            for h in range(4):
                bk = bE if h < 2 else bF
                off = (h % 2) * 129
                STT(Q[:, h, :], Q[:, h, :], dprev[:, h:h + 1], ps[bk][:, off:off + 129], ALU.mult, ALU.add,
                    ["Q", dkey] + pk(bk), ["Q"])
            yield
            for h in range(4):
                bk = bC if h < 2 else bD
                off = (h % 2) * 129
                STT(mlo[mi][:, h * 128:(h + 1) * 128], ps[bk][:, off:off + 128], rr[:, 8 + h:9 + h],
                    sgo[bi][:, h * 128:(h + 1) * 128], ALU.mult, ALU.mult,
                    pk(bk) + ["rrm", ("sgo", bi)], [("mlo", mi)])
            yield
            bt = newbank()
            for c in range(4):
                TR(psb[bt][:, c * 128:(c + 1) * 128], mlo[mi][:, c * 128:(c + 1) * 128], [("mlo", mi)],
                   pk(bt, 0, 256), c == 3)
            ACT(mixT[:, 4:8, blk], psb[bt][:, 0:512].rearrange("p (c n) -> p c n", c=4), AF.Copy,
                pk(bt, 0, 256), [("mixT", c) for c in range(4, 8)])
            yield

        def run_interleaved(gens):
            gens = list(gens)
            while gens:
                for g in list(gens):
                    try:
                        next(g)
                    except StopIteration:
                        gens.remove(g)

        def mixer_back(t):
            xb = t % 2
            S.mark(f"m{t}.blocks")
            for bi in range(NB):
                if os.environ.get('NO_IL2'):
                    for _ in swa_block(t, bi):
                        pass
                    for _ in ml_block(t, bi):
                        pass
                else:
                    run_interleaved([swa_block(t, bi), ml_block(t, bi)])
            S.mark(f"m{t}.wout")
            fs = FusedStats(xb)
            for s2 in range(2):
                wv, wkey = W.get("wout", s2 * 512, 8, 512)
                for m in range(4):
                    mm = s2 * 4 + m
                    b = newbank()
                    proj_group(b, T, lambda k: wv[:, k, m * 128:(m + 1) * 128], lambda k: mixT[:, k, :], 8,
                               lambda k: [wkey, ("mixT", k)])
                    TT(xT[xb][:, mm, :], xT[xb][:, mm, :], ps[b][:], ALU.add, pk(b) + [xk(xb, mm)], [xk(xb, mm)])
                    fs.add(mm)
                    fs.flush(3)
            fs.flush(0)

        def xattn(t):
            xb = t % 2
            if os.environ.get('NO_FUSED'):
                norm_stats(lambda _: xT[xb][:], lambda c: xk(xb, c), T)
            else:
                stats_finish(T)
            norm_apply(xb, 1)
            for s2 in range(2):
                wv, wkey = W.get("wq", s2 * 512, 8, 512)
                for m in range(4):
                    mm = s2 * 4 + m
                    b = newbank()
                    proj_group(b, T, lambda k: wv[:, k, m * 128:(m + 1) * 128], lambda k: hT[:, k, :], 8,
                               lambda k: [wkey, ("hT", k)])
                    ACT(qkT[:, mm, :], ps[b][:], AF.Copy, pk(b), [("qkT", mm)], scale=0.0625)
            for h in range(4):
                pidx = [(h % 2) * 2 + mb for mb in range(2)]
                for mb in range(2):
                    b = newbank()
                    for dc in range(2):
                        MM(ps[b][:], KTx[:, 2 * h + dc, mb * 128:(mb + 1) * 128], qkT[:, 2 * h + dc, :], dc == 0, dc == 1,
                           ["KTx", ("qkT", 2 * h + dc)], pk(b), dc == 1)
                    ACT(pT[pidx[mb]][:], ps[b][:], AF.Exp, pk(b), [("pT", pidx[mb])])
                bs = newbank()
                for mb in range(2):
                    MM(ps[bs][:], ones[:], pT[pidx[mb]][:], mb == 0, mb == 1, ["ones", ("pT", pidx[mb])], pk(bs), mb == 1)
                ri = h % 2
                ACT(lnv[:], ps[bs][:], AF.Ln, pk(bs), ["lnv"])
                ACT(rs[ri][:], lnv[:], AF.Exp, ["lnv"], [("rs", ri)], scale=-1.0)
                for ec in range(2):
                    b = newbank()
                    for mb in range(2):
                        MM(ps[b][:], Vx[mb][:, (2 * h + ec) * 128:(2 * h + ec + 1) * 128], pT[pidx[mb]][:], mb == 0, mb == 1,
                           [("Vx", mb), ("pT", pidx[mb])], pk(b), mb == 1)
                    TT(mixT[:, 2 * h + ec, :], ps[b][:], rs[ri][:], ALU.mult, pk(b) + [("rs", ri)], [("mixT", 2 * h + ec)])
            fs = FusedStats(xb)
            for s2 in range(2):
                wv, wkey = W.get("wo", s2 * 512, 8, 512)
                for m in range(4):
                    mm = s2 * 4 + m
                    b = newbank()
                    proj_group(b, T, lambda k: wv[:, k, m * 128:(m + 1) * 128], lambda k: mixT[:, k, :], 8,
                               lambda k: [wkey, ("mixT", k)])
                    TT(xT[xb][:, mm, :], xT[xb][:, mm, :], ps[b][:], ALU.add, pk(b) + [xk(xb, mm)], [xk(xb, mm)])
                    fs.add(mm)
                    fs.flush(3)
            fs.flush(0)

        def ffn(t):
            xb = t % 2
            if os.environ.get('NO_FUSED'):
                norm_stats(lambda _: xT[xb][:], lambda c: xk(xb, c), T)
            else:
                stats_finish(T)
            norm_apply(xb, 3)
            early = (t + 1 < NT) and ((t + 1) % ntps != 0) and bool(os.environ.get('USE_EARLY'))
            early_stats = (t + 1 < NT) and ((t + 1) % ntps != 0) and (not os.environ.get('NO_EARLY_STATS')) and not early
            if early_stats:
                nxb = (t + 1) % 2
                stats_full(xT[nxb][:], [xk(nxb, c) for c in range(8)], T)
                stats_finish(T, which=1)
            if early:
                nxb = (t + 1) % 2
                stats_full(xT[nxb][:], [xk(nxb, c) for c in range(8)], T)
                stats_finish(T, which=1)
            for s11 in range(11):
                wv, wkey = W.get("wup", s11 * 512, 8, 512)
                for jj in range(2):
                    jx = s11 * 2 + jj
                    bgk = newbank()
                    proj_group(bgk, T, lambda k: wv[:, k, jj * 128:(jj + 1) * 128], lambda k: hT[:, k, :], 8,
                               lambda k: [wkey, ("hT", k)])
                    buk = newbank()
                    proj_group(buk, T, lambda k: wv[:, k, 256 + jj * 128:256 + (jj + 1) * 128], lambda k: hT[:, k, :], 8,
                               lambda k: [wkey, ("hT", k)])
                    gi = jx % 3
                    gb = gbuf[gi]
                    gk = ("gbuf", gi)
                    tt = ct[jx % 2]
                    tk = ("ct", jx % 2)
                    si = jx % 2
                    ACT(gb[:, 4:T + 4], ps[bgk][:], AF.Copy, pk(bgk), [gk])
                    CP(gb[:, 0:4], ghalo[:, jx, :], [("ghalo", jx)], [gk])
                    TS(tt[:], gb[:, 2:T + 2], cfw[:, jx, 0:1], None, ALU.mult, None, [gk, "cfw"], [tk])
                    STT(tt[:], gb[:, 3:T + 3], cfw[:, jx, 1:2], tt[:], ALU.mult, ALU.add, [gk, "cfw", tk], [tk])
                    STT(tt[:], gb[:, 4:T + 4], cfw[:, jx, 2:3], tt[:], ALU.mult, ALU.add, [gk, "cfw", tk], [tk])
                    CP(ghalo[:, jx, :], gb[:, T:T + 4], [gk], [("ghalo", jx)])
                    ACT(sgt[si][:], tt[:], AF.Silu, [tk, "cfb"], [("sgt", si)], bias=cfb[:, jx:jx + 1])
                    TT(aT[:, jx, :], sgt[si][:], ps[buk][:], ALU.mult, [("sgt", si)] + pk(buk), [("aT", jx)])
            fg = None
            if early:
                norm_apply((t + 1) % 2, 0, which=1)
                fg = mixer_front(t + 1)
            fs = FusedStats(xb)
            for m in range(8):
                wv, wkey = W.get("wdn", m * 128, NJ, 128)
                b = newbank()
                proj_group(b, T, lambda k: wv[:, k, :], lambda k: aT[:, k, :], NJ, lambda k: [wkey, ("aT", k)])
                TT(xT[xb][:, m, :], xT[xb][:, m, :], ps[b][:], ALU.add, pk(b) + [xk(xb, m)], [xk(xb, m)])
                fs.add(m)
                fs.flush(2)
                if fg is not None and not os.environ.get('NO_IL1'):
                    next(fg, None)
            fs.flush(0)
            return fg

        def final(t):
            xb = t % 2
            if os.environ.get('NO_FUSED'):
                norm_stats(lambda _: xT[xb][:], lambda c: xk(xb, c), T)
            else:
                stats_finish(T)
            for c in range(8):
                STT(xT[xb][:, c, :], xT[xb][:, c, :], gains[:, 4, c:c + 1], rstd[:], ALU.mult, ALU.mult,
                    [xk(xb, c), "rstd", "gains"], [xk(xb, c)])
            S.dma("sp", f"o{xb}", outT_d[:, :, t * T:(t + 1) * T], xT[xb][:], [xk(xb, c) for c in range(8)], ())

        try:
            load_x(0)
            pending_front = None
            for t in range(NT):
                if t % ntps == 0:
                    mem_stage(t // ntps)
                if t + 1 < NT:
                    load_x(t + 1)
                S.mark(f"mixer{t}")
                if pending_front is None:
                    pending_front = mixer_front(t)
                for _ in pending_front:
                    pass
                mixer_back(t)
                S.mark(f"xattn{t}")
                xattn(t)
                S.mark(f"ffn{t}")
                pending_front = ffn(t)
                S.mark(f"final{t}")
                final(t)
        except StopBuild as _e:
            print('STOPPED AT', _e)
        for xb in range(2):
            nm = f"o{xb}"
            if nm in S.sems:
                nc.sync.wait_ge(S.sems[nm], S.cnt[nm])
        build_program.stats = (S.nops, S.nwaits, dict(S.cnt))
        build_program.marks = list(S.marks)
        build_program.wrec = list(W.rec)
    return nc


def _chunked(w):
    K, N = w.shape
    return np.ascontiguousarray(w.reshape(K // 128, 128, N).transpose(1, 0, 2))


def _consts():
    slopes = 2.0 ** (-(np.arange(8) + 1.0))
    k = np.arange(128)[:, None]
    q = np.arange(128)[None, :]
    swab = np.zeros((4, 128, 512), np.float32)
    for kv in range(2):
        for g in range(4):
            s = slopes[kv * 4 + g]
            d_prev = q + 128 - k
            prev = np.where(k > q, -s * d_prev, -30000.0)
            d_cur = q - k
            cur = np.where(k <= q, -s * d_cur, -30000.0)
            swab[kv * 2 + 0][:, g * 128:(g + 1) * 128] = prev
            swab[kv * 2 + 1][:, g * 128:(g + 1) * 128] = cur
    cm = np.zeros((5, 128, 128), np.float32)
    s_ = np.arange(128)[:, None]
    t_ = np.arange(128)[None, :]
    cm[0] = np.where(s_ <= t_, 128.0 ** -0.5, 0.0)
    cm[1] = np.where(s_ <= t_, -1.0, 0.0)
    cm[2] = -1.0
    cm[3] = np.eye(128)
    cm[4] = 1.0
    return swab, cm


def _shared_inputs(inp):
    f = lambda a: np.asarray(a, dtype=np.float32)
    w_in = f(inp["w_in"])[0]
    o0, o1, o2 = 512, 640, 768
    o3, o4, o5 = o2 + 1024, o2 + 1536, o2 + 2048
    qcols = []
    for c in range(4):
        for kv in range(2):
            h = kv * 4 + c
            qcols += list(range(h * 64, (h + 1) * 64))
    colsF = qcols + list(range(o2, o2 + 512)) + list(range(o2 + 512, o3)) + list(range(o0, o1))
    colsT = list(range(o3, o4)) + list(range(o4, o5)) + list(range(o1, o2)) + list(range(o5, o5 + 8))
    w_up = f(inp["w_up"])[0]
    upcols = []
    for s in range(11):
        for jj in range(2):
            j = 2 * s + jj
            upcols += list(range(j * 128, (j + 1) * 128))
        for jj in range(2):
            j = 2 * s + jj
            upcols += list(range(DFF + j * 128, DFF + (j + 1) * 128))
    gains = np.stack([f(inp["norm_mix_g"])[0], f(inp["norm_xattn_g"])[0], f(inp["norm_mem_g"])[0],
                      f(inp["norm_ffn_g"])[0], f(inp["norm_final_g"])], 0)
    gains = np.ascontiguousarray(gains.reshape(5, 8, 128).transpose(2, 0, 1))
    swab, cm = _consts()
    sh = {
        "w_inF": _chunked(w_in[:, colsF]),
        "w_inT": _chunked(w_in[:, colsT]),
        "w_out": _chunked(f(inp["w_out"])[0]),
        "wq": _chunked(f(inp["wq_x"])[0]),
        "wkv": _chunked(f(inp["wkv_x"])[0]),
        "wo": _chunked(f(inp["wo_x"])[0]),
        "w_up": _chunked(w_up[:, upcols]),
        "w_down": _chunked(f(inp["w_down"])[0]),
        "gains": gains,
        "cqw": np.ascontiguousarray(f(inp["conv_qk_w"])[0].reshape(4, 8, 128).transpose(2, 1, 0)),
        "cqb": np.ascontiguousarray(f(inp["conv_qk_b"])[0].reshape(8, 128).T),
        "cfw": np.ascontiguousarray(f(inp["conv_ffn_w"])[0].reshape(3, NJ, 128).transpose(2, 1, 0)),
        "cfb": np.ascontiguousarray(f(inp["conv_ffn_b"])[0].reshape(NJ, 128).T),
        "bgate": np.ascontiguousarray(f(inp["b_gate_if"])[0]),
        "sinks": np.ascontiguousarray(f(inp["attn_sinks"])[0]),
        "swab": swab,
        "cmat": cm,
    }
    return sh


def _core_inputs(x, mem, nseq, ntok_per_seq):
    xs = x[:, :ntok_per_seq, :].reshape(nseq * ntok_per_seq, D)
    xT = np.ascontiguousarray(xs.T.reshape(8, 128, -1).transpose(1, 0, 2))
    memT = np.ascontiguousarray(mem.transpose(0, 2, 1).reshape(nseq, 8, 128, MEM).transpose(0, 2, 1, 3))
    return {"xT": xT, "memT": memT}


def build_two_pass(nseq, ntps):
    build_program(nseq, ntps)
    return build_program(nseq, ntps, wplan=list(build_program.wrec))


_NC_CACHE = {}


def kernel(**inputs):
    x = np.asarray(inputs["x"], dtype=np.float32)
    mem = np.asarray(inputs["mem"], dtype=np.float32)
    nseq = BATCH // NCORES
    ntps = SEQ // T
    key = (nseq, ntps)
    if key not in _NC_CACHE:
        _NC_CACHE[key] = build_two_pass(nseq, ntps)
    nc = _NC_CACHE[key]
    sh = _shared_inputs(inputs)
    in_maps = []
    for c in range(NCORES):
        m = dict(sh)
        m.update(_core_inputs(x[c * nseq:(c + 1) * nseq], mem[c * nseq:(c + 1) * nseq], nseq, SEQ))
        in_maps.append(m)
    res = run_bass_kernel_spmd(nc, in_maps, core_ids=list(range(NCORES)))
    out = np.empty((BATCH, SEQ, D), np.float32)
    for c in range(NCORES):
        oT = np.asarray(res.results[c]["outT"], dtype=np.float32)
        o = oT.transpose(2, 1, 0).reshape(nseq, SEQ, D)
        out[c * nseq:(c + 1) * nseq] = o
    return out
```

```python
import os
import numpy as np
import ml_dtypes
from contextlib import ExitStack
import concourse.bass as bass
import concourse.mybir as mybir
from concourse.bass_utils import run_bass_kernel_spmd

F32 = mybir.dt.float32
BF16 = mybir.dt.bfloat16
ALU = mybir.AluOpType
AF = mybir.ActivationFunctionType

D = 1024
SEQ = 2048
BATCH = 16
NCORES = 8
T = 512
NB = 4
DFF = 2816
NJ = 22
MEM = 256
EPS = 1e-6
NSLAB = 3
SLAB_ELEMS = 4096


class StopBuild(Exception):
    pass


class Sched:
    def __init__(self, nc, es):
        self.nc = nc
        self.es = es
        self.engs = {"pe": nc.tensor, "act": nc.scalar, "dve": nc.vector, "pool": nc.gpsimd, "sp": nc.sync}
        self.sems = {}
        self.cnt = {}
        self.seen = {e: {} for e in self.engs}
        self.w = {}
        self.r = {}
        for e in ("pe", "act", "dve", "pool"):
            self.new_sem(e)
        self.nwaits = 0
        self.nops = 0
        self.marks = []
        self._pending = None

    def new_sem(self, name):
        self.sems[name] = self.es.enter_context(self.nc.semaphore("s_" + name))
        self.cnt[name] = 0

    def _deps(self, eng, reads, writes):
        deps = []
        pe = (eng == "pe")
        for k in reads:
            t = self.w.get(k)
            if t is not None and not (pe and t[2] == "pe"):
                deps.append(t)
        for k in writes:
            t = self.w.get(k)
            if t is not None and not (pe and t[2] == "pe"):
                deps.append(t)
            for t in self.r.get(k, ()):
                if not (pe and t[2] == "pe"):
                    deps.append(t)
        return deps

    def _wait(self, eng, deps):
        need = {}
        for (s, v, _) in deps:
            if v > need.get(s, 0):
                need[s] = v
        for s, v in need.items():
            if self.seen[eng].get(s, 0) < v:
                assert v <= self.cnt[s], ("wait on unsignalled", eng, s, v, self.cnt[s])
                self.engs[eng].wait_ge(self.sems[s], v)
                self.seen[eng][s] = v
                self.nwaits += 1

    def _record(self, tok, reads, writes):
        for k in reads:
            self.r.setdefault(k, []).append(tok)
        for k in writes:
            self.w[k] = tok
            self.r[k] = []

    def op(self, eng, fn, reads=(), writes=(), signal=True):
        self._wait(eng, self._deps(eng, reads, writes))
        ins = fn(self.engs[eng])
        self.nops += 1
        if self._pending is not None:
            self.marks.append((self._pending, ins.ins.name))
            self._pending = None
        if signal:
            self.cnt[eng] += 1
            ins.then_inc(self.sems[eng], 1)
            tok = (eng, self.cnt[eng], eng)
        else:
            tok = (eng, self.cnt[eng] + 1, eng)
        self._record(tok, reads, writes)
        return ins

    def mark(self, label):
        if os.environ.get('STOP_AT') == label:
            raise StopBuild(label)
        self._pending = label

    def dma(self, issuer, sem, out, in_, reads=(), writes=()):
        if sem not in self.sems:
            self.new_sem(sem)
        self._wait(issuer, self._deps("dma:" + sem, reads, writes))
        ins = self.engs[issuer].dma_start(out=out, in_=in_)
        self.cnt[sem] += 16
        ins.then_inc(self.sems[sem], 16)
        tok = (sem, self.cnt[sem], "dma:" + sem)
        self._record(tok, reads, writes)
        return ins


def build_program(nseq, ntps, dbg=False, wplan=None):
    NT = nseq * ntps
    NTOK = NT * T
    nc = bass.Bass("TRN2", target_bir_lowering=False)

    def din(name, shape):
        return nc.dram_tensor(name, list(shape), F32, kind="ExternalInput").ap()

    xT_d = din("xT", [128, 8, NTOK])
    memT_d = din("memT", [nseq, 128, 8, MEM])
    winF_d = din("w_inF", [128, 8, 1664])
    winT_d = din("w_inT", [128, 8, 1160])
    wout_d = din("w_out", [128, 8, 1024])
    wq_d = din("wq", [128, 8, 1024])
    wkv_d = din("wkv", [128, 8, 2048])
    wo_d = din("wo", [128, 8, 1024])
    wup_d = din("w_up", [128, 8, 2 * DFF])
    wdn_d = din("w_down", [128, NJ, 1024])
    gains_d = din("gains", [128, 5, 8])
    cqw_d = din("cqw", [128, 8, 4])
    cqb_d = din("cqb", [128, 8])
    cfw_d = din("cfw", [128, NJ, 3])
    cfb_d = din("cfb", [128, NJ])
    bgate_d = din("bgate", [8])
    sinks_d = din("sinks", [8])
    swab_d = din("swab", [4, 128, 512])
    cmat_d = din("cmat", [5, 128, 128])
    outT_d = nc.dram_tensor("outT", [128, 8, NTOK], F32, kind="ExternalOutput").ap()
    if dbg:
        dbg_d = nc.dram_tensor("dbg", [128, 8, NTOK], F32, kind="ExternalOutput").ap()

    with ExitStack() as es:
        S = Sched(nc, es)

        def sb(name, shape, dt):
            return es.enter_context(nc.sbuf_tensor("sb_" + name, list(shape), dt))

        xT = [sb(f"xT{i}", [128, 8, T], F32) for i in range(2)]
        hT = sb("hT", [128, 8, T], BF16)
        mixT = sb("mixT", [128, 8, T], BF16)
        lnv = sb("lnv", [128, T], F32)
        rstd = sb("rstd", [128, T], F32)
        lnv2 = sb("lnv2", [128, T], F32)
        rstd2 = sb("rstd2", [128, T], F32)
        sqb = sb("sqb", [128, 8, T], BF16)
        qTa = sb("qTa", [128, 4, T], BF16)
        kTa = [sb(f"kTa{i}", [128, 128 + T], BF16) for i in range(2)]
        pre = sb("pre", [128, 8, T + 3], F32)
        ct = [sb(f"ct{i}", [128, T], F32) for i in range(2)]
        qkT = sb("qkT", [128, 8, T], BF16)
        vaug = [sb(f"vaug{i}", [128, 2, 65], BF16) for i in range(NB + 1)]
        vpr = [sb(f"vpr{i}", [128, 4, 129], BF16) for i in range(NB)]
        sgo = [sb(f"sgo{i}", [128, T], BF16) for i in range(NB)]
        G = sb("G", [128, NB, 8], F32)
        GE = sb("GE", [128, NB, 4], F32)
        SPV = sb("SPV", [128, NB, 4], F32)
        GA = sb("GA", [128, NB, 4], F32)
        WS = sb("WS", [128, NB * 4], F32)
        CL = sb("CL", [128, NB * 4], F32)
        DEC = [sb(f"DEC{i}", [128, NB * 4], F32) for i in range(2)]
        DECQ = [sb(f"DECQ{i}", [128, NB * 4], F32) for i in range(2)]
        ktok = sb("ktok", [128, 4, 128], BF16)
        scb = sb("scb", [128, 4, 128], BF16)
        Q = sb("Q", [128, 4, 129], F32)
        Pq = sb("Pq", [128, 4, 129], BF16)
        dn = sb("dn", [128, 16], F32)
        rr = sb("rr", [128, 16], F32)
        mlo = [sb(f"mlo{i}", [128, T], BF16) for i in range(2)]
        att = [sb(f"att{i}", [128, T], BF16) for i in range(2)]
        pT = [sb(f"pT{i}", [128, T], BF16) for i in range(4)]
        KTx = sb("KTx", [128, 8, MEM], BF16)
        Vx = [sb(f"Vx{i}", [128, 1024], BF16) for i in range(2)]
        rs = [sb(f"rs{i}", [128, T], F32) for i in range(2)]
        aT = sb("aT", [128, NJ, T], BF16)
        gbuf = [sb(f"gbuf{i}", [128, T + 4], F32) for i in range(3)]
        sgt = [sb(f"sgt{i}", [128, T], F32) for i in range(2)]
        ghalo = sb("ghalo", [128, NJ, 4], F32)
        slab = [sb(f"slab{i}", [128, SLAB_ELEMS], BF16) for i in range(NSLAB)]
        swab = sb("swab", [128, 4, 512], BF16)
        cmatf = sb("cmatf", [128, 5, 128], F32)
        ident = sb("ident", [128, 128], BF16)
        ones = sb("ones", [128, 128], BF16)
        onesdiv = sb("onesdiv", [128, 128], BF16)
        gains = sb("gains", [128, 5, 8], F32)
        cqw = sb("cqw", [128, 8, 4], F32)
        cqb = sb("cqb", [128, 8], F32)
        cfw = sb("cfw", [128, NJ, 3], F32)
        cfb = sb("cfb", [128, NJ], F32)
        bgate = sb("bgate", [128, 8], F32)
        esink = sb("esink", [128, 8], F32)
        mmask = cmatf[:, 0, :]
        tri = cmatf[:, 1, :]
        negones = cmatf[:, 2, :]

        ps = [es.enter_context(nc.psum_tensor(f"ps{i}", [128, 512], F32)) for i in range(8)]
        psb = [p[:].bitcast(BF16) for p in ps]
        bank_ctr = [0]

        def newbank():
            b = bank_ctr[0] % 7
            bank_ctr[0] += 1
            return b

        def pk(bank, lo=0, hi=512):
            return [("ps", bank)]

        def ACT(out, in_, func, reads, writes, bias=None, scale=None):
            kw = {}
            if bias is not None:
                kw["bias"] = bias
            if scale is not None:
                kw["scale"] = scale
            return S.op("act", lambda e: e.activation(out=out, in_=in_, func=func, **kw), reads, writes)

        def TT(out, in0, in1, op, reads, writes, eng="dve"):
            return S.op(eng, lambda e: e.tensor_tensor(out=out, in0=in0, in1=in1, op=op), reads, writes)

        def TS(out, in0, s1, s2, op0, op1, reads, writes, eng="dve"):
            if s2 is None:
                return S.op(eng, lambda e: e.tensor_scalar(out=out, in0=in0, scalar1=s1, scalar2=None, op0=op0), reads, writes)
            return S.op(eng, lambda e: e.tensor_scalar(out=out, in0=in0, scalar1=s1, scalar2=s2, op0=op0, op1=op1), reads, writes)

        def STT(out, in0, scalar, in1, op0, op1, reads, writes):
            return S.op("dve", lambda e: e.scalar_tensor_tensor(out=out, in0=in0, scalar=scalar, in1=in1, op0=op0, op1=op1), reads, writes)

        def CP(out, in_, reads, writes, eng="dve"):
            return S.op(eng, lambda e: e.tensor_copy(out=out, in_=in_), reads, writes)

        def MS(ap, val, writes, eng="dve"):
            return S.op(eng, lambda e: e.memset(ap, val), (), writes)

        def MM(out, lhsT, rhs, start, stop, reads, writes, signal):
            return S.op("pe", lambda e: e.matmul(out, lhsT=lhsT, rhs=rhs, start=start, stop=stop), reads, writes, signal=signal)

        def TR(out, in_, reads, writes, signal):
            return S.op("pe", lambda e: e.transpose(out, in_, ident[:]), list(reads) + ["ident"], writes, signal=signal)

        S.dma("sp", "c0", cmatf[:], cmat_d.rearrange("c p f -> p c f"), (), ["cmatf"])
        S.dma("pool", "c1", swab[:], swab_d.rearrange("c p f -> p c f"), (), ["swab"])
        S.dma("sp", "c2", gains[:], gains_d, (), ["gains"])
        S.dma("sp", "c3", cqw[:], cqw_d, (), ["cqw"])
        S.dma("sp", "c4", cqb[:], cqb_d, (), ["cqb"])
        S.dma("sp", "c5", cfw[:], cfw_d, (), ["cfw"])
        S.dma("sp", "c6", cfb[:], cfb_d, (), ["cfb"])
        S.dma("sp", "c7", bgate[:], bgate_d.partition_broadcast(128), (), ["bgate"])
        S.dma("sp", "c8", esink[:], sinks_d.partition_broadcast(128), (), ["esink"])
        CP(ident[:], cmatf[:, 3, :], ["cmatf"], ["ident"])
        CP(ones[:], cmatf[:, 4, :], ["cmatf"], ["ones"])
        TS(onesdiv[:], cmatf[:, 4, :], 1.0 / D, None, ALU.mult, None, ["cmatf"], ["onesdiv"])
        ACT(esink[:], esink[:], AF.Exp, ["esink"], ["esink"])
        for i in range(NB + 1):
            MS(vaug[i][:], 1.0, [("vaug", i)])
        for i in range(2):
            MS(kTa[i][:], 0.0, [("kTa", i)])
            MS(DEC[i][:], 0.0, [("DEC", i)])
            MS(DECQ[i][:], 0.0, [("DECQ", i)])

        class WStream:
            def __init__(self, plan):
                self.plan = plan
                self.req = []
                self.rec = []
                self.issued = 0
                self.cur = 0

            def view(self, i, kdim, ncols):
                return slab[i % NSLAB][:, 0:kdim * ncols].rearrange("p (k n) -> p k n", k=kdim)

            def _issue(self, i, spec):
                name, c0, kdim, ncols = spec
                b = i % NSLAB
                S.dma("pool", f"ws{b}", self.view(i, kdim, ncols), WSRC[name][:, :, c0:c0 + ncols], (), [("slab", b)])

            def get(self, name, c0, kdim, ncols):
                spec = (name, c0, kdim, ncols)
                i = self.cur
                self.rec.append(spec)
                if self.plan is None:
                    self._issue(i, spec)
                else:
                    assert self.plan[i] == spec, (i, self.plan[i], spec)
                    while self.issued < min(len(self.plan), i + NSLAB):
                        self._issue(self.issued, self.plan[self.issued])
                        self.issued += 1
                self.cur += 1
                return self.view(i, kdim, ncols), ("slab", i % NSLAB)

        WSRC = {"wkv": wkv_d, "winF": winF_d, "winT": winT_d, "wout": wout_d, "wq": wq_d, "wo": wo_d,
                "wup": wup_d, "wdn": wdn_d}
        W = WStream(wplan)

        def xk(xb, c):
            return ("x", xb, c)

        SB7 = [("ps", 7)]

        def stats_finish(n, which=0):
            lv, rv = (lnv, rstd) if which == 0 else (lnv2, rstd2)
            lk, rk = ("lnv", "rstd") if which == 0 else ("lnv2", "rstd2")
            ACT(lv[:, 0:n], ps[7][:, 0:n], AF.Ln, SB7 + ["epsb"], [lk], bias=epsb[:, 0:1])
            ACT(rv[:, 0:n], lv[:, 0:n], AF.Exp, [lk], [rk], scale=-0.5)

        def stats_full(src_ap, src_keys, n):
            S.op("act", lambda e: e.activation(out=sqb[:, :, 0:n], in_=src_ap, func=AF.Square),
                 src_keys, [("sqb", c) for c in range(8)])
            for c in range(8):
                MM(ps[7][:, 0:n], onesdiv[:], sqb[:, c, 0:n], c == 0, c == 7, [("sqb", c), "onesdiv"], SB7, c == 7)

        def norm_stats(src_ap_fn, src_keys_fn, n):
            stats_full(src_ap_fn(None), [src_keys_fn(c) for c in range(8)], n)
            stats_finish(n)

        class FusedStats:
            def __init__(self, xb):
                self.xb = xb
                self.pending = []

            def add(self, mm):
                if os.environ.get('NO_FUSED'):
                    return
                ACT(sqb[:, mm, :], xT[self.xb][:, mm, :], AF.Square, [xk(self.xb, mm)], [("sqb", mm)])
                self.pending.append(mm)

            def flush(self, keep):
                if os.environ.get('NO_FUSED'):
                    return
                while len(self.pending) > keep:
                    c = self.pending.pop(0)
                    MM(ps[7][:], onesdiv[:], sqb[:, c, :], c == 0, c == 7, [("sqb", c), "onesdiv"], SB7, c == 7)

        def norm_apply(xb, gi, which=0):
            rv, rk = (rstd, "rstd") if which == 0 else (rstd2, "rstd2")
            for c in range(8):
                STT(hT[:, c, :], xT[xb][:, c, :], gains[:, gi, c:c + 1], rv[:], ALU.mult, ALU.mult,
                    [xk(xb, c), rk, "gains"], [("hT", c)])

        def proj_group(b, n, lhs_fn, rhs_fn, nk, reads_fn, width=None):
            for k in range(nk):
                MM(ps[b][:, 0:n], lhs_fn(k), rhs_fn(k), k == 0, k == nk - 1, reads_fn(k), pk(b, 0, n), k == nk - 1)

        epsb = sb("epsb", [128, 8], F32)
        MS(epsb[:], EPS, ["epsb"])

        def load_x(t):
            xb = t % 2
            S.dma("sp", f"x{xb}", xT[xb][:], xT_d[:, :, t * T:(t + 1) * T], (), [xk(xb, c) for c in range(8)])

        def mem_stage(s):
            S.dma("sp", "mem", pre[:, :, 0:MEM], memT_d[s], (), [("pre", c) for c in range(8)])
            norm_stats(lambda _: pre[:, :, 0:MEM], lambda c: ("pre", c), MEM)
            for c in range(8):
                STT(hT[:, c, 0:MEM], pre[:, c, 0:MEM], gains[:, 2, c:c + 1], rstd[:, 0:MEM], ALU.mult, ALU.mult,
                    [("pre", c), "rstd", "gains"], [("hT", c)])
            for s4 in range(4):
                wv, wkey = W.get("wkv", s4 * 512, 8, 512)
                if s4 < 2:
                    for m in range(4):
                        b = newbank()
                        proj_group(b, MEM, lambda k: wv[:, k, m * 128:(m + 1) * 128], lambda k: hT[:, k, 0:MEM], 8,
                                   lambda k: [wkey, ("hT", k)])
                        ACT(KTx[:, s4 * 4 + m, :], ps[b][:, 0:MEM], AF.Copy, pk(b, 0, MEM), ["KTx"])
                else:
                    for mb in range(2):
                        b = newbank()
                        proj_group(b, 512, lambda k: hT[:, k, mb * 128:(mb + 1) * 128], lambda k: wv[:, k, :], 8,
                                   lambda k: [wkey, ("hT", k)])
                        ACT(Vx[mb][:, (s4 - 2) * 512:(s4 - 1) * 512], ps[b][:], AF.Copy, pk(b), [("Vx", mb)])

        def mixer_front(t):
            xb = t % 2
            j = t % ntps
            first = (j == 0)
            tb = t % 2
            if first:
                MS(pre[:, :, 0:3], 0.0, [("pre", c) for c in range(8)])
                MS(Q[:], 0.0, ["Q"])
                MS(ghalo[:], 0.0, [("ghalo", jx_) for jx_ in range(NJ)])
            else:
                CP(pre[:, :, 0:3], pre[:, :, T:T + 3], [("pre", c) for c in range(8)], [("pre", c) for c in range(8)])
                CP(kTa[0][0:64, 0:128], kTa[0][0:64, T:T + 128], [("kTa", 0)], [("kTa", 0)])
                CP(kTa[1][64:128, 0:128], kTa[1][64:128, T:T + 128], [("kTa", 1)], [("kTa", 1)])
                CP(vaug[0][:], vaug[NB][:], [("vaug", NB)], [("vaug", 0)])
            if first or t == 0 or not os.environ.get('USE_EARLY'):
                norm_stats(lambda _: xT[xb][:], lambda c: xk(xb, c), T)
                norm_apply(xb, 0)
            S.mark(f"m{t}.F0")
            wv, wkey = W.get("winF", 0, 8, 512)
            for m in range(4):
                b = newbank()
                proj_group(b, T, lambda k: wv[:, k, m * 128:(m + 1) * 128], lambda k: hT[:, k, :], 8,
                           lambda k: [wkey, ("hT", k)])
                ACT(qTa[:, m, :], ps[b][:], AF.Copy, pk(b), ["qTa"], scale=0.125)
            yield
            S.mark(f"m{t}.F12")
            for f in range(2):
                wv, wkey = W.get("winF", 512 + f * 512, 8, 512)
                for m in range(4):
                    c = f * 4 + m
                    b = newbank()
                    proj_group(b, T, lambda k: wv[:, k, m * 128:(m + 1) * 128], lambda k: hT[:, k, :], 8,
                               lambda k: [wkey, ("hT", k)])
                    ACT(pre[:, c, 3:T + 3], ps[b][:], AF.Copy, pk(b), [("pre", c)])
                    tt = ct[c % 2]
                    tk = ("ct", c % 2)
                    TS(tt[:], pre[:, c, 0:T], cqw[:, c, 0:1], None, ALU.mult, None, [("pre", c), "cqw"], [tk])
                    STT(tt[:], pre[:, c, 1:T + 1], cqw[:, c, 1:2], tt[:], ALU.mult, ALU.add, [("pre", c), "cqw", tk], [tk])
                    STT(tt[:], pre[:, c, 2:T + 2], cqw[:, c, 2:3], tt[:], ALU.mult, ALU.add, [("pre", c), "cqw", tk], [tk])
                    STT(tt[:], pre[:, c, 3:T + 3], cqw[:, c, 3:4], tt[:], ALU.mult, ALU.add, [("pre", c), "cqw", tk], [tk])
                    ACT(qkT[:, c, :], tt[:], AF.Silu, [tk, "cqb"], [("qkT", c)], bias=cqb[:, c:c + 1])
                yield
            S.mark(f"m{t}.F3")
            wv, wkey = W.get("winF", 1536, 8, 128)
            b = newbank()
            proj_group(b, T, lambda k: wv[:, k, 0:128], lambda k: hT[:, k, :], 8, lambda k: [wkey, ("hT", k)])
            ACT(kTa[0][0:64, 128:128 + T], ps[b][0:64, :], AF.Copy, pk(b), [("kTa", 0)])
            ACT(kTa[1][64:128, 128:128 + T], ps[b][64:128, :], AF.Copy, pk(b), [("kTa", 1)])
            yield
            S.mark(f"m{t}.T2")
            wv, wkey = W.get("winT", 1024, 8, 136)
            for bi in range(NB):
                b = newbank()
                proj_group(b, 136, lambda k: hT[:, k, bi * 128:(bi + 1) * 128], lambda k: wv[:, k, :], 8,
                           lambda k: [wkey, ("hT", k)])
                ACT(vaug[1 + bi][:, :, 0:64], ps[b][:, 0:128].rearrange("p (a e) -> p a e", a=2), AF.Copy,
                    pk(b, 0, 136), [("vaug", 1 + bi)])
                TT(G[:, bi, :], ps[b][:, 128:136], bgate[:], ALU.add, pk(b, 0, 136) + ["bgate"], ["G"])
            yield
            S.mark(f"m{t}.gates")
            ACT(GE[:], G[:, :, 4:8], AF.Exp, ["G"], ["GE"], scale=-1.0)
            ACT(SPV[:], GE[:], AF.Ln, ["GE"], ["SPV"], bias=1.0)
            bg = newbank()
            for bi in range(NB):
                MM(ps[bg][:, bi * 4:(bi + 1) * 4], tri, SPV[:, bi, :], True, True, ["SPV", "cmatf"], pk(bg, 0, 32), False)
            for bi in range(NB):
                MM(ps[bg][:, 16 + bi * 4:16 + (bi + 1) * 4], negones, SPV[:, bi, :], True, True, ["SPV", "cmatf"],
                   pk(bg, 0, 32), bi == NB - 1)
            TT(GA[:], G[:, :, 0:4], ps[bg][:, 0:16].rearrange("p (a e) -> p a e", a=NB), ALU.subtract,
               ["G"] + pk(bg, 0, 32), ["GA"])
            ACT(WS[:], GA[:].rearrange("p a e -> p (a e)"), AF.Exp, ["GA"], ["WS"])
            ACT(CL[:], ps[bg][:, 0:16], AF.Exp, pk(bg, 0, 32), ["CL"], scale=-1.0)
            ACT(DEC[tb][:], ps[bg][:, 16:32], AF.Exp, pk(bg, 0, 32), [("DEC", tb)])
            ACT(DECQ[tb][:], DEC[tb][:], AF.Copy, [("DEC", tb)], [("DECQ", tb)], scale=float(128 ** -0.5))
            yield
            S.mark(f"m{t}.T0")
            wv, wkey = W.get("winT", 0, 8, 512)
            for bi in range(NB):
                b = newbank()
                proj_group(b, 512, lambda k: hT[:, k, bi * 128:(bi + 1) * 128], lambda k: wv[:, k, :], 8,
                           lambda k: [wkey, ("hT", k)])
                TT(vpr[bi][:, :, 0:128], ps[b][:].rearrange("p (a e) -> p a e", a=4),
                   WS[:, bi * 4:(bi + 1) * 4].unsqueeze(2).to_broadcast([128, 4, 128]), ALU.mult,
                   pk(b) + ["WS"], [("vpr", bi)])
                CP(vpr[bi][:, :, 128], WS[:, bi * 4:(bi + 1) * 4], ["WS"], [("vpr", bi)])
            yield
            S.mark(f"m{t}.T1")
            wv, wkey = W.get("winT", 512, 8, 512)
            for bi in range(NB):
                b = newbank()
                proj_group(b, 512, lambda k: hT[:, k, bi * 128:(bi + 1) * 128], lambda k: wv[:, k, :], 8,
                           lambda k: [wkey, ("hT", k)])
                ACT(sgo[bi][:], ps[b][:], AF.Tanh, pk(b), [("sgo", bi)], scale=0.5)
                TS(sgo[bi][:], sgo[bi][:], 0.5, 0.5, ALU.mult, ALU.add, [("sgo", bi)], [("sgo", bi)])
            yield

        def swa_block(t, bi):
            j = t % ntps
            blk = slice(bi * 128, (bi + 1) * 128)
            nb = j * NB + bi
            ai = bi % 2
            kbs = [("cur", 1 + bi, slice(128 + bi * 128, 256 + bi * 128), 1)]
            if nb > 0:
                kbs.append(("prev", bi, slice(bi * 128, 128 + bi * 128), 0))
            for kv in range(2):
                for (_, slot, kc, kbi) in kbs:
                    b = newbank()
                    MM(ps[b][:], kTa[kv][:, kc], qTa[:, :, blk], True, False, [("kTa", kv), "qTa"], pk(b), False)
                    MM(ps[b][:], ident[:], swab[:, kv * 2 + kbi, :], False, True, ["ident", "swab"], pk(b), True)
                    ACT(pT[kv * 2 + kbi][:], ps[b][:], AF.Exp, pk(b), [("pT", kv * 2 + kbi)])
                yield
            bos = []
            for kv in range(2):
                bo = newbank()
                bos.append(bo)
                for g in range(4):
                    for ii, (_, slot, kc, kbi) in enumerate(kbs):
                        MM(ps[bo][:, g * 65:(g + 1) * 65], pT[kv * 2 + kbi][:, g * 128:(g + 1) * 128],
                           vaug[slot][:, kv, :], ii == 0, ii == len(kbs) - 1,
                           [("pT", kv * 2 + kbi), ("vaug", slot)], pk(bo, 0, 260),
                           g == 3 and ii == len(kbs) - 1)
                yield
            for kv in range(2):
                bo = bos[kv]
                o3 = ps[bo][:, 0:260].rearrange("p (g e) -> p g e", g=4)
                TT(dn[:, kv * 4:(kv + 1) * 4], o3[:, :, 64], esink[:, kv * 4:(kv + 1) * 4], ALU.add,
                   pk(bo, 0, 260) + ["esink"], [("dn", kv)])
                S.op("dve", lambda e: e.reciprocal(out=rr[:, kv * 4:(kv + 1) * 4], in_=dn[:, kv * 4:(kv + 1) * 4]),
                     [("dn", kv)], [("rr", kv)])
                TT(att[ai][:, kv * 256:(kv + 1) * 256].rearrange("p (g e) -> p g e", g=4), o3[:, :, 0:64],
                   rr[:, kv * 4:(kv + 1) * 4].unsqueeze(2).to_broadcast([128, 4, 64]), ALU.mult,
                   pk(bo, 0, 260) + [("rr", kv)], [("att", ai)])
                yield
            bt = newbank()
            for c in range(4):
                TR(psb[bt][:, c * 128:(c + 1) * 128], att[ai][:, c * 128:(c + 1) * 128], [("att", ai)],
                   pk(bt, 0, 256), c == 3)
            ACT(mixT[:, 0:4, blk], psb[bt][:, 0:512].rearrange("p (c n) -> p c n", c=4), AF.Copy,
                pk(bt, 0, 256), [("mixT", c) for c in range(4)])
            yield

        def ml_block(t, bi):
            tb = t % 2
            blk = slice(bi * 128, (bi + 1) * 128)
            if bi == 0:
                dprev, dqprev = DEC[1 - tb][:, 12:16], DECQ[1 - tb][:, 12:16]
                dkey, dqkey = ("DEC", 1 - tb), ("DECQ", 1 - tb)
            else:
                dprev, dqprev = DEC[tb][:, (bi - 1) * 4:bi * 4], DECQ[tb][:, (bi - 1) * 4:bi * 4]
                dkey, dqkey = ("DEC", tb), ("DECQ", tb)
            mi = bi % 2
            bA = newbank()
            for h in range(4):
                TR(psb[bA][:, h * 128:(h + 1) * 128], qkT[:, 4 + h, blk], [("qkT", 4 + h)], pk(bA), h == 3)
            ACT(ktok[:].rearrange("p h d -> p (h d)"), psb[bA][:, 0:512], AF.Copy, pk(bA), ["ktok"])
            bB = newbank()
            for h in range(4):
                MM(ps[bB][:, h * 128:(h + 1) * 128], qkT[:, 4 + h, blk], qkT[:, h, blk], True, True,
                   [("qkT", 4 + h), ("qkT", h)], pk(bB), h == 3)
            TT(scb[:], ps[bB][:].rearrange("p (h t) -> p h t", h=4),
               cmatf[:, 0:1, :].to_broadcast([128, 4, 128]), ALU.mult, pk(bB) + ["cmatf"], ["scb"])
            TT(Pq[:], Q[:], dqprev.unsqueeze(2).to_broadcast([128, 4, 129]), ALU.mult, ["Q", dqkey], ["Pq"])
            yield
            bC, bD = newbank(), newbank()
            for h in range(4):
                bk = bC if h < 2 else bD
                off = (h % 2) * 129
                MM(ps[bk][:, off:off + 129], qkT[:, h, blk], Pq[:, h, :], True, False, [("qkT", h), "Pq"], pk(bk), False)
                MM(ps[bk][:, off:off + 129], scb[:, h, :], vpr[bi][:, h, :], False, True, ["scb", ("vpr", bi)], pk(bk),
                   h == 1)
            for h in range(4):
                MM(ps[bD][:, 258 + 2 * h:260 + 2 * h], qkT[:, h, blk], Pq[:, h, 127:129], True, False,
                   [("qkT", h), "Pq"], pk(bD), False)
                MM(ps[bD][:, 258 + 2 * h:260 + 2 * h], scb[:, h, :], vpr[bi][:, h, 127:129], False, True,
                   ["scb", ("vpr", bi)], pk(bD), h == 3)
            bE, bF = newbank(), newbank()
            for h in range(4):
                bk = bE if h < 2 else bF
                off = (h % 2) * 129
                MM(ps[bk][:, off:off + 129], ktok[:, h, :], vpr[bi][:, h, :], True, True, ["ktok", ("vpr", bi)], pk(bk),
                   h % 2 == 1)
            yield
            ACT(dn[:, 8:12], ps[bD][:, 258:266].rearrange("p (h two) -> p h two", two=2)[:, :, 1], AF.Abs,
                pk(bD), ["dnm"])
            TT(dn[:, 8:12], dn[:, 8:12], CL[:, bi * 4:(bi + 1) * 4], ALU.max, ["dnm", "CL"], ["dnm"])
            S.op("dve", lambda e: e.reciprocal(out=rr[:, 8:12], in_=dn[:, 8:12]), ["dnm"], ["rrm"])
            for h in range(4):
                bk = bE if h < 2 else bF
                off = (h % 2) * 129
                STT(Q[:, h, :], Q[:, h, :], dprev[:, h:h + 1], ps[bk][:, off:off + 129], ALU.mult, ALU.add,
                    ["Q", dkey] + pk(bk), ["Q"])
            yield
            for h in range(4):
                bk = bC if h < 2 else bD
                off = (h % 2) * 129
                STT(mlo[mi][:, h * 128:(h + 1) * 128], ps[bk][:, off:off + 128], rr[:, 8 + h:9 + h],
                    sgo[bi][:, h * 128:(h + 1) * 128], ALU.mult, ALU.mult,
                    pk(bk) + ["rrm", ("sgo", bi)], [("mlo", mi)])
            yield
            bt = newbank()
            for c in range(4):
                TR(psb[bt][:, c * 128:(c + 1) * 128], mlo[mi][:, c * 128:(c + 1) * 128], [("mlo", mi)],
                   pk(bt, 0, 256), c == 3)
            ACT(mixT[:, 4:8, blk], psb[bt][:, 0:512].rearrange("p (c n) -> p c n", c=4), AF.Copy,
                pk(bt, 0, 256), [("mixT", c) for c in range(4, 8)])
            yield

        def run_interleaved(gens):
            gens = list(gens)
            while gens:
                for g in list(gens):
                    try:
                        next(g)
                    except StopIteration:
                        gens.remove(g)

        def mixer_back(t):
            xb = t % 2
            S.mark(f"m{t}.blocks")
            for bi in range(NB):
                if os.environ.get('NO_IL2'):
                    for _ in swa_block(t, bi):
                        pass
                    for _ in ml_block(t, bi):
                        pass
                else:
                    run_interleaved([swa_block(t, bi), ml_block(t, bi)])
            S.mark(f"m{t}.wout")
            fs = FusedStats(xb)
            for s2 in range(2):
                wv, wkey = W.get("wout", s2 * 512, 8, 512)
                for m in range(4):
                    mm = s2 * 4 + m
                    b = newbank()
                    proj_group(b, T, lambda k: wv[:, k, m * 128:(m + 1) * 128], lambda k: mixT[:, k, :], 8,
                               lambda k: [wkey, ("mixT", k)])
                    TT(xT[xb][:, mm, :], xT[xb][:, mm, :], ps[b][:], ALU.add, pk(b) + [xk(xb, mm)], [xk(xb, mm)])
                    fs.add(mm)
                    fs.flush(3)
            fs.flush(0)

        def xattn(t):
            xb = t % 2
            if os.environ.get('NO_FUSED'):
                norm_stats(lambda _: xT[xb][:], lambda c: xk(xb, c), T)
            else:
                stats_finish(T)
            norm_apply(xb, 1)
            for s2 in range(2):
                wv, wkey = W.get("wq", s2 * 512, 8, 512)
                for m in range(4):
                    mm = s2 * 4 + m
                    b = newbank()
                    proj_group(b, T, lambda k: wv[:, k, m * 128:(m + 1) * 128], lambda k: hT[:, k, :], 8,
                               lambda k: [wkey, ("hT", k)])
                    ACT(qkT[:, mm, :], ps[b][:], AF.Copy, pk(b), [("qkT", mm)], scale=0.0625)
            def xhead(h):
                pidx = [(h % 2) * 2 + mb for mb in range(2)]
                ri = h % 2
                lv, lk = (lnv, "lnv") if ri == 0 else (lnv2, "lnv2")
                for mb in range(2):
                    b = newbank()
                    for dc in range(2):
                        MM(ps[b][:], KTx[:, 2 * h + dc, mb * 128:(mb + 1) * 128], qkT[:, 2 * h + dc, :], dc == 0, dc == 1,
                           ["KTx", ("qkT", 2 * h + dc)], pk(b), dc == 1)
                    ACT(pT[pidx[mb]][:], ps[b][:], AF.Exp, pk(b), [("pT", pidx[mb])])
                yield
                bs = newbank()
                for mb in range(2):
                    MM(ps[bs][:], ones[:], pT[pidx[mb]][:], mb == 0, mb == 1, ["ones", ("pT", pidx[mb])], pk(bs), mb == 1)
                ACT(lv[:], ps[bs][:], AF.Ln, pk(bs), [lk])
                ACT(rs[ri][:], lv[:], AF.Exp, [lk], [("rs", ri)], scale=-1.0)
                yield
                for ec in range(2):
                    b = newbank()
                    for mb in range(2):
                        MM(ps[b][:], Vx[mb][:, (2 * h + ec) * 128:(2 * h + ec + 1) * 128], pT[pidx[mb]][:], mb == 0, mb == 1,
                           [("Vx", mb), ("pT", pidx[mb])], pk(b), mb == 1)
                    TT(mixT[:, 2 * h + ec, :], ps[b][:], rs[ri][:], ALU.mult, pk(b) + [("rs", ri)], [("mixT", 2 * h + ec)])
                    yield

            for hp in range(2):
                run_interleaved([xhead(2 * hp), xhead(2 * hp + 1)])
            fs = FusedStats(xb)
            for s2 in range(2):
                wv, wkey = W.get("wo", s2 * 512, 8, 512)
                for m in range(4):
                    mm = s2 * 4 + m
                    b = newbank()
                    proj_group(b, T, lambda k: wv[:, k, m * 128:(m + 1) * 128], lambda k: mixT[:, k, :], 8,
                               lambda k: [wkey, ("mixT", k)])
                    TT(xT[xb][:, mm, :], xT[xb][:, mm, :], ps[b][:], ALU.add, pk(b) + [xk(xb, mm)], [xk(xb, mm)])
                    fs.add(mm)
                    fs.flush(3)
            fs.flush(0)

        def ffn(t):
            xb = t % 2
            if os.environ.get('NO_FUSED'):
                norm_stats(lambda _: xT[xb][:], lambda c: xk(xb, c), T)
            else:
                stats_finish(T)
            norm_apply(xb, 3)
            early = (t + 1 < NT) and ((t + 1) % ntps != 0) and bool(os.environ.get('USE_EARLY'))
            if early:
                nxb = (t + 1) % 2
                stats_full(xT[nxb][:], [xk(nxb, c) for c in range(8)], T)
                stats_finish(T, which=1)
            for s11 in range(11):
                wv, wkey = W.get("wup", s11 * 512, 8, 512)
                for jj in range(2):
                    jx = s11 * 2 + jj
                    bgk = newbank()
                    proj_group(bgk, T, lambda k: wv[:, k, jj * 128:(jj + 1) * 128], lambda k: hT[:, k, :], 8,
                               lambda k: [wkey, ("hT", k)])
                    buk = newbank()
                    proj_group(buk, T, lambda k: wv[:, k, 256 + jj * 128:256 + (jj + 1) * 128], lambda k: hT[:, k, :], 8,
                               lambda k: [wkey, ("hT", k)])
                    gi = jx % 3
                    gb = gbuf[gi]
                    gk = ("gbuf", gi)
                    tt = ct[jx % 2]
                    tk = ("ct", jx % 2)
                    si = jx % 2
                    ACT(gb[:, 4:T + 4], ps[bgk][:], AF.Copy, pk(bgk), [gk])
                    CP(gb[:, 0:4], ghalo[:, jx, :], [("ghalo", jx)], [gk])
                    TS(tt[:], gb[:, 2:T + 2], cfw[:, jx, 0:1], None, ALU.mult, None, [gk, "cfw"], [tk])
                    STT(tt[:], gb[:, 3:T + 3], cfw[:, jx, 1:2], tt[:], ALU.mult, ALU.add, [gk, "cfw", tk], [tk])
                    STT(tt[:], gb[:, 4:T + 4], cfw[:, jx, 2:3], tt[:], ALU.mult, ALU.add, [gk, "cfw", tk], [tk])
                    CP(ghalo[:, jx, :], gb[:, T:T + 4], [gk], [("ghalo", jx)])
                    ACT(sgt[si][:], tt[:], AF.Silu, [tk, "cfb"], [("sgt", si)], bias=cfb[:, jx:jx + 1])
                    TT(aT[:, jx, :], sgt[si][:], ps[buk][:], ALU.mult, [("sgt", si)] + pk(buk), [("aT", jx)])
            fg = None
            if early:
                norm_apply((t + 1) % 2, 0, which=1)
                fg = mixer_front(t + 1)
            fs = FusedStats(xb)
            for m in range(8):
                wv, wkey = W.get("wdn", m * 128, NJ, 128)
                b = newbank()
                proj_group(b, T, lambda k: wv[:, k, :], lambda k: aT[:, k, :], NJ, lambda k: [wkey, ("aT", k)])
                TT(xT[xb][:, m, :], xT[xb][:, m, :], ps[b][:], ALU.add, pk(b) + [xk(xb, m)], [xk(xb, m)])
                fs.add(m)
                fs.flush(2)
                if fg is not None and not os.environ.get('NO_IL1'):
                    next(fg, None)
            fs.flush(0)
            return fg

        def final(t):
            xb = t % 2
            if os.environ.get('NO_FUSED'):
                norm_stats(lambda _: xT[xb][:], lambda c: xk(xb, c), T)
            else:
                stats_finish(T)
            for c in range(8):
                STT(xT[xb][:, c, :], xT[xb][:, c, :], gains[:, 4, c:c + 1], rstd[:], ALU.mult, ALU.mult,
                    [xk(xb, c), "rstd", "gains"], [xk(xb, c)])
            S.dma("sp", f"o{xb}", outT_d[:, :, t * T:(t + 1) * T], xT[xb][:], [xk(xb, c) for c in range(8)], ())

        try:
            load_x(0)
            pending_front = None
            for t in range(NT):
                if t % ntps == 0:
                    mem_stage(t // ntps)
                if t + 1 < NT:
                    load_x(t + 1)
                S.mark(f"mixer{t}")
                if pending_front is None:
                    pending_front = mixer_front(t)
                for _ in pending_front:
                    pass
                mixer_back(t)
                S.mark(f"xattn{t}")
                xattn(t)
                S.mark(f"ffn{t}")
                pending_front = ffn(t)
                S.mark(f"final{t}")
                final(t)
        except StopBuild as _e:
            print('STOPPED AT', _e)
        for xb in range(2):
            nm = f"o{xb}"
            if nm in S.sems:
                nc.sync.wait_ge(S.sems[nm], S.cnt[nm])
        build_program.stats = (S.nops, S.nwaits, dict(S.cnt))
        build_program.marks = list(S.marks)
        build_program.wrec = list(W.rec)
    return nc


def _chunked(w):
    K, N = w.shape
    return np.ascontiguousarray(w.reshape(K // 128, 128, N).transpose(1, 0, 2))


def _consts():
    slopes = 2.0 ** (-(np.arange(8) + 1.0))
    k = np.arange(128)[:, None]
    q = np.arange(128)[None, :]
    swab = np.zeros((4, 128, 512), np.float32)
    for kv in range(2):
        for g in range(4):
            s = slopes[kv * 4 + g]
            d_prev = q + 128 - k
            prev = np.where(k > q, -s * d_prev, -30000.0)
            d_cur = q - k
            cur = np.where(k <= q, -s * d_cur, -30000.0)
            swab[kv * 2 + 0][:, g * 128:(g + 1) * 128] = prev
            swab[kv * 2 + 1][:, g * 128:(g + 1) * 128] = cur
    cm = np.zeros((5, 128, 128), np.float32)
    s_ = np.arange(128)[:, None]
    t_ = np.arange(128)[None, :]
    cm[0] = np.where(s_ <= t_, 128.0 ** -0.5, 0.0)
    cm[1] = np.where(s_ <= t_, -1.0, 0.0)
    cm[2] = -1.0
    cm[3] = np.eye(128)
    cm[4] = 1.0
    return swab, cm


def _shared_inputs(inp):
    f = lambda a: np.asarray(a, dtype=np.float32)
    w_in = f(inp["w_in"])[0]
    o0, o1, o2 = 512, 640, 768
    o3, o4, o5 = o2 + 1024, o2 + 1536, o2 + 2048
    qcols = []
    for c in range(4):
        for kv in range(2):
            h = kv * 4 + c
            qcols += list(range(h * 64, (h + 1) * 64))
    colsF = qcols + list(range(o2, o2 + 512)) + list(range(o2 + 512, o3)) + list(range(o0, o1))
    colsT = list(range(o3, o4)) + list(range(o4, o5)) + list(range(o1, o2)) + list(range(o5, o5 + 8))
    w_up = f(inp["w_up"])[0]
    upcols = []
    for s in range(11):
        for jj in range(2):
            j = 2 * s + jj
            upcols += list(range(j * 128, (j + 1) * 128))
        for jj in range(2):
            j = 2 * s + jj
            upcols += list(range(DFF + j * 128, DFF + (j + 1) * 128))
    gains = np.stack([f(inp["norm_mix_g"])[0], f(inp["norm_xattn_g"])[0], f(inp["norm_mem_g"])[0],
                      f(inp["norm_ffn_g"])[0], f(inp["norm_final_g"])], 0)
    gains = np.ascontiguousarray(gains.reshape(5, 8, 128).transpose(2, 0, 1))
    swab, cm = _consts()
    sh = {
        "w_inF": _chunked(w_in[:, colsF]),
        "w_inT": _chunked(w_in[:, colsT]),
        "w_out": _chunked(f(inp["w_out"])[0]),
        "wq": _chunked(f(inp["wq_x"])[0]),
        "wkv": _chunked(f(inp["wkv_x"])[0]),
        "wo": _chunked(f(inp["wo_x"])[0]),
        "w_up": _chunked(w_up[:, upcols]),
        "w_down": _chunked(f(inp["w_down"])[0]),
        "gains": gains,
        "cqw": np.ascontiguousarray(f(inp["conv_qk_w"])[0].reshape(4, 8, 128).transpose(2, 1, 0)),
        "cqb": np.ascontiguousarray(f(inp["conv_qk_b"])[0].reshape(8, 128).T),
        "cfw": np.ascontiguousarray(f(inp["conv_ffn_w"])[0].reshape(3, NJ, 128).transpose(2, 1, 0)),
        "cfb": np.ascontiguousarray(f(inp["conv_ffn_b"])[0].reshape(NJ, 128).T),
        "bgate": np.ascontiguousarray(f(inp["b_gate_if"])[0]),
        "sinks": np.ascontiguousarray(f(inp["attn_sinks"])[0]),
        "swab": swab,
        "cmat": cm,
    }
    return sh


def _core_inputs(x, mem, nseq, ntok_per_seq):
    xs = x[:, :ntok_per_seq, :].reshape(nseq * ntok_per_seq, D)
    xT = np.ascontiguousarray(xs.T.reshape(8, 128, -1).transpose(1, 0, 2))
    memT = np.ascontiguousarray(mem.transpose(0, 2, 1).reshape(nseq, 8, 128, MEM).transpose(0, 2, 1, 3))
    return {"xT": xT, "memT": memT}


def build_two_pass(nseq, ntps):
    build_program(nseq, ntps)
    return build_program(nseq, ntps, wplan=list(build_program.wrec))


_NC_CACHE = {}


def kernel(**inputs):
    x = np.asarray(inputs["x"], dtype=np.float32)
    mem = np.asarray(inputs["mem"], dtype=np.float32)
    nseq = BATCH // NCORES
    ntps = SEQ // T
    key = (nseq, ntps)
    if key not in _NC_CACHE:
        _NC_CACHE[key] = build_two_pass(nseq, ntps)
    nc = _NC_CACHE[key]
    sh = _shared_inputs(inputs)
    in_maps = []
    for c in range(NCORES):
        m = dict(sh)
        m.update(_core_inputs(x[c * nseq:(c + 1) * nseq], mem[c * nseq:(c + 1) * nseq], nseq, SEQ))
        in_maps.append(m)
    res = run_bass_kernel_spmd(nc, in_maps, core_ids=list(range(NCORES)))
    out = np.empty((BATCH, SEQ, D), np.float32)
    for c in range(NCORES):
        oT = np.asarray(res.results[c]["outT"], dtype=np.float32)
        o = oT.transpose(2, 1, 0).reshape(nseq, SEQ, D)
        out[c * nseq:(c + 1) * nseq] = o
    return out
```

```python
import os
import numpy as np
import ml_dtypes
from contextlib import ExitStack
import concourse.bass as bass
import concourse.mybir as mybir
from concourse.bass_utils import run_bass_kernel_spmd

F32 = mybir.dt.float32
BF16 = mybir.dt.bfloat16
ALU = mybir.AluOpType
AF = mybir.ActivationFunctionType

D = 1024
SEQ = 2048
BATCH = 16
NCORES = 8
T = 512
NB = 4
DFF = 2816
NJ = 22
MEM = 256
EPS = 1e-6
NSLAB = 3
SLAB_ELEMS = 4096


class StopBuild(Exception):
    pass


class Sched:
    def __init__(self, nc, es):
        self.nc = nc
        self.es = es
        self.engs = {"pe": nc.tensor, "act": nc.scalar, "dve": nc.vector, "pool": nc.gpsimd, "sp": nc.sync}
        self.sems = {}
        self.cnt = {}
        self.seen = {e: {} for e in self.engs}
        self.w = {}
        self.r = {}
        for e in ("pe", "act", "dve", "pool"):
            self.new_sem(e)
        self.nwaits = 0
        self.nops = 0
        self.marks = []
        self._pending = None

    def new_sem(self, name):
        self.sems[name] = self.es.enter_context(self.nc.semaphore("s_" + name))
        self.cnt[name] = 0

    def _deps(self, eng, reads, writes):
        deps = []
        pe = (eng == "pe")
        for k in reads:
            t = self.w.get(k)
            if t is not None and not (pe and t[2] == "pe"):
                deps.append(t)
        for k in writes:
            t = self.w.get(k)
            if t is not None and not (pe and t[2] == "pe"):
                deps.append(t)
            for t in self.r.get(k, ()):
                if not (pe and t[2] == "pe"):
                    deps.append(t)
        return deps

    def _wait(self, eng, deps):
        need = {}
        for (s, v, _) in deps:
            if v > need.get(s, 0):
                need[s] = v
        for s, v in need.items():
            if self.seen[eng].get(s, 0) < v:
                assert v <= self.cnt[s], ("wait on unsignalled", eng, s, v, self.cnt[s])
                self.engs[eng].wait_ge(self.sems[s], v)
                self.seen[eng][s] = v
                self.nwaits += 1

    def _record(self, tok, reads, writes):
        for k in reads:
            self.r.setdefault(k, []).append(tok)
        for k in writes:
            self.w[k] = tok
            self.r[k] = []

    def op(self, eng, fn, reads=(), writes=(), signal=True):
        self._wait(eng, self._deps(eng, reads, writes))
        ins = fn(self.engs[eng])
        self.nops += 1
        if self._pending is not None:
            self.marks.append((self._pending, ins.ins.name))
            self._pending = None
        if signal:
            self.cnt[eng] += 1
            ins.then_inc(self.sems[eng], 1)
            tok = (eng, self.cnt[eng], eng)
        else:
            tok = (eng, self.cnt[eng] + 1, eng)
        self._record(tok, reads, writes)
        return ins

    def mark(self, label):
        if os.environ.get('STOP_AT') == label:
            raise StopBuild(label)
        self._pending = label

    def dma(self, issuer, sem, out, in_, reads=(), writes=()):
        if sem not in self.sems:
            self.new_sem(sem)
        self._wait(issuer, self._deps("dma:" + sem, reads, writes))
        ins = self.engs[issuer].dma_start(out=out, in_=in_)
        self.cnt[sem] += 16
        ins.then_inc(self.sems[sem], 16)
        tok = (sem, self.cnt[sem], "dma:" + sem)
        self._record(tok, reads, writes)
        return ins


def build_program(nseq, ntps, dbg=False, wplan=None):
    NT = nseq * ntps
    NTOK = NT * T
    nc = bass.Bass("TRN2", target_bir_lowering=False)

    def din(name, shape):
        return nc.dram_tensor(name, list(shape), F32, kind="ExternalInput").ap()

    xT_d = din("xT", [128, 8, NTOK])
    memT_d = din("memT", [nseq, 128, 8, MEM])
    winF_d = din("w_inF", [128, 8, 1664])
    winT_d = din("w_inT", [128, 8, 1160])
    wout_d = din("w_out", [128, 8, 1024])
    wq_d = din("wq", [128, 8, 1024])
    wkv_d = din("wkv", [128, 8, 2048])
    wo_d = din("wo", [128, 8, 1024])
    wup_d = din("w_up", [128, 8, 2 * DFF])
    wdn_d = din("w_down", [128, NJ, 1024])
    gains_d = din("gains", [128, 5, 8])
    cqw_d = din("cqw", [128, 8, 4])
    cqb_d = din("cqb", [128, 8])
    cfw_d = din("cfw", [128, NJ, 3])
    cfb_d = din("cfb", [128, NJ])
    bgate_d = din("bgate", [8])
    sinks_d = din("sinks", [8])
    swab_d = din("swab", [4, 128, 512])
    cmat_d = din("cmat", [5, 128, 128])
    outT_d = nc.dram_tensor("outT", [128, 8, NTOK], F32, kind="ExternalOutput").ap()
    if dbg:
        dbg_d = nc.dram_tensor("dbg", [128, 8, NTOK], F32, kind="ExternalOutput").ap()

    with ExitStack() as es:
        S = Sched(nc, es)

        def sb(name, shape, dt):
            return es.enter_context(nc.sbuf_tensor("sb_" + name, list(shape), dt))

        xT = [sb(f"xT{i}", [128, 8, T], F32) for i in range(2)]
        hT = sb("hT", [128, 8, T], BF16)
        mixT = sb("mixT", [128, 8, T], BF16)
        lnv = sb("lnv", [128, T], F32)
        rstd = sb("rstd", [128, T], F32)
        lnv2 = sb("lnv2", [128, T], F32)
        rstd2 = sb("rstd2", [128, T], F32)
        sqb = sb("sqb", [128, 8, T], BF16)
        qTa = sb("qTa", [128, 4, T], BF16)
        kTa = [sb(f"kTa{i}", [128, 128 + T], BF16) for i in range(2)]
        pre = sb("pre", [128, 8, T + 3], F32)
        ct = [sb(f"ct{i}", [128, T], F32) for i in range(2)]
        qkT = sb("qkT", [128, 8, T], BF16)
        vaug = [sb(f"vaug{i}", [128, 2, 65], BF16) for i in range(NB + 1)]
        vpr = [sb(f"vpr{i}", [128, 4, 129], BF16) for i in range(NB)]
        sgo = [sb(f"sgo{i}", [128, T], BF16) for i in range(NB)]
        G = sb("G", [128, NB, 8], F32)
        GE = sb("GE", [128, NB, 4], F32)
        SPV = sb("SPV", [128, NB, 4], F32)
        GA = sb("GA", [128, NB, 4], F32)
        WS = sb("WS", [128, NB * 4], F32)
        CL = sb("CL", [128, NB * 4], F32)
        DEC = [sb(f"DEC{i}", [128, NB * 4], F32) for i in range(2)]
        DECQ = [sb(f"DECQ{i}", [128, NB * 4], F32) for i in range(2)]
        ktok = sb("ktok", [128, 4, 128], BF16)
        scb = sb("scb", [128, 4, 128], BF16)
        Q = sb("Q", [128, 4, 129], F32)
        Pq = sb("Pq", [128, 4, 129], BF16)
        dn = sb("dn", [128, 16], F32)
        rr = sb("rr", [128, 16], F32)
        mlo = [sb(f"mlo{i}", [128, T], BF16) for i in range(2)]
        att = [sb(f"att{i}", [128, T], BF16) for i in range(2)]
        pT = [sb(f"pT{i}", [128, T], BF16) for i in range(4)]
        KTx = sb("KTx", [128, 8, MEM], BF16)
        Vx = [sb(f"Vx{i}", [128, 1024], BF16) for i in range(2)]
        rs = [sb(f"rs{i}", [128, T], F32) for i in range(2)]
        aT = sb("aT", [128, NJ, T], BF16)
        gbuf = [sb(f"gbuf{i}", [128, T + 4], F32) for i in range(3)]
        sgt = [sb(f"sgt{i}", [128, T], F32) for i in range(2)]
        ghalo = sb("ghalo", [128, NJ, 4], F32)
        slab = [sb(f"slab{i}", [128, SLAB_ELEMS], BF16) for i in range(NSLAB)]
        swab = sb("swab", [128, 4, 512], BF16)
        cmatf = sb("cmatf", [128, 5, 128], F32)
        ident = sb("ident", [128, 128], BF16)
        ones = sb("ones", [128, 128], BF16)
        onesdiv = sb("onesdiv", [128, 128], BF16)
        gains = sb("gains", [128, 5, 8], F32)
        cqw = sb("cqw", [128, 8, 4], F32)
        cqb = sb("cqb", [128, 8], F32)
        cfw = sb("cfw", [128, NJ, 3], F32)
        cfb = sb("cfb", [128, NJ], F32)
        bgate = sb("bgate", [128, 8], F32)
        esink = sb("esink", [128, 8], F32)
        mmask = cmatf[:, 0, :]
        tri = cmatf[:, 1, :]
        negones = cmatf[:, 2, :]

        ps = [es.enter_context(nc.psum_tensor(f"ps{i}", [128, 512], F32)) for i in range(8)]
        psb = [p[:].bitcast(BF16) for p in ps]
        bank_ctr = [0]

        def newbank():
            b = bank_ctr[0] % 7
            bank_ctr[0] += 1
            return b

        def pk(bank, lo=0, hi=512):
            return [("ps", bank)]

        def ACT(out, in_, func, reads, writes, bias=None, scale=None):
            kw = {}
            if bias is not None:
                kw["bias"] = bias
            if scale is not None:
                kw["scale"] = scale
            return S.op("act", lambda e: e.activation(out=out, in_=in_, func=func, **kw), reads, writes)

        def TT(out, in0, in1, op, reads, writes, eng="dve"):
            return S.op(eng, lambda e: e.tensor_tensor(out=out, in0=in0, in1=in1, op=op), reads, writes)

        def TS(out, in0, s1, s2, op0, op1, reads, writes, eng="dve"):
            if s2 is None:
                return S.op(eng, lambda e: e.tensor_scalar(out=out, in0=in0, scalar1=s1, scalar2=None, op0=op0), reads, writes)
            return S.op(eng, lambda e: e.tensor_scalar(out=out, in0=in0, scalar1=s1, scalar2=s2, op0=op0, op1=op1), reads, writes)

        def STT(out, in0, scalar, in1, op0, op1, reads, writes):
            return S.op("dve", lambda e: e.scalar_tensor_tensor(out=out, in0=in0, scalar=scalar, in1=in1, op0=op0, op1=op1), reads, writes)

        def CP(out, in_, reads, writes, eng="dve"):
            return S.op(eng, lambda e: e.tensor_copy(out=out, in_=in_), reads, writes)

        def MS(ap, val, writes, eng="dve"):
            return S.op(eng, lambda e: e.memset(ap, val), (), writes)

        def MM(out, lhsT, rhs, start, stop, reads, writes, signal):
            return S.op("pe", lambda e: e.matmul(out, lhsT=lhsT, rhs=rhs, start=start, stop=stop), reads, writes, signal=signal)

        def TR(out, in_, reads, writes, signal):
            return S.op("pe", lambda e: e.transpose(out, in_, ident[:]), list(reads) + ["ident"], writes, signal=signal)

        S.dma("sp", "c0", cmatf[:], cmat_d.rearrange("c p f -> p c f"), (), ["cmatf"])
        S.dma("pool", "c1", swab[:], swab_d.rearrange("c p f -> p c f"), (), ["swab"])
        S.dma("sp", "c2", gains[:], gains_d, (), ["gains"])
        S.dma("sp", "c3", cqw[:], cqw_d, (), ["cqw"])
        S.dma("sp", "c4", cqb[:], cqb_d, (), ["cqb"])
        S.dma("sp", "c5", cfw[:], cfw_d, (), ["cfw"])
        S.dma("sp", "c6", cfb[:], cfb_d, (), ["cfb"])
        S.dma("sp", "c7", bgate[:], bgate_d.partition_broadcast(128), (), ["bgate"])
        S.dma("sp", "c8", esink[:], sinks_d.partition_broadcast(128), (), ["esink"])
        CP(ident[:], cmatf[:, 3, :], ["cmatf"], ["ident"])
        CP(ones[:], cmatf[:, 4, :], ["cmatf"], ["ones"])
        TS(onesdiv[:], cmatf[:, 4, :], 1.0 / D, None, ALU.mult, None, ["cmatf"], ["onesdiv"])
        ACT(esink[:], esink[:], AF.Exp, ["esink"], ["esink"])
        for i in range(NB + 1):
            MS(vaug[i][:], 1.0, [("vaug", i)])
        for i in range(2):
            MS(kTa[i][:], 0.0, [("kTa", i)])
            MS(DEC[i][:], 0.0, [("DEC", i)])
            MS(DECQ[i][:], 0.0, [("DECQ", i)])

        class WStream:
            def __init__(self, plan):
                self.plan = plan
                self.req = []
                self.rec = []
                self.issued = 0
                self.cur = 0

            def view(self, i, kdim, ncols):
                return slab[i % NSLAB][:, 0:kdim * ncols].rearrange("p (k n) -> p k n", k=kdim)

            def _issue(self, i, spec):
                name, c0, kdim, ncols = spec
                b = i % NSLAB
                S.dma("pool", f"ws{b}", self.view(i, kdim, ncols), WSRC[name][:, :, c0:c0 + ncols], (), [("slab", b)])

            def get(self, name, c0, kdim, ncols):
                spec = (name, c0, kdim, ncols)
                i = self.cur
                self.rec.append(spec)
                if self.plan is None:
                    self._issue(i, spec)
                else:
                    assert self.plan[i] == spec, (i, self.plan[i], spec)
                    while self.issued < min(len(self.plan), i + NSLAB):
                        self._issue(self.issued, self.plan[self.issued])
                        self.issued += 1
                self.cur += 1
                return self.view(i, kdim, ncols), ("slab", i % NSLAB)

        WSRC = {"wkv": wkv_d, "winF": winF_d, "winT": winT_d, "wout": wout_d, "wq": wq_d, "wo": wo_d,
                "wup": wup_d, "wdn": wdn_d}
        W = WStream(wplan)

        def xk(xb, c):
            return ("x", xb, c)

        SB7 = [("ps", 7)]

        def stats_finish(n, which=0):
            lv, rv = (lnv, rstd) if which == 0 else (lnv2, rstd2)
            lk, rk = ("lnv", "rstd") if which == 0 else ("lnv2", "rstd2")
            ACT(lv[:, 0:n], ps[7][:, 0:n], AF.Ln, SB7 + ["epsb"], [lk], bias=epsb[:, 0:1])
            ACT(rv[:, 0:n], lv[:, 0:n], AF.Exp, [lk], [rk], scale=-0.5)

        def stats_full(src_ap, src_keys, n):
            S.op("act", lambda e: e.activation(out=sqb[:, :, 0:n], in_=src_ap, func=AF.Square),
                 src_keys, [("sqb", c) for c in range(8)])
            for c in range(8):
                MM(ps[7][:, 0:n], onesdiv[:], sqb[:, c, 0:n], c == 0, c == 7, [("sqb", c), "onesdiv"], SB7, c == 7)

        def norm_stats(src_ap_fn, src_keys_fn, n):
            stats_full(src_ap_fn(None), [src_keys_fn(c) for c in range(8)], n)
            stats_finish(n)

        class FusedStats:
            def __init__(self, xb):
                self.xb = xb
                self.pending = []

            def add(self, mm):
                if os.environ.get('NO_FUSED'):
                    return
                ACT(sqb[:, mm, :], xT[self.xb][:, mm, :], AF.Square, [xk(self.xb, mm)], [("sqb", mm)])
                self.pending.append(mm)

            def flush(self, keep):
                if os.environ.get('NO_FUSED'):
                    return
                while len(self.pending) > keep:
                    c = self.pending.pop(0)
                    MM(ps[7][:], onesdiv[:], sqb[:, c, :], c == 0, c == 7, [("sqb", c), "onesdiv"], SB7, c == 7)

        def norm_apply(xb, gi, which=0):
            rv, rk = (rstd, "rstd") if which == 0 else (rstd2, "rstd2")
            for c in range(8):
                STT(hT[:, c, :], xT[xb][:, c, :], gains[:, gi, c:c + 1], rv[:], ALU.mult, ALU.mult,
                    [xk(xb, c), rk, "gains"], [("hT", c)])

        def proj_group(b, n, lhs_fn, rhs_fn, nk, reads_fn, width=None):
            for k in range(nk):
                MM(ps[b][:, 0:n], lhs_fn(k), rhs_fn(k), k == 0, k == nk - 1, reads_fn(k), pk(b, 0, n), k == nk - 1)

        epsb = sb("epsb", [128, 8], F32)
        MS(epsb[:], EPS, ["epsb"])

        def load_x(t):
            xb = t % 2
            S.dma("sp", f"x{xb}", xT[xb][:], xT_d[:, :, t * T:(t + 1) * T], (), [xk(xb, c) for c in range(8)])

        def mem_stage(s):
            S.dma("sp", "mem", pre[:, :, 0:MEM], memT_d[s], (), [("pre", c) for c in range(8)])
            norm_stats(lambda _: pre[:, :, 0:MEM], lambda c: ("pre", c), MEM)
            for c in range(8):
                STT(hT[:, c, 0:MEM], pre[:, c, 0:MEM], gains[:, 2, c:c + 1], rstd[:, 0:MEM], ALU.mult, ALU.mult,
                    [("pre", c), "rstd", "gains"], [("hT", c)])
            for s4 in range(4):
                wv, wkey = W.get("wkv", s4 * 512, 8, 512)
                if s4 < 2:
                    for m in range(4):
                        b = newbank()
                        proj_group(b, MEM, lambda k: wv[:, k, m * 128:(m + 1) * 128], lambda k: hT[:, k, 0:MEM], 8,
                                   lambda k: [wkey, ("hT", k)])
                        ACT(KTx[:, s4 * 4 + m, :], ps[b][:, 0:MEM], AF.Copy, pk(b, 0, MEM), ["KTx"])
                else:
                    for mb in range(2):
                        b = newbank()
                        proj_group(b, 512, lambda k: hT[:, k, mb * 128:(mb + 1) * 128], lambda k: wv[:, k, :], 8,
                                   lambda k: [wkey, ("hT", k)])
                        ACT(Vx[mb][:, (s4 - 2) * 512:(s4 - 1) * 512], ps[b][:], AF.Copy, pk(b), [("Vx", mb)])

        def mixer_front(t):
            xb = t % 2
            j = t % ntps
            first = (j == 0)
            tb = t % 2
            if first:
                MS(pre[:, :, 0:3], 0.0, [("pre", c) for c in range(8)])
                MS(Q[:], 0.0, ["Q"])
                MS(ghalo[:], 0.0, [("ghalo", jx_) for jx_ in range(NJ)])
            else:
                CP(pre[:, :, 0:3], pre[:, :, T:T + 3], [("pre", c) for c in range(8)], [("pre", c) for c in range(8)])
                CP(kTa[0][0:64, 0:128], kTa[0][0:64, T:T + 128], [("kTa", 0)], [("kTa", 0)])
                CP(kTa[1][64:128, 0:128], kTa[1][64:128, T:T + 128], [("kTa", 1)], [("kTa", 1)])
                CP(vaug[0][:], vaug[NB][:], [("vaug", NB)], [("vaug", 0)])
            if first or t == 0 or not os.environ.get('USE_EARLY'):
                norm_stats(lambda _: xT[xb][:], lambda c: xk(xb, c), T)
                norm_apply(xb, 0)
            S.mark(f"m{t}.F0")
            wv, wkey = W.get("winF", 0, 8, 512)
            for m in range(4):
                b = newbank()
                proj_group(b, T, lambda k: wv[:, k, m * 128:(m + 1) * 128], lambda k: hT[:, k, :], 8,
                           lambda k: [wkey, ("hT", k)])
                ACT(qTa[:, m, :], ps[b][:], AF.Copy, pk(b), ["qTa"], scale=0.125)
            yield
            S.mark(f"m{t}.F12")
            for f in range(2):
                wv, wkey = W.get("winF", 512 + f * 512, 8, 512)
                for m in range(4):
                    c = f * 4 + m
                    b = newbank()
                    proj_group(b, T, lambda k: wv[:, k, m * 128:(m + 1) * 128], lambda k: hT[:, k, :], 8,
                               lambda k: [wkey, ("hT", k)])
                    ACT(pre[:, c, 3:T + 3], ps[b][:], AF.Copy, pk(b), [("pre", c)])
                    tt = ct[c % 2]
                    tk = ("ct", c % 2)
                    TS(tt[:], pre[:, c, 0:T], cqw[:, c, 0:1], None, ALU.mult, None, [("pre", c), "cqw"], [tk])
                    STT(tt[:], pre[:, c, 1:T + 1], cqw[:, c, 1:2], tt[:], ALU.mult, ALU.add, [("pre", c), "cqw", tk], [tk])
                    STT(tt[:], pre[:, c, 2:T + 2], cqw[:, c, 2:3], tt[:], ALU.mult, ALU.add, [("pre", c), "cqw", tk], [tk])
                    STT(tt[:], pre[:, c, 3:T + 3], cqw[:, c, 3:4], tt[:], ALU.mult, ALU.add, [("pre", c), "cqw", tk], [tk])
                    ACT(qkT[:, c, :], tt[:], AF.Silu, [tk, "cqb"], [("qkT", c)], bias=cqb[:, c:c + 1])
                yield
            S.mark(f"m{t}.F3")
            wv, wkey = W.get("winF", 1536, 8, 128)
            b = newbank()
            proj_group(b, T, lambda k: wv[:, k, 0:128], lambda k: hT[:, k, :], 8, lambda k: [wkey, ("hT", k)])
            ACT(kTa[0][0:64, 128:128 + T], ps[b][0:64, :], AF.Copy, pk(b), [("kTa", 0)])
            ACT(kTa[1][64:128, 128:128 + T], ps[b][64:128, :], AF.Copy, pk(b), [("kTa", 1)])
            yield
            S.mark(f"m{t}.T2")
            wv, wkey = W.get("winT", 1024, 8, 136)
            for bi in range(NB):
                b = newbank()
                proj_group(b, 136, lambda k: hT[:, k, bi * 128:(bi + 1) * 128], lambda k: wv[:, k, :], 8,
                           lambda k: [wkey, ("hT", k)])
                ACT(vaug[1 + bi][:, :, 0:64], ps[b][:, 0:128].rearrange("p (a e) -> p a e", a=2), AF.Copy,
                    pk(b, 0, 136), [("vaug", 1 + bi)])
                TT(G[:, bi, :], ps[b][:, 128:136], bgate[:], ALU.add, pk(b, 0, 136) + ["bgate"], ["G"])
            yield
            S.mark(f"m{t}.gates")
            ACT(GE[:], G[:, :, 4:8], AF.Exp, ["G"], ["GE"], scale=-1.0)
            ACT(SPV[:], GE[:], AF.Ln, ["GE"], ["SPV"], bias=1.0)
            bg = newbank()
            for bi in range(NB):
                MM(ps[bg][:, bi * 4:(bi + 1) * 4], tri, SPV[:, bi, :], True, True, ["SPV", "cmatf"], pk(bg, 0, 32), False)
            for bi in range(NB):
                MM(ps[bg][:, 16 + bi * 4:16 + (bi + 1) * 4], negones, SPV[:, bi, :], True, True, ["SPV", "cmatf"],
                   pk(bg, 0, 32), bi == NB - 1)
            TT(GA[:], G[:, :, 0:4], ps[bg][:, 0:16].rearrange("p (a e) -> p a e", a=NB), ALU.subtract,
               ["G"] + pk(bg, 0, 32), ["GA"])
            ACT(WS[:], GA[:].rearrange("p a e -> p (a e)"), AF.Exp, ["GA"], ["WS"])
            ACT(CL[:], ps[bg][:, 0:16], AF.Exp, pk(bg, 0, 32), ["CL"], scale=-1.0)
            ACT(DEC[tb][:], ps[bg][:, 16:32], AF.Exp, pk(bg, 0, 32), [("DEC", tb)])
            ACT(DECQ[tb][:], DEC[tb][:], AF.Copy, [("DEC", tb)], [("DECQ", tb)], scale=float(128 ** -0.5))
            yield
            S.mark(f"m{t}.T0")
            wv, wkey = W.get("winT", 0, 8, 512)
            for bi in range(NB):
                b = newbank()
                proj_group(b, 512, lambda k: hT[:, k, bi * 128:(bi + 1) * 128], lambda k: wv[:, k, :], 8,
                           lambda k: [wkey, ("hT", k)])
                TT(vpr[bi][:, :, 0:128], ps[b][:].rearrange("p (a e) -> p a e", a=4),
                   WS[:, bi * 4:(bi + 1) * 4].unsqueeze(2).to_broadcast([128, 4, 128]), ALU.mult,
                   pk(b) + ["WS"], [("vpr", bi)])
                CP(vpr[bi][:, :, 128], WS[:, bi * 4:(bi + 1) * 4], ["WS"], [("vpr", bi)])
            yield
            S.mark(f"m{t}.T1")
            wv, wkey = W.get("winT", 512, 8, 512)
            for bi in range(NB):
                b = newbank()
                proj_group(b, 512, lambda k: hT[:, k, bi * 128:(bi + 1) * 128], lambda k: wv[:, k, :], 8,
                           lambda k: [wkey, ("hT", k)])
                ACT(sgo[bi][:], ps[b][:], AF.Tanh, pk(b), [("sgo", bi)], scale=0.5)
                TS(sgo[bi][:], sgo[bi][:], 0.5, 0.5, ALU.mult, ALU.add, [("sgo", bi)], [("sgo", bi)])
            yield

        def swa_block(t, bi):
            j = t % ntps
            blk = slice(bi * 128, (bi + 1) * 128)
            nb = j * NB + bi
            ai = bi % 2
            kbs = [("cur", 1 + bi, slice(128 + bi * 128, 256 + bi * 128), 1)]
            if nb > 0:
                kbs.append(("prev", bi, slice(bi * 128, 128 + bi * 128), 0))
            for kv in range(2):
                for (_, slot, kc, kbi) in kbs:
                    b = newbank()
                    MM(ps[b][:], kTa[kv][:, kc], qTa[:, :, blk], True, False, [("kTa", kv), "qTa"], pk(b), False)
                    MM(ps[b][:], ident[:], swab[:, kv * 2 + kbi, :], False, True, ["ident", "swab"], pk(b), True)
                    ACT(pT[kv * 2 + kbi][:], ps[b][:], AF.Exp, pk(b), [("pT", kv * 2 + kbi)])
                yield
            bos = []
            for kv in range(2):
                bo = newbank()
                bos.append(bo)
                for g in range(4):
                    for ii, (_, slot, kc, kbi) in enumerate(kbs):
                        MM(ps[bo][:, g * 65:(g + 1) * 65], pT[kv * 2 + kbi][:, g * 128:(g + 1) * 128],
                           vaug[slot][:, kv, :], ii == 0, ii == len(kbs) - 1,
                           [("pT", kv * 2 + kbi), ("vaug", slot)], pk(bo, 0, 260),
                           g == 3 and ii == len(kbs) - 1)
                yield
            for kv in range(2):
                bo = bos[kv]
                o3 = ps[bo][:, 0:260].rearrange("p (g e) -> p g e", g=4)
                TT(dn[:, kv * 4:(kv + 1) * 4], o3[:, :, 64], esink[:, kv * 4:(kv + 1) * 4], ALU.add,
                   pk(bo, 0, 260) + ["esink"], [("dn", kv)])
                S.op("dve", lambda e: e.reciprocal(out=rr[:, kv * 4:(kv + 1) * 4], in_=dn[:, kv * 4:(kv + 1) * 4]),
                     [("dn", kv)], [("rr", kv)])
                TT(att[ai][:, kv * 256:(kv + 1) * 256].rearrange("p (g e) -> p g e", g=4), o3[:, :, 0:64],
                   rr[:, kv * 4:(kv + 1) * 4].unsqueeze(2).to_broadcast([128, 4, 64]), ALU.mult,
                   pk(bo, 0, 260) + [("rr", kv)], [("att", ai)])
                yield
            bt = newbank()
            for c in range(4):
                TR(psb[bt][:, c * 128:(c + 1) * 128], att[ai][:, c * 128:(c + 1) * 128], [("att", ai)],
                   pk(bt, 0, 256), c == 3)
            ACT(mixT[:, 0:4, blk], psb[bt][:, 0:512].rearrange("p (c n) -> p c n", c=4), AF.Copy,
                pk(bt, 0, 256), [("mixT", c) for c in range(4)])
            yield

        def ml_block(t, bi):
            tb = t % 2
            blk = slice(bi * 128, (bi + 1) * 128)
            if bi == 0:
                dprev, dqprev = DEC[1 - tb][:, 12:16], DECQ[1 - tb][:, 12:16]
                dkey, dqkey = ("DEC", 1 - tb), ("DECQ", 1 - tb)
            else:
                dprev, dqprev = DEC[tb][:, (bi - 1) * 4:bi * 4], DECQ[tb][:, (bi - 1) * 4:bi * 4]
                dkey, dqkey = ("DEC", tb), ("DECQ", tb)
            mi = bi % 2
            bA = newbank()
            for h in range(4):
                TR(psb[bA][:, h * 128:(h + 1) * 128], qkT[:, 4 + h, blk], [("qkT", 4 + h)], pk(bA), h == 3)
            ACT(ktok[:].rearrange("p h d -> p (h d)"), psb[bA][:, 0:512], AF.Copy, pk(bA), ["ktok"])
            bB = newbank()
            for h in range(4):
                MM(ps[bB][:, h * 128:(h + 1) * 128], qkT[:, 4 + h, blk], qkT[:, h, blk], True, True,
                   [("qkT", 4 + h), ("qkT", h)], pk(bB), h == 3)
            TT(scb[:], ps[bB][:].rearrange("p (h t) -> p h t", h=4),
               cmatf[:, 0:1, :].to_broadcast([128, 4, 128]), ALU.mult, pk(bB) + ["cmatf"], ["scb"])
            TT(Pq[:], Q[:], dqprev.unsqueeze(2).to_broadcast([128, 4, 129]), ALU.mult, ["Q", dqkey], ["Pq"])
            yield
            bC, bD = newbank(), newbank()
            for h in range(4):
                bk = bC if h < 2 else bD
                off = (h % 2) * 129
                MM(ps[bk][:, off:off + 129], qkT[:, h, blk], Pq[:, h, :], True, False, [("qkT", h), "Pq"], pk(bk), False)
                MM(ps[bk][:, off:off + 129], scb[:, h, :], vpr[bi][:, h, :], False, True, ["scb", ("vpr", bi)], pk(bk),
                   h == 1)
            for h in range(4):
                MM(ps[bD][:, 258 + 2 * h:260 + 2 * h], qkT[:, h, blk], Pq[:, h, 127:129], True, False,
                   [("qkT", h), "Pq"], pk(bD), False)
                MM(ps[bD][:, 258 + 2 * h:260 + 2 * h], scb[:, h, :], vpr[bi][:, h, 127:129], False, True,
                   ["scb", ("vpr", bi)], pk(bD), h == 3)
            bE, bF = newbank(), newbank()
            for h in range(4):
                bk = bE if h < 2 else bF
                off = (h % 2) * 129
                MM(ps[bk][:, off:off + 129], ktok[:, h, :], vpr[bi][:, h, :], True, True, ["ktok", ("vpr", bi)], pk(bk),
                   h % 2 == 1)
            yield
            ACT(dn[:, 8:12], ps[bD][:, 258:266].rearrange("p (h two) -> p h two", two=2)[:, :, 1], AF.Abs,
                pk(bD), ["dnm"])
            TT(dn[:, 8:12], dn[:, 8:12], CL[:, bi * 4:(bi + 1) * 4], ALU.max, ["dnm", "CL"], ["dnm"])
            S.op("dve", lambda e: e.reciprocal(out=rr[:, 8:12], in_=dn[:, 8:12]), ["dnm"], ["rrm"])
            for h in range(4):
                bk = bE if h < 2 else bF
                off = (h % 2) * 129
                STT(Q[:, h, :], Q[:, h, :], dprev[:, h:h + 1], ps[bk][:, off:off + 129], ALU.mult, ALU.add,
                    ["Q", dkey] + pk(bk), ["Q"])
            yield
            for h in range(4):
                bk = bC if h < 2 else bD
                off = (h % 2) * 129
                STT(mlo[mi][:, h * 128:(h + 1) * 128], ps[bk][:, off:off + 128], rr[:, 8 + h:9 + h],
                    sgo[bi][:, h * 128:(h + 1) * 128], ALU.mult, ALU.mult,
                    pk(bk) + ["rrm", ("sgo", bi)], [("mlo", mi)])
            yield
            bt = newbank()
            for c in range(4):
                TR(psb[bt][:, c * 128:(c + 1) * 128], mlo[mi][:, c * 128:(c + 1) * 128], [("mlo", mi)],
                   pk(bt, 0, 256), c == 3)
            ACT(mixT[:, 4:8, blk], psb[bt][:, 0:512].rearrange("p (c n) -> p c n", c=4), AF.Copy,
                pk(bt, 0, 256), [("mixT", c) for c in range(4, 8)])
            yield

        def run_interleaved(gens):
            gens = list(gens)
            while gens:
                for g in list(gens):
                    try:
                        next(g)
                    except StopIteration:
                        gens.remove(g)

        def mixer_back(t):
            xb = t % 2
            S.mark(f"m{t}.blocks")
            for bi in range(NB):
                if os.environ.get('NO_IL2'):
                    for _ in swa_block(t, bi):
                        pass
                    for _ in ml_block(t, bi):
                        pass
                else:
                    run_interleaved([swa_block(t, bi), ml_block(t, bi)])
            S.mark(f"m{t}.wout")
            fs = FusedStats(xb)
            for s2 in range(2):
                wv, wkey = W.get("wout", s2 * 512, 8, 512)
                for m in range(4):
                    mm = s2 * 4 + m
                    b = newbank()
                    proj_group(b, T, lambda k: wv[:, k, m * 128:(m + 1) * 128], lambda k: mixT[:, k, :], 8,
                               lambda k: [wkey, ("mixT", k)])
                    TT(xT[xb][:, mm, :], xT[xb][:, mm, :], ps[b][:], ALU.add, pk(b) + [xk(xb, mm)], [xk(xb, mm)])
                    fs.add(mm)
                    fs.flush(3)
            fs.flush(0)

        def xattn(t):
            xb = t % 2
            if os.environ.get('NO_FUSED'):
                norm_stats(lambda _: xT[xb][:], lambda c: xk(xb, c), T)
            else:
                stats_finish(T)
            norm_apply(xb, 1)
            for s2 in range(2):
                wv, wkey = W.get("wq", s2 * 512, 8, 512)
                for m in range(4):
                    mm = s2 * 4 + m
                    b = newbank()
                    proj_group(b, T, lambda k: wv[:, k, m * 128:(m + 1) * 128], lambda k: hT[:, k, :], 8,
                               lambda k: [wkey, ("hT", k)])
                    ACT(qkT[:, mm, :], ps[b][:], AF.Copy, pk(b), [("qkT", mm)], scale=0.0625)
            for h in range(4):
                pidx = [(h % 2) * 2 + mb for mb in range(2)]
                for mb in range(2):
                    b = newbank()
                    for dc in range(2):
                        MM(ps[b][:], KTx[:, 2 * h + dc, mb * 128:(mb + 1) * 128], qkT[:, 2 * h + dc, :], dc == 0, dc == 1,
                           ["KTx", ("qkT", 2 * h + dc)], pk(b), dc == 1)
                    ACT(pT[pidx[mb]][:], ps[b][:], AF.Exp, pk(b), [("pT", pidx[mb])])
                bs = newbank()
                for mb in range(2):
                    MM(ps[bs][:], ones[:], pT[pidx[mb]][:], mb == 0, mb == 1, ["ones", ("pT", pidx[mb])], pk(bs), mb == 1)
                ri = h % 2
                ACT(lnv[:], ps[bs][:], AF.Ln, pk(bs), ["lnv"])
                ACT(rs[ri][:], lnv[:], AF.Exp, ["lnv"], [("rs", ri)], scale=-1.0)
                for ec in range(2):
                    b = newbank()
                    for mb in range(2):
                        MM(ps[b][:], Vx[mb][:, (2 * h + ec) * 128:(2 * h + ec + 1) * 128], pT[pidx[mb]][:], mb == 0, mb == 1,
                           [("Vx", mb), ("pT", pidx[mb])], pk(b), mb == 1)
                    TT(mixT[:, 2 * h + ec, :], ps[b][:], rs[ri][:], ALU.mult, pk(b) + [("rs", ri)], [("mixT", 2 * h + ec)])
            fs = FusedStats(xb)
            for s2 in range(2):
                wv, wkey = W.get("wo", s2 * 512, 8, 512)
                for m in range(4):
                    mm = s2 * 4 + m
                    b = newbank()
                    proj_group(b, T, lambda k: wv[:, k, m * 128:(m + 1) * 128], lambda k: mixT[:, k, :], 8,
                               lambda k: [wkey, ("mixT", k)])
                    TT(xT[xb][:, mm, :], xT[xb][:, mm, :], ps[b][:], ALU.add, pk(b) + [xk(xb, mm)], [xk(xb, mm)])
                    fs.add(mm)
                    fs.flush(3)
            fs.flush(0)

        def ffn(t):
            xb = t % 2
            if os.environ.get('NO_FUSED'):
                norm_stats(lambda _: xT[xb][:], lambda c: xk(xb, c), T)
            else:
                stats_finish(T)
            norm_apply(xb, 3)
            early = (t + 1 < NT) and ((t + 1) % ntps != 0) and bool(os.environ.get('USE_EARLY'))
            if early:
                nxb = (t + 1) % 2
                stats_full(xT[nxb][:], [xk(nxb, c) for c in range(8)], T)
                stats_finish(T, which=1)
            for s11 in range(11):
                wv, wkey = W.get("wup", s11 * 512, 8, 512)
                info = []
                for jj in range(2):
                    jx = s11 * 2 + jj
                    bgk = newbank()
                    proj_group(bgk, T, lambda k: wv[:, k, jj * 128:(jj + 1) * 128], lambda k: hT[:, k, :], 8,
                               lambda k: [wkey, ("hT", k)])
                    buk = newbank()
                    proj_group(buk, T, lambda k: wv[:, k, 256 + jj * 128:256 + (jj + 1) * 128], lambda k: hT[:, k, :], 8,
                               lambda k: [wkey, ("hT", k)])
                    gi = jx % 3
                    info.append((jx, bgk, buk, gbuf[gi], ("gbuf", gi), ct[jx % 2], ("ct", jx % 2), jx % 2))
                for (jx, bgk, buk, gb, gk, tt, tk, si) in info:
                    ACT(gb[:, 4:T + 4], ps[bgk][:], AF.Copy, pk(bgk), [gk])
                for (jx, bgk, buk, gb, gk, tt, tk, si) in info:
                    CP(gb[:, 0:4], ghalo[:, jx, :], [("ghalo", jx)], [gk])
                for (jx, bgk, buk, gb, gk, tt, tk, si) in info:
                    TS(tt[:], gb[:, 2:T + 2], cfw[:, jx, 0:1], None, ALU.mult, None, [gk, "cfw"], [tk])
                for (jx, bgk, buk, gb, gk, tt, tk, si) in info:
                    STT(tt[:], gb[:, 3:T + 3], cfw[:, jx, 1:2], tt[:], ALU.mult, ALU.add, [gk, "cfw", tk], [tk])
                for (jx, bgk, buk, gb, gk, tt, tk, si) in info:
                    STT(tt[:], gb[:, 4:T + 4], cfw[:, jx, 2:3], tt[:], ALU.mult, ALU.add, [gk, "cfw", tk], [tk])
                for (jx, bgk, buk, gb, gk, tt, tk, si) in info:
                    CP(ghalo[:, jx, :], gb[:, T:T + 4], [gk], [("ghalo", jx)])
                for (jx, bgk, buk, gb, gk, tt, tk, si) in info:
                    ACT(sgt[si][:], tt[:], AF.Silu, [tk, "cfb"], [("sgt", si)], bias=cfb[:, jx:jx + 1])
                for (jx, bgk, buk, gb, gk, tt, tk, si) in info:
                    TT(aT[:, jx, :], sgt[si][:], ps[buk][:], ALU.mult, [("sgt", si)] + pk(buk), [("aT", jx)])
            fg = None
            if early:
                norm_apply((t + 1) % 2, 0, which=1)
                fg = mixer_front(t + 1)
            fs = FusedStats(xb)
            for m in range(8):
                wv, wkey = W.get("wdn", m * 128, NJ, 128)
                b = newbank()
                proj_group(b, T, lambda k: wv[:, k, :], lambda k: aT[:, k, :], NJ, lambda k: [wkey, ("aT", k)])
                TT(xT[xb][:, m, :], xT[xb][:, m, :], ps[b][:], ALU.add, pk(b) + [xk(xb, m)], [xk(xb, m)])
                fs.add(m)
                fs.flush(2)
                if fg is not None and not os.environ.get('NO_IL1'):
                    next(fg, None)
            fs.flush(0)
            return fg

        def final(t):
            xb = t % 2
            if os.environ.get('NO_FUSED'):
                norm_stats(lambda _: xT[xb][:], lambda c: xk(xb, c), T)
            else:
                stats_finish(T)
            for c in range(8):
                STT(xT[xb][:, c, :], xT[xb][:, c, :], gains[:, 4, c:c + 1], rstd[:], ALU.mult, ALU.mult,
                    [xk(xb, c), "rstd", "gains"], [xk(xb, c)])
            S.dma("sp", f"o{xb}", outT_d[:, :, t * T:(t + 1) * T], xT[xb][:], [xk(xb, c) for c in range(8)], ())

        try:
            load_x(0)
            pending_front = None
            for t in range(NT):
                if t % ntps == 0:
                    mem_stage(t // ntps)
                if t + 1 < NT:
                    load_x(t + 1)
                S.mark(f"mixer{t}")
                if pending_front is None:
                    pending_front = mixer_front(t)
                for _ in pending_front:
                    pass
                mixer_back(t)
                S.mark(f"xattn{t}")
                xattn(t)
                S.mark(f"ffn{t}")
                pending_front = ffn(t)
                S.mark(f"final{t}")
                final(t)
        except StopBuild as _e:
            print('STOPPED AT', _e)
        for xb in range(2):
            nm = f"o{xb}"
            if nm in S.sems:
                nc.sync.wait_ge(S.sems[nm], S.cnt[nm])
        build_program.stats = (S.nops, S.nwaits, dict(S.cnt))
        build_program.marks = list(S.marks)
        build_program.wrec = list(W.rec)
    return nc


def _chunked(w):
    K, N = w.shape
    return np.ascontiguousarray(w.reshape(K // 128, 128, N).transpose(1, 0, 2))


def _consts():
    slopes = 2.0 ** (-(np.arange(8) + 1.0))
    k = np.arange(128)[:, None]
    q = np.arange(128)[None, :]
    swab = np.zeros((4, 128, 512), np.float32)
    for kv in range(2):
        for g in range(4):
            s = slopes[kv * 4 + g]
            d_prev = q + 128 - k
            prev = np.where(k > q, -s * d_prev, -30000.0)
            d_cur = q - k
            cur = np.where(k <= q, -s * d_cur, -30000.0)
            swab[kv * 2 + 0][:, g * 128:(g + 1) * 128] = prev
            swab[kv * 2 + 1][:, g * 128:(g + 1) * 128] = cur
    cm = np.zeros((5, 128, 128), np.float32)
    s_ = np.arange(128)[:, None]
    t_ = np.arange(128)[None, :]
    cm[0] = np.where(s_ <= t_, 128.0 ** -0.5, 0.0)
    cm[1] = np.where(s_ <= t_, -1.0, 0.0)
    cm[2] = -1.0
    cm[3] = np.eye(128)
    cm[4] = 1.0
    return swab, cm


def _shared_inputs(inp):
    f = lambda a: np.asarray(a, dtype=np.float32)
    w_in = f(inp["w_in"])[0]
    o0, o1, o2 = 512, 640, 768
    o3, o4, o5 = o2 + 1024, o2 + 1536, o2 + 2048
    qcols = []
    for c in range(4):
        for kv in range(2):
            h = kv * 4 + c
            qcols += list(range(h * 64, (h + 1) * 64))
    colsF = qcols + list(range(o2, o2 + 512)) + list(range(o2 + 512, o3)) + list(range(o0, o1))
    colsT = list(range(o3, o4)) + list(range(o4, o5)) + list(range(o1, o2)) + list(range(o5, o5 + 8))
    w_up = f(inp["w_up"])[0]
    upcols = []
    for s in range(11):
        for jj in range(2):
            j = 2 * s + jj
            upcols += list(range(j * 128, (j + 1) * 128))
        for jj in range(2):
            j = 2 * s + jj
            upcols += list(range(DFF + j * 128, DFF + (j + 1) * 128))
    gains = np.stack([f(inp["norm_mix_g"])[0], f(inp["norm_xattn_g"])[0], f(inp["norm_mem_g"])[0],
                      f(inp["norm_ffn_g"])[0], f(inp["norm_final_g"])], 0)
    gains = np.ascontiguousarray(gains.reshape(5, 8, 128).transpose(2, 0, 1))
    swab, cm = _consts()
    sh = {
        "w_inF": _chunked(w_in[:, colsF]),
        "w_inT": _chunked(w_in[:, colsT]),
        "w_out": _chunked(f(inp["w_out"])[0]),
        "wq": _chunked(f(inp["wq_x"])[0]),
        "wkv": _chunked(f(inp["wkv_x"])[0]),
        "wo": _chunked(f(inp["wo_x"])[0]),
        "w_up": _chunked(w_up[:, upcols]),
        "w_down": _chunked(f(inp["w_down"])[0]),
        "gains": gains,
        "cqw": np.ascontiguousarray(f(inp["conv_qk_w"])[0].reshape(4, 8, 128).transpose(2, 1, 0)),
        "cqb": np.ascontiguousarray(f(inp["conv_qk_b"])[0].reshape(8, 128).T),
        "cfw": np.ascontiguousarray(f(inp["conv_ffn_w"])[0].reshape(3, NJ, 128).transpose(2, 1, 0)),
        "cfb": np.ascontiguousarray(f(inp["conv_ffn_b"])[0].reshape(NJ, 128).T),
        "bgate": np.ascontiguousarray(f(inp["b_gate_if"])[0]),
        "sinks": np.ascontiguousarray(f(inp["attn_sinks"])[0]),
        "swab": swab,
        "cmat": cm,
    }
    return sh


def _core_inputs(x, mem, nseq, ntok_per_seq):
    xs = x[:, :ntok_per_seq, :].reshape(nseq * ntok_per_seq, D)
    xT = np.ascontiguousarray(xs.T.reshape(8, 128, -1).transpose(1, 0, 2))
    memT = np.ascontiguousarray(mem.transpose(0, 2, 1).reshape(nseq, 8, 128, MEM).transpose(0, 2, 1, 3))
    return {"xT": xT, "memT": memT}


def build_two_pass(nseq, ntps):
    build_program(nseq, ntps)
    return build_program(nseq, ntps, wplan=list(build_program.wrec))


_NC_CACHE = {}


def kernel(**inputs):
    x = np.asarray(inputs["x"], dtype=np.float32)
    mem = np.asarray(inputs["mem"], dtype=np.float32)
    nseq = BATCH // NCORES
    ntps = SEQ // T
    key = (nseq, ntps)
    if key not in _NC_CACHE:
        _NC_CACHE[key] = build_two_pass(nseq, ntps)
    nc = _NC_CACHE[key]
    sh = _shared_inputs(inputs)
    in_maps = []
    for c in range(NCORES):
        m = dict(sh)
        m.update(_core_inputs(x[c * nseq:(c + 1) * nseq], mem[c * nseq:(c + 1) * nseq], nseq, SEQ))
        in_maps.append(m)
    res = run_bass_kernel_spmd(nc, in_maps, core_ids=list(range(NCORES)))
    out = np.empty((BATCH, SEQ, D), np.float32)
    for c in range(NCORES):
        oT = np.asarray(res.results[c]["outT"], dtype=np.float32)
        o = oT.transpose(2, 1, 0).reshape(nseq, SEQ, D)
        out[c * nseq:(c + 1) * nseq] = o
    return out
```

```python
import os
import numpy as np
import ml_dtypes
from contextlib import ExitStack
import concourse.bass as bass
import concourse.mybir as mybir
from concourse.bass_utils import run_bass_kernel_spmd

F32 = mybir.dt.float32
BF16 = mybir.dt.bfloat16
ALU = mybir.AluOpType
AF = mybir.ActivationFunctionType

D = 1024
SEQ = 2048
BATCH = 16
NCORES = 8
T = 512
NB = 4
DFF = 2816
NJ = 22
MEM = 256
EPS = 1e-6
NSLAB = 3
SLAB_ELEMS = 4096


class StopBuild(Exception):
    pass


class Sched:
    def __init__(self, nc, es):
        self.nc = nc
        self.es = es
        self.engs = {"pe": nc.tensor, "act": nc.scalar, "dve": nc.vector, "pool": nc.gpsimd, "sp": nc.sync}
        self.sems = {}
        self.cnt = {}
        self.seen = {e: {} for e in self.engs}
        self.w = {}
        self.r = {}
        for e in ("pe", "act", "dve", "pool"):
            self.new_sem(e)
        self.nwaits = 0
        self.nops = 0
        self.marks = []
        self._pending = None

    def new_sem(self, name):
        self.sems[name] = self.es.enter_context(self.nc.semaphore("s_" + name))
        self.cnt[name] = 0

    def _deps(self, eng, reads, writes):
        deps = []
        pe = (eng == "pe")
        for k in reads:
            t = self.w.get(k)
            if t is not None and not (pe and t[2] == "pe"):
                deps.append(t)
        for k in writes:
            t = self.w.get(k)
            if t is not None and not (pe and t[2] == "pe"):
                deps.append(t)
            for t in self.r.get(k, ()):
                if not (pe and t[2] == "pe"):
                    deps.append(t)
        return deps

    def _wait(self, eng, deps):
        need = {}
        for (s, v, _) in deps:
            if v > need.get(s, 0):
                need[s] = v
        for s, v in need.items():
            if self.seen[eng].get(s, 0) < v:
                assert v <= self.cnt[s], ("wait on unsignalled", eng, s, v, self.cnt[s])
                self.engs[eng].wait_ge(self.sems[s], v)
                self.seen[eng][s] = v
                self.nwaits += 1

    def _record(self, tok, reads, writes):
        for k in reads:
            self.r.setdefault(k, []).append(tok)
        for k in writes:
            self.w[k] = tok
            self.r[k] = []

    def op(self, eng, fn, reads=(), writes=(), signal=True):
        self._wait(eng, self._deps(eng, reads, writes))
        ins = fn(self.engs[eng])
        self.nops += 1
        if self._pending is not None:
            self.marks.append((self._pending, ins.ins.name))
            self._pending = None
        if signal:
            self.cnt[eng] += 1
            ins.then_inc(self.sems[eng], 1)
            tok = (eng, self.cnt[eng], eng)
        else:
            tok = (eng, self.cnt[eng] + 1, eng)
        self._record(tok, reads, writes)
        return ins

    def mark(self, label):
        if os.environ.get('STOP_AT') == label:
            raise StopBuild(label)
        self._pending = label

    def dma(self, issuer, sem, out, in_, reads=(), writes=()):
        if sem not in self.sems:
            self.new_sem(sem)
        self._wait(issuer, self._deps("dma:" + sem, reads, writes))
        ins = self.engs[issuer].dma_start(out=out, in_=in_)
        self.cnt[sem] += 16
        ins.then_inc(self.sems[sem], 16)
        tok = (sem, self.cnt[sem], "dma:" + sem)
        self._record(tok, reads, writes)
        return ins


def build_program(nseq, ntps, dbg=False, wplan=None):
    NT = nseq * ntps
    NTOK = NT * T
    nc = bass.Bass("TRN2", target_bir_lowering=False)

    def din(name, shape):
        return nc.dram_tensor(name, list(shape), F32, kind="ExternalInput").ap()

    xT_d = din("xT", [128, 8, NTOK])
    memT_d = din("memT", [nseq, 128, 8, MEM])
    winF_d = din("w_inF", [128, 8, 1664])
    winT_d = din("w_inT", [128, 8, 1160])
    wout_d = din("w_out", [128, 8, 1024])
    wq_d = din("wq", [128, 8, 1024])
    wkv_d = din("wkv", [128, 8, 2048])
    wo_d = din("wo", [128, 8, 1024])
    wup_d = din("w_up", [128, 8, 2 * DFF])
    wdn_d = din("w_down", [128, NJ, 1024])
    gains_d = din("gains", [128, 5, 8])
    cqw_d = din("cqw", [128, 8, 4])
    cqb_d = din("cqb", [128, 8])
    cfw_d = din("cfw", [128, NJ, 3])
    cfb_d = din("cfb", [128, NJ])
    bgate_d = din("bgate", [8])
    sinks_d = din("sinks", [8])
    swab_d = din("swab", [4, 128, 512])
    cmat_d = din("cmat", [5, 128, 128])
    outT_d = nc.dram_tensor("outT", [128, 8, NTOK], F32, kind="ExternalOutput").ap()
    if dbg:
        dbg_d = nc.dram_tensor("dbg", [128, 8, NTOK], F32, kind="ExternalOutput").ap()

    with ExitStack() as es:
        S = Sched(nc, es)

        def sb(name, shape, dt):
            return es.enter_context(nc.sbuf_tensor("sb_" + name, list(shape), dt))

        xT = [sb(f"xT{i}", [128, 8, T], F32) for i in range(2)]
        hT = sb("hT", [128, 8, T], BF16)
        mixT = sb("mixT", [128, 8, T], BF16)
        lnv = sb("lnv", [128, T], F32)
        rstd = sb("rstd", [128, T], F32)
        lnv2 = sb("lnv2", [128, T], F32)
        rstd2 = sb("rstd2", [128, T], F32)
        sqb = sb("sqb", [128, 8, T], BF16)
        qTa = sb("qTa", [128, 4, T], BF16)
        kTa = [sb(f"kTa{i}", [128, 128 + T], BF16) for i in range(2)]
        pre = sb("pre", [128, 8, T + 3], F32)
        ct = [sb(f"ct{i}", [128, T], F32) for i in range(2)]
        qkT = sb("qkT", [128, 8, T], BF16)
        vaug = [sb(f"vaug{i}", [128, 2, 65], BF16) for i in range(NB + 1)]
        vpr = [sb(f"vpr{i}", [128, 4, 129], BF16) for i in range(NB)]
        sgo = [sb(f"sgo{i}", [128, T], BF16) for i in range(NB)]
        G = sb("G", [128, NB, 8], F32)
        GE = sb("GE", [128, NB, 4], F32)
        SPV = sb("SPV", [128, NB, 4], F32)
        GA = sb("GA", [128, NB, 4], F32)
        WS = sb("WS", [128, NB * 4], F32)
        CL = sb("CL", [128, NB * 4], F32)
        DEC = [sb(f"DEC{i}", [128, NB * 4], F32) for i in range(2)]
        DECQ = [sb(f"DECQ{i}", [128, NB * 4], F32) for i in range(2)]
        ktok = sb("ktok", [128, 4, 128], BF16)
        scb = sb("scb", [128, 4, 128], BF16)
        Q = sb("Q", [128, 4, 129], F32)
        Pq = sb("Pq", [128, 4, 129], BF16)
        dn = sb("dn", [128, 16], F32)
        rr = sb("rr", [128, 16], F32)
        mlo = [sb(f"mlo{i}", [128, T], BF16) for i in range(2)]
        att = [sb(f"att{i}", [128, T], BF16) for i in range(2)]
        pT = [sb(f"pT{i}", [128, T], BF16) for i in range(4)]
        KTx = sb("KTx", [128, 8, MEM], BF16)
        Vx = [sb(f"Vx{i}", [128, 1024], BF16) for i in range(2)]
        rs = [sb(f"rs{i}", [128, T], F32) for i in range(2)]
        aT = sb("aT", [128, NJ, T], BF16)
        gbuf = [sb(f"gbuf{i}", [128, T + 4], F32) for i in range(3)]
        sgt = [sb(f"sgt{i}", [128, T], F32) for i in range(2)]
        ghalo = sb("ghalo", [128, NJ, 4], F32)
        slab = [sb(f"slab{i}", [128, SLAB_ELEMS], BF16) for i in range(NSLAB)]
        swab = sb("swab", [128, 4, 512], BF16)
        cmatf = sb("cmatf", [128, 5, 128], F32)
        ident = sb("ident", [128, 128], BF16)
        ones = sb("ones", [128, 128], BF16)
        onesdiv = sb("onesdiv", [128, 128], BF16)
        gains = sb("gains", [128, 5, 8], F32)
        cqw = sb("cqw", [128, 8, 4], F32)
        cqb = sb("cqb", [128, 8], F32)
        cfw = sb("cfw", [128, NJ, 3], F32)
        cfb = sb("cfb", [128, NJ], F32)
        bgate = sb("bgate", [128, 8], F32)
        esink = sb("esink", [128, 8], F32)
        mmask = cmatf[:, 0, :]
        tri = cmatf[:, 1, :]
        negones = cmatf[:, 2, :]

        ps = [es.enter_context(nc.psum_tensor(f"ps{i}", [128, 512], F32)) for i in range(8)]
        psb = [p[:].bitcast(BF16) for p in ps]
        bank_ctr = [0]

        def newbank():
            b = bank_ctr[0] % 7
            bank_ctr[0] += 1
            return b

        def pk(bank, lo=0, hi=512):
            return [("ps", bank)]

        def ACT(out, in_, func, reads, writes, bias=None, scale=None):
            kw = {}
            if bias is not None:
                kw["bias"] = bias
            if scale is not None:
                kw["scale"] = scale
            return S.op("act", lambda e: e.activation(out=out, in_=in_, func=func, **kw), reads, writes)

        def TT(out, in0, in1, op, reads, writes, eng="dve"):
            return S.op(eng, lambda e: e.tensor_tensor(out=out, in0=in0, in1=in1, op=op), reads, writes)

        def TS(out, in0, s1, s2, op0, op1, reads, writes, eng="dve"):
            if s2 is None:
                return S.op(eng, lambda e: e.tensor_scalar(out=out, in0=in0, scalar1=s1, scalar2=None, op0=op0), reads, writes)
            return S.op(eng, lambda e: e.tensor_scalar(out=out, in0=in0, scalar1=s1, scalar2=s2, op0=op0, op1=op1), reads, writes)

        def STT(out, in0, scalar, in1, op0, op1, reads, writes):
            return S.op("dve", lambda e: e.scalar_tensor_tensor(out=out, in0=in0, scalar=scalar, in1=in1, op0=op0, op1=op1), reads, writes)

        def CP(out, in_, reads, writes, eng="dve"):
            return S.op(eng, lambda e: e.tensor_copy(out=out, in_=in_), reads, writes)

        def MS(ap, val, writes, eng="dve"):
            return S.op(eng, lambda e: e.memset(ap, val), (), writes)

        def MM(out, lhsT, rhs, start, stop, reads, writes, signal):
            return S.op("pe", lambda e: e.matmul(out, lhsT=lhsT, rhs=rhs, start=start, stop=stop), reads, writes, signal=signal)

        def TR(out, in_, reads, writes, signal):
            return S.op("pe", lambda e: e.transpose(out, in_, ident[:]), list(reads) + ["ident"], writes, signal=signal)

        S.dma("sp", "c0", cmatf[:], cmat_d.rearrange("c p f -> p c f"), (), ["cmatf"])
        S.dma("pool", "c1", swab[:], swab_d.rearrange("c p f -> p c f"), (), ["swab"])
        S.dma("sp", "c2", gains[:], gains_d, (), ["gains"])
        S.dma("sp", "c3", cqw[:], cqw_d, (), ["cqw"])
        S.dma("sp", "c4", cqb[:], cqb_d, (), ["cqb"])
        S.dma("sp", "c5", cfw[:], cfw_d, (), ["cfw"])
        S.dma("sp", "c6", cfb[:], cfb_d, (), ["cfb"])
        S.dma("sp", "c7", bgate[:], bgate_d.partition_broadcast(128), (), ["bgate"])
        S.dma("sp", "c8", esink[:], sinks_d.partition_broadcast(128), (), ["esink"])
        CP(ident[:], cmatf[:, 3, :], ["cmatf"], ["ident"])
        CP(ones[:], cmatf[:, 4, :], ["cmatf"], ["ones"])
        TS(onesdiv[:], cmatf[:, 4, :], 1.0 / D, None, ALU.mult, None, ["cmatf"], ["onesdiv"])
        ACT(esink[:], esink[:], AF.Exp, ["esink"], ["esink"])
        for i in range(NB + 1):
            MS(vaug[i][:], 1.0, [("vaug", i)])
        for i in range(2):
            MS(kTa[i][:], 0.0, [("kTa", i)])
            MS(DEC[i][:], 0.0, [("DEC", i)])
            MS(DECQ[i][:], 0.0, [("DECQ", i)])

        class WStream:
            def __init__(self, plan):
                self.plan = plan
                self.req = []
                self.rec = []
                self.issued = 0
                self.cur = 0

            def view(self, i, kdim, ncols):
                return slab[i % NSLAB][:, 0:kdim * ncols].rearrange("p (k n) -> p k n", k=kdim)

            def _issue(self, i, spec):
                name, c0, kdim, ncols = spec
                b = i % NSLAB
                S.dma("pool", f"ws{b}", self.view(i, kdim, ncols), WSRC[name][:, :, c0:c0 + ncols], (), [("slab", b)])

            def get(self, name, c0, kdim, ncols):
                spec = (name, c0, kdim, ncols)
                i = self.cur
                self.rec.append(spec)
                if self.plan is None:
                    self._issue(i, spec)
                else:
                    assert self.plan[i] == spec, (i, self.plan[i], spec)
                    while self.issued < min(len(self.plan), i + NSLAB):
                        self._issue(self.issued, self.plan[self.issued])
                        self.issued += 1
                self.cur += 1
                return self.view(i, kdim, ncols), ("slab", i % NSLAB)

        WSRC = {"wkv": wkv_d, "winF": winF_d, "winT": winT_d, "wout": wout_d, "wq": wq_d, "wo": wo_d,
                "wup": wup_d, "wdn": wdn_d}
        W = WStream(wplan)

        def xk(xb, c):
            return ("x", xb, c)

        SB7 = [("ps", 7)]

        def stats_finish(n, which=0):
            lv, rv = (lnv, rstd) if which == 0 else (lnv2, rstd2)
            lk, rk = ("lnv", "rstd") if which == 0 else ("lnv2", "rstd2")
            ACT(lv[:, 0:n], ps[7][:, 0:n], AF.Ln, SB7 + ["epsb"], [lk], bias=epsb[:, 0:1])
            ACT(rv[:, 0:n], lv[:, 0:n], AF.Exp, [lk], [rk], scale=-0.5)

        def stats_full(src_ap, src_keys, n):
            S.op("act", lambda e: e.activation(out=sqb[:, :, 0:n], in_=src_ap, func=AF.Square),
                 src_keys, [("sqb", c) for c in range(8)])
            for c in range(8):
                MM(ps[7][:, 0:n], onesdiv[:], sqb[:, c, 0:n], c == 0, c == 7, [("sqb", c), "onesdiv"], SB7, c == 7)

        def norm_stats(src_ap_fn, src_keys_fn, n):
            stats_full(src_ap_fn(None), [src_keys_fn(c) for c in range(8)], n)
            stats_finish(n)

        class FusedStats:
            def __init__(self, xb):
                self.xb = xb
                self.pending = []

            def add(self, mm):
                if os.environ.get('NO_FUSED'):
                    return
                ACT(sqb[:, mm, :], xT[self.xb][:, mm, :], AF.Square, [xk(self.xb, mm)], [("sqb", mm)])
                self.pending.append(mm)

            def flush(self, keep):
                if os.environ.get('NO_FUSED'):
                    return
                while len(self.pending) > keep:
                    c = self.pending.pop(0)
                    MM(ps[7][:], onesdiv[:], sqb[:, c, :], c == 0, c == 7, [("sqb", c), "onesdiv"], SB7, c == 7)

        def norm_apply(xb, gi, which=0):
            rv, rk = (rstd, "rstd") if which == 0 else (rstd2, "rstd2")
            for c in range(8):
                STT(hT[:, c, :], xT[xb][:, c, :], gains[:, gi, c:c + 1], rv[:], ALU.mult, ALU.mult,
                    [xk(xb, c), rk, "gains"], [("hT", c)])

        def proj_group(b, n, lhs_fn, rhs_fn, nk, reads_fn, width=None):
            for k in range(nk):
                MM(ps[b][:, 0:n], lhs_fn(k), rhs_fn(k), k == 0, k == nk - 1, reads_fn(k), pk(b, 0, n), k == nk - 1)

        epsb = sb("epsb", [128, 8], F32)
        MS(epsb[:], EPS, ["epsb"])

        def load_x(t):
            xb = t % 2
            S.dma("sp", f"x{xb}", xT[xb][:], xT_d[:, :, t * T:(t + 1) * T], (), [xk(xb, c) for c in range(8)])

        def mem_stage(s):
            S.dma("sp", "mem", pre[:, :, 0:MEM], memT_d[s], (), [("pre", c) for c in range(8)])
            norm_stats(lambda _: pre[:, :, 0:MEM], lambda c: ("pre", c), MEM)
            for c in range(8):
                STT(hT[:, c, 0:MEM], pre[:, c, 0:MEM], gains[:, 2, c:c + 1], rstd[:, 0:MEM], ALU.mult, ALU.mult,
                    [("pre", c), "rstd", "gains"], [("hT", c)])
            for s4 in range(4):
                wv, wkey = W.get("wkv", s4 * 512, 8, 512)
                if s4 < 2:
                    for m in range(4):
                        b = newbank()
                        proj_group(b, MEM, lambda k: wv[:, k, m * 128:(m + 1) * 128], lambda k: hT[:, k, 0:MEM], 8,
                                   lambda k: [wkey, ("hT", k)])
                        ACT(KTx[:, s4 * 4 + m, :], ps[b][:, 0:MEM], AF.Copy, pk(b, 0, MEM), ["KTx"])
                else:
                    for mb in range(2):
                        b = newbank()
                        proj_group(b, 512, lambda k: hT[:, k, mb * 128:(mb + 1) * 128], lambda k: wv[:, k, :], 8,
                                   lambda k: [wkey, ("hT", k)])
                        ACT(Vx[mb][:, (s4 - 2) * 512:(s4 - 1) * 512], ps[b][:], AF.Copy, pk(b), [("Vx", mb)])

        def mixer_front(t):
            xb = t % 2
            j = t % ntps
            first = (j == 0)
            tb = t % 2
            if first:
                MS(pre[:, :, 0:3], 0.0, [("pre", c) for c in range(8)])
                MS(Q[:], 0.0, ["Q"])
                MS(ghalo[:], 0.0, [("ghalo", jx_) for jx_ in range(NJ)])
            else:
                CP(pre[:, :, 0:3], pre[:, :, T:T + 3], [("pre", c) for c in range(8)], [("pre", c) for c in range(8)])
                CP(kTa[0][0:64, 0:128], kTa[0][0:64, T:T + 128], [("kTa", 0)], [("kTa", 0)])
                CP(kTa[1][64:128, 0:128], kTa[1][64:128, T:T + 128], [("kTa", 1)], [("kTa", 1)])
                CP(vaug[0][:], vaug[NB][:], [("vaug", NB)], [("vaug", 0)])
            if first or t == 0 or not os.environ.get('USE_EARLY'):
                norm_stats(lambda _: xT[xb][:], lambda c: xk(xb, c), T)
                norm_apply(xb, 0)
            S.mark(f"m{t}.F0")
            wv, wkey = W.get("winF", 0, 8, 512)
            for m in range(4):
                b = newbank()
                proj_group(b, T, lambda k: wv[:, k, m * 128:(m + 1) * 128], lambda k: hT[:, k, :], 8,
                           lambda k: [wkey, ("hT", k)])
                ACT(qTa[:, m, :], ps[b][:], AF.Copy, pk(b), ["qTa"], scale=0.125)
            yield
            S.mark(f"m{t}.F12")
            for f in range(2):
                wv, wkey = W.get("winF", 512 + f * 512, 8, 512)
                for mp in range(2):
                    info = []
                    for m in (2 * mp, 2 * mp + 1):
                        c = f * 4 + m
                        b = newbank()
                        proj_group(b, T, lambda k: wv[:, k, m * 128:(m + 1) * 128], lambda k: hT[:, k, :], 8,
                                   lambda k: [wkey, ("hT", k)])
                        info.append((c, b, ct[c % 2], ("ct", c % 2)))
                    for (c, b, tt, tk) in info:
                        ACT(pre[:, c, 3:T + 3], ps[b][:], AF.Copy, pk(b), [("pre", c)])
                    for (c, b, tt, tk) in info:
                        TS(tt[:], pre[:, c, 0:T], cqw[:, c, 0:1], None, ALU.mult, None, [("pre", c), "cqw"], [tk])
                    for j4 in (1, 2, 3):
                        for (c, b, tt, tk) in info:
                            STT(tt[:], pre[:, c, j4:T + j4], cqw[:, c, j4:j4 + 1], tt[:], ALU.mult, ALU.add,
                                [("pre", c), "cqw", tk], [tk])
                    for (c, b, tt, tk) in info:
                        ACT(qkT[:, c, :], tt[:], AF.Silu, [tk, "cqb"], [("qkT", c)], bias=cqb[:, c:c + 1])
                yield
            S.mark(f"m{t}.F3")
            wv, wkey = W.get("winF", 1536, 8, 128)
            b = newbank()
            proj_group(b, T, lambda k: wv[:, k, 0:128], lambda k: hT[:, k, :], 8, lambda k: [wkey, ("hT", k)])
            ACT(kTa[0][0:64, 128:128 + T], ps[b][0:64, :], AF.Copy, pk(b), [("kTa", 0)])
            ACT(kTa[1][64:128, 128:128 + T], ps[b][64:128, :], AF.Copy, pk(b), [("kTa", 1)])
            yield
            S.mark(f"m{t}.T2")
            wv, wkey = W.get("winT", 1024, 8, 136)
            for bi in range(NB):
                b = newbank()
                proj_group(b, 136, lambda k: hT[:, k, bi * 128:(bi + 1) * 128], lambda k: wv[:, k, :], 8,
                           lambda k: [wkey, ("hT", k)])
                ACT(vaug[1 + bi][:, :, 0:64], ps[b][:, 0:128].rearrange("p (a e) -> p a e", a=2), AF.Copy,
                    pk(b, 0, 136), [("vaug", 1 + bi)])
                TT(G[:, bi, :], ps[b][:, 128:136], bgate[:], ALU.add, pk(b, 0, 136) + ["bgate"], ["G"])
            yield
            S.mark(f"m{t}.gates")
            ACT(GE[:], G[:, :, 4:8], AF.Exp, ["G"], ["GE"], scale=-1.0)
            ACT(SPV[:], GE[:], AF.Ln, ["GE"], ["SPV"], bias=1.0)
            bg = newbank()
            for bi in range(NB):
                MM(ps[bg][:, bi * 4:(bi + 1) * 4], tri, SPV[:, bi, :], True, True, ["SPV", "cmatf"], pk(bg, 0, 32), False)
            for bi in range(NB):
                MM(ps[bg][:, 16 + bi * 4:16 + (bi + 1) * 4], negones, SPV[:, bi, :], True, True, ["SPV", "cmatf"],
                   pk(bg, 0, 32), bi == NB - 1)
            TT(GA[:], G[:, :, 0:4], ps[bg][:, 0:16].rearrange("p (a e) -> p a e", a=NB), ALU.subtract,
               ["G"] + pk(bg, 0, 32), ["GA"])
            ACT(WS[:], GA[:].rearrange("p a e -> p (a e)"), AF.Exp, ["GA"], ["WS"])
            ACT(CL[:], ps[bg][:, 0:16], AF.Exp, pk(bg, 0, 32), ["CL"], scale=-1.0)
            ACT(DEC[tb][:], ps[bg][:, 16:32], AF.Exp, pk(bg, 0, 32), [("DEC", tb)])
            ACT(DECQ[tb][:], DEC[tb][:], AF.Copy, [("DEC", tb)], [("DECQ", tb)], scale=float(128 ** -0.5))
            yield
            S.mark(f"m{t}.T0")
            wv, wkey = W.get("winT", 0, 8, 512)
            for bi in range(NB):
                b = newbank()
                proj_group(b, 512, lambda k: hT[:, k, bi * 128:(bi + 1) * 128], lambda k: wv[:, k, :], 8,
                           lambda k: [wkey, ("hT", k)])
                TT(vpr[bi][:, :, 0:128], ps[b][:].rearrange("p (a e) -> p a e", a=4),
                   WS[:, bi * 4:(bi + 1) * 4].unsqueeze(2).to_broadcast([128, 4, 128]), ALU.mult,
                   pk(b) + ["WS"], [("vpr", bi)])
                CP(vpr[bi][:, :, 128], WS[:, bi * 4:(bi + 1) * 4], ["WS"], [("vpr", bi)])
            yield
            S.mark(f"m{t}.T1")
            wv, wkey = W.get("winT", 512, 8, 512)
            for bi in range(NB):
                b = newbank()
                proj_group(b, 512, lambda k: hT[:, k, bi * 128:(bi + 1) * 128], lambda k: wv[:, k, :], 8,
                           lambda k: [wkey, ("hT", k)])
                ACT(sgo[bi][:], ps[b][:], AF.Tanh, pk(b), [("sgo", bi)], scale=0.5)
                TS(sgo[bi][:], sgo[bi][:], 0.5, 0.5, ALU.mult, ALU.add, [("sgo", bi)], [("sgo", bi)])
            yield

        def swa_block(t, bi):
            j = t % ntps
            blk = slice(bi * 128, (bi + 1) * 128)
            nb = j * NB + bi
            ai = bi % 2
            kbs = [("cur", 1 + bi, slice(128 + bi * 128, 256 + bi * 128), 1)]
            if nb > 0:
                kbs.append(("prev", bi, slice(bi * 128, 128 + bi * 128), 0))
            for kv in range(2):
                for (_, slot, kc, kbi) in kbs:
                    b = newbank()
                    MM(ps[b][:], kTa[kv][:, kc], qTa[:, :, blk], True, False, [("kTa", kv), "qTa"], pk(b), False)
                    MM(ps[b][:], ident[:], swab[:, kv * 2 + kbi, :], False, True, ["ident", "swab"], pk(b), True)
                    ACT(pT[kv * 2 + kbi][:], ps[b][:], AF.Exp, pk(b), [("pT", kv * 2 + kbi)])
                yield
            bos = []
            for kv in range(2):
                bo = newbank()
                bos.append(bo)
                for g in range(4):
                    for ii, (_, slot, kc, kbi) in enumerate(kbs):
                        MM(ps[bo][:, g * 65:(g + 1) * 65], pT[kv * 2 + kbi][:, g * 128:(g + 1) * 128],
                           vaug[slot][:, kv, :], ii == 0, ii == len(kbs) - 1,
                           [("pT", kv * 2 + kbi), ("vaug", slot)], pk(bo, 0, 260),
                           g == 3 and ii == len(kbs) - 1)
                yield
            for kv in range(2):
                bo = bos[kv]
                o3 = ps[bo][:, 0:260].rearrange("p (g e) -> p g e", g=4)
                TT(dn[:, kv * 4:(kv + 1) * 4], o3[:, :, 64], esink[:, kv * 4:(kv + 1) * 4], ALU.add,
                   pk(bo, 0, 260) + ["esink"], [("dn", kv)])
                S.op("dve", lambda e: e.reciprocal(out=rr[:, kv * 4:(kv + 1) * 4], in_=dn[:, kv * 4:(kv + 1) * 4]),
                     [("dn", kv)], [("rr", kv)])
                TT(att[ai][:, kv * 256:(kv + 1) * 256].rearrange("p (g e) -> p g e", g=4), o3[:, :, 0:64],
                   rr[:, kv * 4:(kv + 1) * 4].unsqueeze(2).to_broadcast([128, 4, 64]), ALU.mult,
                   pk(bo, 0, 260) + [("rr", kv)], [("att", ai)])
                yield
            bt = newbank()
            for c in range(4):
                TR(psb[bt][:, c * 128:(c + 1) * 128], att[ai][:, c * 128:(c + 1) * 128], [("att", ai)],
                   pk(bt, 0, 256), c == 3)
            ACT(mixT[:, 0:4, blk], psb[bt][:, 0:512].rearrange("p (c n) -> p c n", c=4), AF.Copy,
                pk(bt, 0, 256), [("mixT", c) for c in range(4)])
            yield

        def ml_block(t, bi):
            tb = t % 2
            blk = slice(bi * 128, (bi + 1) * 128)
            if bi == 0:
                dprev, dqprev = DEC[1 - tb][:, 12:16], DECQ[1 - tb][:, 12:16]
                dkey, dqkey = ("DEC", 1 - tb), ("DECQ", 1 - tb)
            else:
                dprev, dqprev = DEC[tb][:, (bi - 1) * 4:bi * 4], DECQ[tb][:, (bi - 1) * 4:bi * 4]
                dkey, dqkey = ("DEC", tb), ("DECQ", tb)
            mi = bi % 2
            bA = newbank()
            for h in range(4):
                TR(psb[bA][:, h * 128:(h + 1) * 128], qkT[:, 4 + h, blk], [("qkT", 4 + h)], pk(bA), h == 3)
            ACT(ktok[:].rearrange("p h d -> p (h d)"), psb[bA][:, 0:512], AF.Copy, pk(bA), ["ktok"])
            bB = newbank()
            for h in range(4):
                MM(ps[bB][:, h * 128:(h + 1) * 128], qkT[:, 4 + h, blk], qkT[:, h, blk], True, True,
                   [("qkT", 4 + h), ("qkT", h)], pk(bB), h == 3)
            TT(scb[:], ps[bB][:].rearrange("p (h t) -> p h t", h=4),
               cmatf[:, 0:1, :].to_broadcast([128, 4, 128]), ALU.mult, pk(bB) + ["cmatf"], ["scb"])
            TT(Pq[:], Q[:], dqprev.unsqueeze(2).to_broadcast([128, 4, 129]), ALU.mult, ["Q", dqkey], ["Pq"])
            yield
            bC, bD = newbank(), newbank()
            for h in range(4):
                bk = bC if h < 2 else bD
                off = (h % 2) * 129
                MM(ps[bk][:, off:off + 129], qkT[:, h, blk], Pq[:, h, :], True, False, [("qkT", h), "Pq"], pk(bk), False)
                MM(ps[bk][:, off:off + 129], scb[:, h, :], vpr[bi][:, h, :], False, True, ["scb", ("vpr", bi)], pk(bk),
                   h == 1)
            for h in range(4):
                MM(ps[bD][:, 258 + 2 * h:260 + 2 * h], qkT[:, h, blk], Pq[:, h, 127:129], True, False,
                   [("qkT", h), "Pq"], pk(bD), False)
                MM(ps[bD][:, 258 + 2 * h:260 + 2 * h], scb[:, h, :], vpr[bi][:, h, 127:129], False, True,
                   ["scb", ("vpr", bi)], pk(bD), h == 3)
            bE, bF = newbank(), newbank()
            for h in range(4):
                bk = bE if h < 2 else bF
                off = (h % 2) * 129
                MM(ps[bk][:, off:off + 129], ktok[:, h, :], vpr[bi][:, h, :], True, True, ["ktok", ("vpr", bi)], pk(bk),
                   h % 2 == 1)
            yield
            ACT(dn[:, 8:12], ps[bD][:, 258:266].rearrange("p (h two) -> p h two", two=2)[:, :, 1], AF.Abs,
                pk(bD), ["dnm"])
            TT(dn[:, 8:12], dn[:, 8:12], CL[:, bi * 4:(bi + 1) * 4], ALU.max, ["dnm", "CL"], ["dnm"])
            S.op("dve", lambda e: e.reciprocal(out=rr[:, 8:12], in_=dn[:, 8:12]), ["dnm"], ["rrm"])
            for h in range(4):
                bk = bE if h < 2 else bF
                off = (h % 2) * 129
                STT(Q[:, h, :], Q[:, h, :], dprev[:, h:h + 1], ps[bk][:, off:off + 129], ALU.mult, ALU.add,
                    ["Q", dkey] + pk(bk), ["Q"])
            yield
            for h in range(4):
                bk = bC if h < 2 else bD
                off = (h % 2) * 129
                STT(mlo[mi][:, h * 128:(h + 1) * 128], ps[bk][:, off:off + 128], rr[:, 8 + h:9 + h],
                    sgo[bi][:, h * 128:(h + 1) * 128], ALU.mult, ALU.mult,
                    pk(bk) + ["rrm", ("sgo", bi)], [("mlo", mi)])
            yield
            bt = newbank()
            for c in range(4):
                TR(psb[bt][:, c * 128:(c + 1) * 128], mlo[mi][:, c * 128:(c + 1) * 128], [("mlo", mi)],
                   pk(bt, 0, 256), c == 3)
            ACT(mixT[:, 4:8, blk], psb[bt][:, 0:512].rearrange("p (c n) -> p c n", c=4), AF.Copy,
                pk(bt, 0, 256), [("mixT", c) for c in range(4, 8)])
            yield

        def run_interleaved(gens):
            gens = list(gens)
            while gens:
                for g in list(gens):
                    try:
                        next(g)
                    except StopIteration:
                        gens.remove(g)

        def mixer_back(t):
            xb = t % 2
            S.mark(f"m{t}.blocks")
            for bi in range(NB):
                if os.environ.get('NO_IL2'):
                    for _ in swa_block(t, bi):
                        pass
                    for _ in ml_block(t, bi):
                        pass
                else:
                    run_interleaved([swa_block(t, bi), ml_block(t, bi)])
            S.mark(f"m{t}.wout")
            fs = FusedStats(xb)
            for s2 in range(2):
                wv, wkey = W.get("wout", s2 * 512, 8, 512)
                for m in range(4):
                    mm = s2 * 4 + m
                    b = newbank()
                    proj_group(b, T, lambda k: wv[:, k, m * 128:(m + 1) * 128], lambda k: mixT[:, k, :], 8,
                               lambda k: [wkey, ("mixT", k)])
                    TT(xT[xb][:, mm, :], xT[xb][:, mm, :], ps[b][:], ALU.add, pk(b) + [xk(xb, mm)], [xk(xb, mm)])
                    fs.add(mm)
                    fs.flush(3)
            fs.flush(0)

        def xattn(t):
            xb = t % 2
            if os.environ.get('NO_FUSED'):
                norm_stats(lambda _: xT[xb][:], lambda c: xk(xb, c), T)
            else:
                stats_finish(T)
            norm_apply(xb, 1)
            for s2 in range(2):
                wv, wkey = W.get("wq", s2 * 512, 8, 512)
                for m in range(4):
                    mm = s2 * 4 + m
                    b = newbank()
                    proj_group(b, T, lambda k: wv[:, k, m * 128:(m + 1) * 128], lambda k: hT[:, k, :], 8,
                               lambda k: [wkey, ("hT", k)])
                    ACT(qkT[:, mm, :], ps[b][:], AF.Copy, pk(b), [("qkT", mm)], scale=0.0625)
            for h in range(4):
                pidx = [(h % 2) * 2 + mb for mb in range(2)]
                for mb in range(2):
                    b = newbank()
                    for dc in range(2):
                        MM(ps[b][:], KTx[:, 2 * h + dc, mb * 128:(mb + 1) * 128], qkT[:, 2 * h + dc, :], dc == 0, dc == 1,
                           ["KTx", ("qkT", 2 * h + dc)], pk(b), dc == 1)
                    ACT(pT[pidx[mb]][:], ps[b][:], AF.Exp, pk(b), [("pT", pidx[mb])])
                bs = newbank()
                for mb in range(2):
                    MM(ps[bs][:], ones[:], pT[pidx[mb]][:], mb == 0, mb == 1, ["ones", ("pT", pidx[mb])], pk(bs), mb == 1)
                ri = h % 2
                ACT(lnv[:], ps[bs][:], AF.Ln, pk(bs), ["lnv"])
                ACT(rs[ri][:], lnv[:], AF.Exp, ["lnv"], [("rs", ri)], scale=-1.0)
                for ec in range(2):
                    b = newbank()
                    for mb in range(2):
                        MM(ps[b][:], Vx[mb][:, (2 * h + ec) * 128:(2 * h + ec + 1) * 128], pT[pidx[mb]][:], mb == 0, mb == 1,
                           [("Vx", mb), ("pT", pidx[mb])], pk(b), mb == 1)
                    TT(mixT[:, 2 * h + ec, :], ps[b][:], rs[ri][:], ALU.mult, pk(b) + [("rs", ri)], [("mixT", 2 * h + ec)])
            fs = FusedStats(xb)
            for s2 in range(2):
                wv, wkey = W.get("wo", s2 * 512, 8, 512)
                for m in range(4):
                    mm = s2 * 4 + m
                    b = newbank()
                    proj_group(b, T, lambda k: wv[:, k, m * 128:(m + 1) * 128], lambda k: mixT[:, k, :], 8,
                               lambda k: [wkey, ("mixT", k)])
                    TT(xT[xb][:, mm, :], xT[xb][:, mm, :], ps[b][:], ALU.add, pk(b) + [xk(xb, mm)], [xk(xb, mm)])
                    fs.add(mm)
                    fs.flush(3)
            fs.flush(0)

        def ffn(t):
            xb = t % 2
            if os.environ.get('NO_FUSED'):
                norm_stats(lambda _: xT[xb][:], lambda c: xk(xb, c), T)
            else:
                stats_finish(T)
            norm_apply(xb, 3)
            early = (t + 1 < NT) and ((t + 1) % ntps != 0) and bool(os.environ.get('USE_EARLY'))
            if early:
                nxb = (t + 1) % 2
                stats_full(xT[nxb][:], [xk(nxb, c) for c in range(8)], T)
                stats_finish(T, which=1)
            for s11 in range(11):
                wv, wkey = W.get("wup", s11 * 512, 8, 512)
                info = []
                for jj in range(2):
                    jx = s11 * 2 + jj
                    bgk = newbank()
                    proj_group(bgk, T, lambda k: wv[:, k, jj * 128:(jj + 1) * 128], lambda k: hT[:, k, :], 8,
                               lambda k: [wkey, ("hT", k)])
                    buk = newbank()
                    proj_group(buk, T, lambda k: wv[:, k, 256 + jj * 128:256 + (jj + 1) * 128], lambda k: hT[:, k, :], 8,
                               lambda k: [wkey, ("hT", k)])
                    gi = jx % 3
                    info.append((jx, bgk, buk, gbuf[gi], ("gbuf", gi), ct[jx % 2], ("ct", jx % 2), jx % 2))
                for (jx, bgk, buk, gb, gk, tt, tk, si) in info:
                    ACT(gb[:, 4:T + 4], ps[bgk][:], AF.Copy, pk(bgk), [gk])
                for (jx, bgk, buk, gb, gk, tt, tk, si) in info:
                    CP(gb[:, 0:4], ghalo[:, jx, :], [("ghalo", jx)], [gk])
                for (jx, bgk, buk, gb, gk, tt, tk, si) in info:
                    TS(tt[:], gb[:, 2:T + 2], cfw[:, jx, 0:1], None, ALU.mult, None, [gk, "cfw"], [tk])
                for (jx, bgk, buk, gb, gk, tt, tk, si) in info:
                    STT(tt[:], gb[:, 3:T + 3], cfw[:, jx, 1:2], tt[:], ALU.mult, ALU.add, [gk, "cfw", tk], [tk])
                for (jx, bgk, buk, gb, gk, tt, tk, si) in info:
                    STT(tt[:], gb[:, 4:T + 4], cfw[:, jx, 2:3], tt[:], ALU.mult, ALU.add, [gk, "cfw", tk], [tk])
                for (jx, bgk, buk, gb, gk, tt, tk, si) in info:
                    CP(ghalo[:, jx, :], gb[:, T:T + 4], [gk], [("ghalo", jx)])
                for (jx, bgk, buk, gb, gk, tt, tk, si) in info:
                    ACT(sgt[si][:], tt[:], AF.Silu, [tk, "cfb"], [("sgt", si)], bias=cfb[:, jx:jx + 1])
                for (jx, bgk, buk, gb, gk, tt, tk, si) in info:
                    TT(aT[:, jx, :], sgt[si][:], ps[buk][:], ALU.mult, [("sgt", si)] + pk(buk), [("aT", jx)])
            fg = None
            if early:
                norm_apply((t + 1) % 2, 0, which=1)
                fg = mixer_front(t + 1)
            fs = FusedStats(xb)
            for m in range(8):
                wv, wkey = W.get("wdn", m * 128, NJ, 128)
                b = newbank()
                proj_group(b, T, lambda k: wv[:, k, :], lambda k: aT[:, k, :], NJ, lambda k: [wkey, ("aT", k)])
                TT(xT[xb][:, m, :], xT[xb][:, m, :], ps[b][:], ALU.add, pk(b) + [xk(xb, m)], [xk(xb, m)])
                fs.add(m)
                fs.flush(2)
                if fg is not None and not os.environ.get('NO_IL1'):
                    next(fg, None)
            fs.flush(0)
            return fg

        def final(t):
            xb = t % 2
            if os.environ.get('NO_FUSED'):
                norm_stats(lambda _: xT[xb][:], lambda c: xk(xb, c), T)
            else:
                stats_finish(T)
            for c in range(8):
                STT(xT[xb][:, c, :], xT[xb][:, c, :], gains[:, 4, c:c + 1], rstd[:], ALU.mult, ALU.mult,
                    [xk(xb, c), "rstd", "gains"], [xk(xb, c)])
            S.dma("sp", f"o{xb}", outT_d[:, :, t * T:(t + 1) * T], xT[xb][:], [xk(xb, c) for c in range(8)], ())

        try:
            load_x(0)
            pending_front = None
            for t in range(NT):
                if t % ntps == 0:
                    mem_stage(t // ntps)
                if t + 1 < NT:
                    load_x(t + 1)
                S.mark(f"mixer{t}")
                if pending_front is None:
                    pending_front = mixer_front(t)
                for _ in pending_front:
                    pass
                mixer_back(t)
                S.mark(f"xattn{t}")
                xattn(t)
                S.mark(f"ffn{t}")
                pending_front = ffn(t)
                S.mark(f"final{t}")
                final(t)
        except StopBuild as _e:
            print('STOPPED AT', _e)
        for xb in range(2):
            nm = f"o{xb}"
            if nm in S.sems:
                nc.sync.wait_ge(S.sems[nm], S.cnt[nm])
        build_program.stats = (S.nops, S.nwaits, dict(S.cnt))
        build_program.marks = list(S.marks)
        build_program.wrec = list(W.rec)
    return nc


def _chunked(w):
    K, N = w.shape
    return np.ascontiguousarray(w.reshape(K // 128, 128, N).transpose(1, 0, 2))


def _consts():
    slopes = 2.0 ** (-(np.arange(8) + 1.0))
    k = np.arange(128)[:, None]
    q = np.arange(128)[None, :]
    swab = np.zeros((4, 128, 512), np.float32)
    for kv in range(2):
        for g in range(4):
            s = slopes[kv * 4 + g]
            d_prev = q + 128 - k
            prev = np.where(k > q, -s * d_prev, -30000.0)
            d_cur = q - k
            cur = np.where(k <= q, -s * d_cur, -30000.0)
            swab[kv * 2 + 0][:, g * 128:(g + 1) * 128] = prev
            swab[kv * 2 + 1][:, g * 128:(g + 1) * 128] = cur
    cm = np.zeros((5, 128, 128), np.float32)
    s_ = np.arange(128)[:, None]
    t_ = np.arange(128)[None, :]
    cm[0] = np.where(s_ <= t_, 128.0 ** -0.5, 0.0)
    cm[1] = np.where(s_ <= t_, -1.0, 0.0)
    cm[2] = -1.0
    cm[3] = np.eye(128)
    cm[4] = 1.0
    return swab, cm


def _shared_inputs(inp):
    f = lambda a: np.asarray(a, dtype=np.float32)
    w_in = f(inp["w_in"])[0]
    o0, o1, o2 = 512, 640, 768
    o3, o4, o5 = o2 + 1024, o2 + 1536, o2 + 2048
    qcols = []
    for c in range(4):
        for kv in range(2):
            h = kv * 4 + c
            qcols += list(range(h * 64, (h + 1) * 64))
    colsF = qcols + list(range(o2, o2 + 512)) + list(range(o2 + 512, o3)) + list(range(o0, o1))
    colsT = list(range(o3, o4)) + list(range(o4, o5)) + list(range(o1, o2)) + list(range(o5, o5 + 8))
    w_up = f(inp["w_up"])[0]
    upcols = []
    for s in range(11):
        for jj in range(2):
            j = 2 * s + jj
            upcols += list(range(j * 128, (j + 1) * 128))
        for jj in range(2):
            j = 2 * s + jj
            upcols += list(range(DFF + j * 128, DFF + (j + 1) * 128))
    gains = np.stack([f(inp["norm_mix_g"])[0], f(inp["norm_xattn_g"])[0], f(inp["norm_mem_g"])[0],
                      f(inp["norm_ffn_g"])[0], f(inp["norm_final_g"])], 0)
    gains = np.ascontiguousarray(gains.reshape(5, 8, 128).transpose(2, 0, 1))
    swab, cm = _consts()
    sh = {
        "w_inF": _chunked(w_in[:, colsF]),
        "w_inT": _chunked(w_in[:, colsT]),
        "w_out": _chunked(f(inp["w_out"])[0]),
        "wq": _chunked(f(inp["wq_x"])[0]),
        "wkv": _chunked(f(inp["wkv_x"])[0]),
        "wo": _chunked(f(inp["wo_x"])[0]),
        "w_up": _chunked(w_up[:, upcols]),
        "w_down": _chunked(f(inp["w_down"])[0]),
        "gains": gains,
        "cqw": np.ascontiguousarray(f(inp["conv_qk_w"])[0].reshape(4, 8, 128).transpose(2, 1, 0)),
        "cqb": np.ascontiguousarray(f(inp["conv_qk_b"])[0].reshape(8, 128).T),
        "cfw": np.ascontiguousarray(f(inp["conv_ffn_w"])[0].reshape(3, NJ, 128).transpose(2, 1, 0)),
        "cfb": np.ascontiguousarray(f(inp["conv_ffn_b"])[0].reshape(NJ, 128).T),
        "bgate": np.ascontiguousarray(f(inp["b_gate_if"])[0]),
        "sinks": np.ascontiguousarray(f(inp["attn_sinks"])[0]),
        "swab": swab,
        "cmat": cm,
    }
    return sh


def _core_inputs(x, mem, nseq, ntok_per_seq):
    xs = x[:, :ntok_per_seq, :].reshape(nseq * ntok_per_seq, D)
    xT = np.ascontiguousarray(xs.T.reshape(8, 128, -1).transpose(1, 0, 2))
    memT = np.ascontiguousarray(mem.transpose(0, 2, 1).reshape(nseq, 8, 128, MEM).transpose(0, 2, 1, 3))
    return {"xT": xT, "memT": memT}


def build_two_pass(nseq, ntps):
    build_program(nseq, ntps)
    return build_program(nseq, ntps, wplan=list(build_program.wrec))


_NC_CACHE = {}


def kernel(**inputs):
    x = np.asarray(inputs["x"], dtype=np.float32)
    mem = np.asarray(inputs["mem"], dtype=np.float32)
    nseq = BATCH // NCORES
    ntps = SEQ // T
    key = (nseq, ntps)
    if key not in _NC_CACHE:
        _NC_CACHE[key] = build_two_pass(nseq, ntps)
    nc = _NC_CACHE[key]
    sh = _shared_inputs(inputs)
    in_maps = []
    for c in range(NCORES):
        m = dict(sh)
        m.update(_core_inputs(x[c * nseq:(c + 1) * nseq], mem[c * nseq:(c + 1) * nseq], nseq, SEQ))
        in_maps.append(m)
    res = run_bass_kernel_spmd(nc, in_maps, core_ids=list(range(NCORES)))
    out = np.empty((BATCH, SEQ, D), np.float32)
    for c in range(NCORES):
        oT = np.asarray(res.results[c]["outT"], dtype=np.float32)
        o = oT.transpose(2, 1, 0).reshape(nseq, SEQ, D)
        out[c * nseq:(c + 1) * nseq] = o
    return out
```

```python
import os
import numpy as np
import ml_dtypes
from contextlib import ExitStack
import concourse.bass as bass
import concourse.mybir as mybir
from concourse.bass_utils import run_bass_kernel_spmd

F32 = mybir.dt.float32
BF16 = mybir.dt.bfloat16
ALU = mybir.AluOpType
AF = mybir.ActivationFunctionType

D = 1024
SEQ = 2048
BATCH = 16
NCORES = 8
T = 512
NB = 4
DFF = 2816
NJ = 22
MEM = 256
EPS = 1e-6
NSLAB = 3
SLAB_ELEMS = 4096


class StopBuild(Exception):
    pass


class Sched:
    def __init__(self, nc, es):
        self.nc = nc
        self.es = es
        self.engs = {"pe": nc.tensor, "act": nc.scalar, "dve": nc.vector, "pool": nc.gpsimd, "sp": nc.sync}
        self.sems = {}
        self.cnt = {}
        self.seen = {e: {} for e in self.engs}
        self.w = {}
        self.r = {}
        for e in ("pe", "act", "dve", "pool"):
            self.new_sem(e)
        self.nwaits = 0
        self.nops = 0
        self.marks = []
        self._pending = None

    def new_sem(self, name):
        self.sems[name] = self.es.enter_context(self.nc.semaphore("s_" + name))
        self.cnt[name] = 0

    def _deps(self, eng, reads, writes):
        deps = []
        pe = (eng == "pe")
        for k in reads:
            t = self.w.get(k)
            if t is not None and not (pe and t[2] == "pe"):
                deps.append(t)
        for k in writes:
            t = self.w.get(k)
            if t is not None and not (pe and t[2] == "pe"):
                deps.append(t)
            for t in self.r.get(k, ()):
                if not (pe and t[2] == "pe"):
                    deps.append(t)
        return deps

    def _wait(self, eng, deps):
        need = {}
        for (s, v, _) in deps:
            if v > need.get(s, 0):
                need[s] = v
        for s, v in need.items():
            if self.seen[eng].get(s, 0) < v:
                assert v <= self.cnt[s], ("wait on unsignalled", eng, s, v, self.cnt[s])
                self.engs[eng].wait_ge(self.sems[s], v)
                self.seen[eng][s] = v
                self.nwaits += 1

    def _record(self, tok, reads, writes):
        for k in reads:
            self.r.setdefault(k, []).append(tok)
        for k in writes:
            self.w[k] = tok
            self.r[k] = []

    def op(self, eng, fn, reads=(), writes=(), signal=True):
        self._wait(eng, self._deps(eng, reads, writes))
        ins = fn(self.engs[eng])
        self.nops += 1
        if self._pending is not None:
            self.marks.append((self._pending, ins.ins.name))
            self._pending = None
        if signal:
            self.cnt[eng] += 1
            ins.then_inc(self.sems[eng], 1)
            tok = (eng, self.cnt[eng], eng)
        else:
            tok = (eng, self.cnt[eng] + 1, eng)
        self._record(tok, reads, writes)
        return ins

    def mark(self, label):
        if os.environ.get('STOP_AT') == label:
            raise StopBuild(label)
        self._pending = label

    def dma(self, issuer, sem, out, in_, reads=(), writes=()):
        if sem not in self.sems:
            self.new_sem(sem)
        self._wait(issuer, self._deps("dma:" + sem, reads, writes))
        ins = self.engs[issuer].dma_start(out=out, in_=in_)
        self.cnt[sem] += 16
        ins.then_inc(self.sems[sem], 16)
        tok = (sem, self.cnt[sem], "dma:" + sem)
        self._record(tok, reads, writes)
        return ins


def build_program(nseq, ntps, dbg=False, wplan=None):
    NT = nseq * ntps
    NTOK = NT * T
    nc = bass.Bass("TRN2", target_bir_lowering=False)

    def din(name, shape):
        return nc.dram_tensor(name, list(shape), F32, kind="ExternalInput").ap()

    xT_d = din("xT", [128, 8, NTOK])
    memT_d = din("memT", [nseq, 128, 8, MEM])
    winF_d = din("w_inF", [128, 8, 1664])
    winT_d = din("w_inT", [128, 8, 1160])
    wout_d = din("w_out", [128, 8, 1024])
    wq_d = din("wq", [128, 8, 1024])
    wkv_d = din("wkv", [128, 8, 2048])
    wo_d = din("wo", [128, 8, 1024])
    wup_d = din("w_up", [128, 8, 2 * DFF])
    wdn_d = din("w_down", [128, NJ, 1024])
    gains_d = din("gains", [128, 5, 8])
    cqw_d = din("cqw", [128, 8, 4])
    cqb_d = din("cqb", [128, 8])
    cfw_d = din("cfw", [128, NJ, 3])
    cfb_d = din("cfb", [128, NJ])
    bgate_d = din("bgate", [8])
    sinks_d = din("sinks", [8])
    swab_d = din("swab", [4, 128, 512])
    cmat_d = din("cmat", [5, 128, 128])
    outT_d = nc.dram_tensor("outT", [128, 8, NTOK], F32, kind="ExternalOutput").ap()
    if dbg:
        dbg_d = nc.dram_tensor("dbg", [128, 8, NTOK], F32, kind="ExternalOutput").ap()

    with ExitStack() as es:
        S = Sched(nc, es)

        def sb(name, shape, dt):
            return es.enter_context(nc.sbuf_tensor("sb_" + name, list(shape), dt))

        xT = [sb(f"xT{i}", [128, 8, T], F32) for i in range(2)]
        hT = sb("hT", [128, 8, T], BF16)
        mixT = sb("mixT", [128, 8, T], BF16)
        lnv = sb("lnv", [128, T], F32)
        rstd = sb("rstd", [128, T], F32)
        lnv2 = sb("lnv2", [128, T], F32)
        rstd2 = sb("rstd2", [128, T], F32)
        sqb = sb("sqb", [128, 8, T], BF16)
        qTa = sb("qTa", [128, 4, T], BF16)
        kTa = [sb(f"kTa{i}", [128, 128 + T], BF16) for i in range(2)]
        pre = sb("pre", [128, 8, T + 3], F32)
        ct = [sb(f"ct{i}", [128, T], F32) for i in range(2)]
        qkT = sb("qkT", [128, 8, T], BF16)
        vaug = [sb(f"vaug{i}", [128, 2, 65], BF16) for i in range(NB + 1)]
        vpr = [sb(f"vpr{i}", [128, 4, 129], BF16) for i in range(NB)]
        sgo = [sb(f"sgo{i}", [128, T], BF16) for i in range(NB)]
        G = sb("G", [128, NB, 8], F32)
        GE = sb("GE", [128, NB, 4], F32)
        SPV = sb("SPV", [128, NB, 4], F32)
        GA = sb("GA", [128, NB, 4], F32)
        WS = sb("WS", [128, NB * 4], F32)
        CL = sb("CL", [128, NB * 4], F32)
        DEC = [sb(f"DEC{i}", [128, NB * 4], F32) for i in range(2)]
        DECQ = [sb(f"DECQ{i}", [128, NB * 4], F32) for i in range(2)]
        ktok = sb("ktok", [128, 4, 128], BF16)
        scb = sb("scb", [128, 4, 128], BF16)
        Q = sb("Q", [128, 4, 129], F32)
        Pq = sb("Pq", [128, 4, 129], BF16)
        dn = sb("dn", [128, 16], F32)
        rr = sb("rr", [128, 16], F32)
        mlo = [sb(f"mlo{i}", [128, T], BF16) for i in range(2)]
        att = [sb(f"att{i}", [128, T], BF16) for i in range(2)]
        pT = [sb(f"pT{i}", [128, T], BF16) for i in range(4)]
        KTx = sb("KTx", [128, 8, MEM], BF16)
        Vx = [sb(f"Vx{i}", [128, 1024], BF16) for i in range(2)]
        rs = [sb(f"rs{i}", [128, T], F32) for i in range(2)]
        aT = sb("aT", [128, NJ, T], BF16)
        gbuf = [sb(f"gbuf{i}", [128, T + 4], F32) for i in range(3)]
        sgt = [sb(f"sgt{i}", [128, T], F32) for i in range(2)]
        ghalo = sb("ghalo", [128, NJ, 4], F32)
        slab = [sb(f"slab{i}", [128, SLAB_ELEMS], BF16) for i in range(NSLAB)]
        swab = sb("swab", [128, 4, 512], BF16)
        cmatf = sb("cmatf", [128, 5, 128], F32)
        ident = sb("ident", [128, 128], BF16)
        ones = sb("ones", [128, 128], BF16)
        onesdiv = sb("onesdiv", [128, 128], BF16)
        gains = sb("gains", [128, 5, 8], F32)
        cqw = sb("cqw", [128, 8, 4], F32)
        cqb = sb("cqb", [128, 8], F32)
        cfw = sb("cfw", [128, NJ, 3], F32)
        cfb = sb("cfb", [128, NJ], F32)
        bgate = sb("bgate", [128, 8], F32)
        esink = sb("esink", [128, 8], F32)
        mmask = cmatf[:, 0, :]
        tri = cmatf[:, 1, :]
        negones = cmatf[:, 2, :]

        ps = [es.enter_context(nc.psum_tensor(f"ps{i}", [128, 512], F32)) for i in range(8)]
        psb = [p[:].bitcast(BF16) for p in ps]
        bank_ctr = [0]

        def newbank():
            b = bank_ctr[0] % 7
            bank_ctr[0] += 1
            return b

        def pk(bank, lo=0, hi=512):
            return [("ps", bank)]

        def ACT(out, in_, func, reads, writes, bias=None, scale=None):
            kw = {}
            if bias is not None:
                kw["bias"] = bias
            if scale is not None:
                kw["scale"] = scale
            return S.op("act", lambda e: e.activation(out=out, in_=in_, func=func, **kw), reads, writes)

        def TT(out, in0, in1, op, reads, writes, eng="dve"):
            return S.op(eng, lambda e: e.tensor_tensor(out=out, in0=in0, in1=in1, op=op), reads, writes)

        def TS(out, in0, s1, s2, op0, op1, reads, writes, eng="dve"):
            if s2 is None:
                return S.op(eng, lambda e: e.tensor_scalar(out=out, in0=in0, scalar1=s1, scalar2=None, op0=op0), reads, writes)
            return S.op(eng, lambda e: e.tensor_scalar(out=out, in0=in0, scalar1=s1, scalar2=s2, op0=op0, op1=op1), reads, writes)

        def STT(out, in0, scalar, in1, op0, op1, reads, writes):
            return S.op("dve", lambda e: e.scalar_tensor_tensor(out=out, in0=in0, scalar=scalar, in1=in1, op0=op0, op1=op1), reads, writes)

        def CP(out, in_, reads, writes, eng="dve"):
            return S.op(eng, lambda e: e.tensor_copy(out=out, in_=in_), reads, writes)

        def MS(ap, val, writes, eng="dve"):
            return S.op(eng, lambda e: e.memset(ap, val), (), writes)

        def MM(out, lhsT, rhs, start, stop, reads, writes, signal):
            return S.op("pe", lambda e: e.matmul(out, lhsT=lhsT, rhs=rhs, start=start, stop=stop), reads, writes, signal=signal)

        def TR(out, in_, reads, writes, signal):
            return S.op("pe", lambda e: e.transpose(out, in_, ident[:]), list(reads) + ["ident"], writes, signal=signal)

        S.dma("sp", "c0", cmatf[:], cmat_d.rearrange("c p f -> p c f"), (), ["cmatf"])
        S.dma("pool", "c1", swab[:], swab_d.rearrange("c p f -> p c f"), (), ["swab"])
        S.dma("sp", "c2", gains[:], gains_d, (), ["gains"])
        S.dma("sp", "c3", cqw[:], cqw_d, (), ["cqw"])
        S.dma("sp", "c4", cqb[:], cqb_d, (), ["cqb"])
        S.dma("sp", "c5", cfw[:], cfw_d, (), ["cfw"])
        S.dma("sp", "c6", cfb[:], cfb_d, (), ["cfb"])
        S.dma("sp", "c7", bgate[:], bgate_d.partition_broadcast(128), (), ["bgate"])
        S.dma("sp", "c8", esink[:], sinks_d.partition_broadcast(128), (), ["esink"])
        CP(ident[:], cmatf[:, 3, :], ["cmatf"], ["ident"])
        CP(ones[:], cmatf[:, 4, :], ["cmatf"], ["ones"])
        TS(onesdiv[:], cmatf[:, 4, :], 1.0 / D, None, ALU.mult, None, ["cmatf"], ["onesdiv"])
        ACT(esink[:], esink[:], AF.Exp, ["esink"], ["esink"])
        for i in range(NB + 1):
            MS(vaug[i][:], 1.0, [("vaug", i)])
        for i in range(2):
            MS(kTa[i][:], 0.0, [("kTa", i)])
            MS(DEC[i][:], 0.0, [("DEC", i)])
            MS(DECQ[i][:], 0.0, [("DECQ", i)])

        class WStream:
            def __init__(self, plan):
                self.plan = plan
                self.req = []
                self.rec = []
                self.issued = 0
                self.cur = 0

            def view(self, i, kdim, ncols):
                return slab[i % NSLAB][:, 0:kdim * ncols].rearrange("p (k n) -> p k n", k=kdim)

            def _issue(self, i, spec):
                name, c0, kdim, ncols = spec
                b = i % NSLAB
                S.dma("pool", f"ws{b}", self.view(i, kdim, ncols), WSRC[name][:, :, c0:c0 + ncols], (), [("slab", b)])

            def get(self, name, c0, kdim, ncols):
                spec = (name, c0, kdim, ncols)
                i = self.cur
                self.rec.append(spec)
                if self.plan is None:
                    self._issue(i, spec)
                else:
                    assert self.plan[i] == spec, (i, self.plan[i], spec)
                    while self.issued < min(len(self.plan), i + NSLAB):
                        self._issue(self.issued, self.plan[self.issued])
                        self.issued += 1
                self.cur += 1
                return self.view(i, kdim, ncols), ("slab", i % NSLAB)

        WSRC = {"wkv": wkv_d, "winF": winF_d, "winT": winT_d, "wout": wout_d, "wq": wq_d, "wo": wo_d,
                "wup": wup_d, "wdn": wdn_d}
        W = WStream(wplan)

        def xk(xb, c):
            return ("x", xb, c)

        SB7 = [("ps", 7)]

        def stats_finish(n, which=0):
            lv, rv = (lnv, rstd) if which == 0 else (lnv2, rstd2)
            lk, rk = ("lnv", "rstd") if which == 0 else ("lnv2", "rstd2")
            ACT(lv[:, 0:n], ps[7][:, 0:n], AF.Ln, SB7 + ["epsb"], [lk], bias=epsb[:, 0:1])
            ACT(rv[:, 0:n], lv[:, 0:n], AF.Exp, [lk], [rk], scale=-0.5)

        def stats_full(src_ap, src_keys, n):
            S.op("act", lambda e: e.activation(out=sqb[:, :, 0:n], in_=src_ap, func=AF.Square),
                 src_keys, [("sqb", c) for c in range(8)])
            for c in range(8):
                MM(ps[7][:, 0:n], onesdiv[:], sqb[:, c, 0:n], c == 0, c == 7, [("sqb", c), "onesdiv"], SB7, c == 7)

        def norm_stats(src_ap_fn, src_keys_fn, n):
            stats_full(src_ap_fn(None), [src_keys_fn(c) for c in range(8)], n)
            stats_finish(n)

        class FusedStats:
            def __init__(self, xb):
                self.xb = xb
                self.pending = []

            def add(self, mm):
                if os.environ.get('NO_FUSED'):
                    return
                ACT(sqb[:, mm, :], xT[self.xb][:, mm, :], AF.Square, [xk(self.xb, mm)], [("sqb", mm)])
                self.pending.append(mm)

            def flush(self, keep):
                if os.environ.get('NO_FUSED'):
                    return
                while len(self.pending) > keep:
                    c = self.pending.pop(0)
                    MM(ps[7][:], onesdiv[:], sqb[:, c, :], c == 0, c == 7, [("sqb", c), "onesdiv"], SB7, c == 7)

        def norm_apply(xb, gi, which=0):
            rv, rk = (rstd, "rstd") if which == 0 else (rstd2, "rstd2")
            for c in range(8):
                STT(hT[:, c, :], xT[xb][:, c, :], gains[:, gi, c:c + 1], rv[:], ALU.mult, ALU.mult,
                    [xk(xb, c), rk, "gains"], [("hT", c)])

        def proj_group(b, n, lhs_fn, rhs_fn, nk, reads_fn, width=None):
            for k in range(nk):
                MM(ps[b][:, 0:n], lhs_fn(k), rhs_fn(k), k == 0, k == nk - 1, reads_fn(k), pk(b, 0, n), k == nk - 1)

        epsb = sb("epsb", [128, 8], F32)
        MS(epsb[:], EPS, ["epsb"])

        def load_x(t):
            xb = t % 2
            S.dma("sp", f"x{xb}", xT[xb][:], xT_d[:, :, t * T:(t + 1) * T], (), [xk(xb, c) for c in range(8)])

        def mem_stage(s):
            S.dma("sp", "mem", pre[:, :, 0:MEM], memT_d[s], (), [("pre", c) for c in range(8)])
            norm_stats(lambda _: pre[:, :, 0:MEM], lambda c: ("pre", c), MEM)
            for c in range(8):
                STT(hT[:, c, 0:MEM], pre[:, c, 0:MEM], gains[:, 2, c:c + 1], rstd[:, 0:MEM], ALU.mult, ALU.mult,
                    [("pre", c), "rstd", "gains"], [("hT", c)])
            for s4 in range(4):
                wv, wkey = W.get("wkv", s4 * 512, 8, 512)
                if s4 < 2:
                    for m in range(4):
                        b = newbank()
                        proj_group(b, MEM, lambda k: wv[:, k, m * 128:(m + 1) * 128], lambda k: hT[:, k, 0:MEM], 8,
                                   lambda k: [wkey, ("hT", k)])
                        ACT(KTx[:, s4 * 4 + m, :], ps[b][:, 0:MEM], AF.Copy, pk(b, 0, MEM), ["KTx"])
                else:
                    for mb in range(2):
                        b = newbank()
                        proj_group(b, 512, lambda k: hT[:, k, mb * 128:(mb + 1) * 128], lambda k: wv[:, k, :], 8,
                                   lambda k: [wkey, ("hT", k)])
                        ACT(Vx[mb][:, (s4 - 2) * 512:(s4 - 1) * 512], ps[b][:], AF.Copy, pk(b), [("Vx", mb)])

        def mixer_front(t):
            xb = t % 2
            j = t % ntps
            first = (j == 0)
            tb = t % 2
            if first:
                MS(pre[:, :, 0:3], 0.0, [("pre", c) for c in range(8)])
                MS(Q[:], 0.0, [("Q", h_) for h_ in range(4)])
                MS(ghalo[:], 0.0, [("ghalo", jx_) for jx_ in range(NJ)])
            else:
                CP(pre[:, :, 0:3], pre[:, :, T:T + 3], [("pre", c) for c in range(8)], [("pre", c) for c in range(8)])
                CP(kTa[0][0:64, 0:128], kTa[0][0:64, T:T + 128], [("kTa", 0)], [("kTa", 0)])
                CP(kTa[1][64:128, 0:128], kTa[1][64:128, T:T + 128], [("kTa", 1)], [("kTa", 1)])
                CP(vaug[0][:], vaug[NB][:], [("vaug", NB)], [("vaug", 0)])
            if first or t == 0 or not os.environ.get('USE_EARLY'):
                norm_stats(lambda _: xT[xb][:], lambda c: xk(xb, c), T)
                norm_apply(xb, 0)
            S.mark(f"m{t}.F0")
            wv, wkey = W.get("winF", 0, 8, 512)
            for m in range(4):
                b = newbank()
                proj_group(b, T, lambda k: wv[:, k, m * 128:(m + 1) * 128], lambda k: hT[:, k, :], 8,
                           lambda k: [wkey, ("hT", k)])
                ACT(qTa[:, m, :], ps[b][:], AF.Copy, pk(b), ["qTa"], scale=0.125)
            yield
            S.mark(f"m{t}.F12")
            for f in range(2):
                wv, wkey = W.get("winF", 512 + f * 512, 8, 512)
                for mp in range(2):
                    info = []
                    for m in (2 * mp, 2 * mp + 1):
                        c = f * 4 + m
                        b = newbank()
                        proj_group(b, T, lambda k: wv[:, k, m * 128:(m + 1) * 128], lambda k: hT[:, k, :], 8,
                                   lambda k: [wkey, ("hT", k)])
                        info.append((c, b, ct[c % 2], ("ct", c % 2)))
                    for (c, b, tt, tk) in info:
                        ACT(pre[:, c, 3:T + 3], ps[b][:], AF.Copy, pk(b), [("pre", c)])
                    for (c, b, tt, tk) in info:
                        TS(tt[:], pre[:, c, 0:T], cqw[:, c, 0:1], None, ALU.mult, None, [("pre", c), "cqw"], [tk])
                    for j4 in (1, 2, 3):
                        for (c, b, tt, tk) in info:
                            STT(tt[:], pre[:, c, j4:T + j4], cqw[:, c, j4:j4 + 1], tt[:], ALU.mult, ALU.add,
                                [("pre", c), "cqw", tk], [tk])
                    for (c, b, tt, tk) in info:
                        ACT(qkT[:, c, :], tt[:], AF.Silu, [tk, "cqb"], [("qkT", c)], bias=cqb[:, c:c + 1])
                yield
            S.mark(f"m{t}.F3")
            wv, wkey = W.get("winF", 1536, 8, 128)
            b = newbank()
            proj_group(b, T, lambda k: wv[:, k, 0:128], lambda k: hT[:, k, :], 8, lambda k: [wkey, ("hT", k)])
            ACT(kTa[0][0:64, 128:128 + T], ps[b][0:64, :], AF.Copy, pk(b), [("kTa", 0)])
            ACT(kTa[1][64:128, 128:128 + T], ps[b][64:128, :], AF.Copy, pk(b), [("kTa", 1)])
            yield
            S.mark(f"m{t}.T2")
            wv, wkey = W.get("winT", 1024, 8, 136)
            for bi in range(NB):
                b = newbank()
                proj_group(b, 136, lambda k: hT[:, k, bi * 128:(bi + 1) * 128], lambda k: wv[:, k, :], 8,
                           lambda k: [wkey, ("hT", k)])
                ACT(vaug[1 + bi][:, :, 0:64], ps[b][:, 0:128].rearrange("p (a e) -> p a e", a=2), AF.Copy,
                    pk(b, 0, 136), [("vaug", 1 + bi)])
                TT(G[:, bi, :], ps[b][:, 128:136], bgate[:], ALU.add, pk(b, 0, 136) + ["bgate"], ["G"])
            yield
            S.mark(f"m{t}.gates")
            ACT(GE[:], G[:, :, 4:8], AF.Exp, ["G"], ["GE"], scale=-1.0)
            ACT(SPV[:], GE[:], AF.Ln, ["GE"], ["SPV"], bias=1.0)
            bg = newbank()
            for bi in range(NB):
                MM(ps[bg][:, bi * 4:(bi + 1) * 4], tri, SPV[:, bi, :], True, True, ["SPV", "cmatf"], pk(bg, 0, 32), False)
            for bi in range(NB):
                MM(ps[bg][:, 16 + bi * 4:16 + (bi + 1) * 4], negones, SPV[:, bi, :], True, True, ["SPV", "cmatf"],
                   pk(bg, 0, 32), bi == NB - 1)
            TT(GA[:], G[:, :, 0:4], ps[bg][:, 0:16].rearrange("p (a e) -> p a e", a=NB), ALU.subtract,
               ["G"] + pk(bg, 0, 32), ["GA"])
            ACT(WS[:], GA[:].rearrange("p a e -> p (a e)"), AF.Exp, ["GA"], ["WS"])
            ACT(CL[:], ps[bg][:, 0:16], AF.Exp, pk(bg, 0, 32), ["CL"], scale=-1.0)
            ACT(DEC[tb][:], ps[bg][:, 16:32], AF.Exp, pk(bg, 0, 32), [("DEC", tb)])
            ACT(DECQ[tb][:], DEC[tb][:], AF.Copy, [("DEC", tb)], [("DECQ", tb)], scale=float(128 ** -0.5))
            yield
            S.mark(f"m{t}.T0")
            wv, wkey = W.get("winT", 0, 8, 512)
            for bi in range(NB):
                b = newbank()
                proj_group(b, 512, lambda k: hT[:, k, bi * 128:(bi + 1) * 128], lambda k: wv[:, k, :], 8,
                           lambda k: [wkey, ("hT", k)])
                TT(vpr[bi][:, :, 0:128], ps[b][:].rearrange("p (a e) -> p a e", a=4),
                   WS[:, bi * 4:(bi + 1) * 4].unsqueeze(2).to_broadcast([128, 4, 128]), ALU.mult,
                   pk(b) + ["WS"], [("vpr", bi)])
                CP(vpr[bi][:, :, 128], WS[:, bi * 4:(bi + 1) * 4], ["WS"], [("vpr", bi)])
            yield
            S.mark(f"m{t}.T1")
            wv, wkey = W.get("winT", 512, 8, 512)
            for bi in range(NB):
                b = newbank()
                proj_group(b, 512, lambda k: hT[:, k, bi * 128:(bi + 1) * 128], lambda k: wv[:, k, :], 8,
                           lambda k: [wkey, ("hT", k)])
                ACT(sgo[bi][:], ps[b][:], AF.Tanh, pk(b), [("sgo", bi)], scale=0.5)
                TS(sgo[bi][:], sgo[bi][:], 0.5, 0.5, ALU.mult, ALU.add, [("sgo", bi)], [("sgo", bi)])
            yield

        def swa_block(t, bi):
            j = t % ntps
            blk = slice(bi * 128, (bi + 1) * 128)
            nb = j * NB + bi
            ai = bi % 2
            kbs = [("cur", 1 + bi, slice(128 + bi * 128, 256 + bi * 128), 1)]
            if nb > 0:
                kbs.append(("prev", bi, slice(bi * 128, 128 + bi * 128), 0))
            for kv in range(2):
                for (_, slot, kc, kbi) in kbs:
                    b = newbank()
                    MM(ps[b][:], kTa[kv][:, kc], qTa[:, :, blk], True, False, [("kTa", kv), "qTa"], pk(b), False)
                    MM(ps[b][:], ident[:], swab[:, kv * 2 + kbi, :], False, True, ["ident", "swab"], pk(b), True)
                    ACT(pT[kv * 2 + kbi][:], ps[b][:], AF.Exp, pk(b), [("pT", kv * 2 + kbi)])
                yield
            bos = []
            for kv in range(2):
                bo = newbank()
                bos.append(bo)
                for g in range(4):
                    for ii, (_, slot, kc, kbi) in enumerate(kbs):
                        MM(ps[bo][:, g * 65:(g + 1) * 65], pT[kv * 2 + kbi][:, g * 128:(g + 1) * 128],
                           vaug[slot][:, kv, :], ii == 0, ii == len(kbs) - 1,
                           [("pT", kv * 2 + kbi), ("vaug", slot)], pk(bo, 0, 260),
                           g == 3 and ii == len(kbs) - 1)
                yield
            for kv in range(2):
                bo = bos[kv]
                o3 = ps[bo][:, 0:260].rearrange("p (g e) -> p g e", g=4)
                TT(dn[:, kv * 4:(kv + 1) * 4], o3[:, :, 64], esink[:, kv * 4:(kv + 1) * 4], ALU.add,
                   pk(bo, 0, 260) + ["esink"], [("dn", kv)])
                S.op("dve", lambda e: e.reciprocal(out=rr[:, kv * 4:(kv + 1) * 4], in_=dn[:, kv * 4:(kv + 1) * 4]),
                     [("dn", kv)], [("rr", kv)])
                TT(att[ai][:, kv * 256:(kv + 1) * 256].rearrange("p (g e) -> p g e", g=4), o3[:, :, 0:64],
                   rr[:, kv * 4:(kv + 1) * 4].unsqueeze(2).to_broadcast([128, 4, 64]), ALU.mult,
                   pk(bo, 0, 260) + [("rr", kv)], [("att", ai)])
                yield
            bt = newbank()
            for c in range(4):
                TR(psb[bt][:, c * 128:(c + 1) * 128], att[ai][:, c * 128:(c + 1) * 128], [("att", ai)],
                   pk(bt, 0, 256), c == 3)
            ACT(mixT[:, 0:4, blk], psb[bt][:, 0:512].rearrange("p (c n) -> p c n", c=4), AF.Copy,
                pk(bt, 0, 256), [("mixT", c) for c in range(4)])
            yield

        def ml_block(t, bi):
            tb = t % 2
            blk = slice(bi * 128, (bi + 1) * 128)
            if bi == 0:
                dprev, dqprev = DEC[1 - tb][:, 12:16], DECQ[1 - tb][:, 12:16]
                dkey, dqkey = ("DEC", 1 - tb), ("DECQ", 1 - tb)
            else:
                dprev, dqprev = DEC[tb][:, (bi - 1) * 4:bi * 4], DECQ[tb][:, (bi - 1) * 4:bi * 4]
                dkey, dqkey = ("DEC", tb), ("DECQ", tb)
            mi = bi % 2
            bA = newbank()
            for h in range(4):
                TR(psb[bA][:, h * 128:(h + 1) * 128], qkT[:, 4 + h, blk], [("qkT", 4 + h)], pk(bA), h == 3)
            ACT(ktok[:].rearrange("p h d -> p (h d)"), psb[bA][:, 0:512], AF.Copy, pk(bA), ["ktok"])
            bB = newbank()
            for h in range(4):
                MM(ps[bB][:, h * 128:(h + 1) * 128], qkT[:, 4 + h, blk], qkT[:, h, blk], True, True,
                   [("qkT", 4 + h), ("qkT", h)], pk(bB), h == 3)
            TT(scb[:], ps[bB][:].rearrange("p (h t) -> p h t", h=4),
               cmatf[:, 0:1, :].to_broadcast([128, 4, 128]), ALU.mult, pk(bB) + ["cmatf"], ["scb"])
            TT(Pq[:], Q[:], dqprev.unsqueeze(2).to_broadcast([128, 4, 129]), ALU.mult, [("Q", h_) for h_ in range(4)] + [dqkey], ["Pq"])
            yield
            bC, bD = newbank(), newbank()
            for h in range(4):
                bk = bC if h < 2 else bD
                off = (h % 2) * 129
                MM(ps[bk][:, off:off + 129], qkT[:, h, blk], Pq[:, h, :], True, False, [("qkT", h), "Pq"], pk(bk), False)
                MM(ps[bk][:, off:off + 129], scb[:, h, :], vpr[bi][:, h, :], False, True, ["scb", ("vpr", bi)], pk(bk),
                   h == 1)
            for h in range(4):
                MM(ps[bD][:, 258 + 2 * h:260 + 2 * h], qkT[:, h, blk], Pq[:, h, 127:129], True, False,
                   [("qkT", h), "Pq"], pk(bD), False)
                MM(ps[bD][:, 258 + 2 * h:260 + 2 * h], scb[:, h, :], vpr[bi][:, h, 127:129], False, True,
                   ["scb", ("vpr", bi)], pk(bD), h == 3)
            bE, bF = newbank(), newbank()
            for h in range(4):
                bk = bE if h < 2 else bF
                off = (h % 2) * 129
                MM(ps[bk][:, off:off + 129], ktok[:, h, :], vpr[bi][:, h, :], True, True, ["ktok", ("vpr", bi)], pk(bk),
                   h % 2 == 1)
            yield
            ACT(dn[:, 8:12], ps[bD][:, 258:266].rearrange("p (h two) -> p h two", two=2)[:, :, 1], AF.Abs,
                pk(bD), ["dnm"])
            TT(dn[:, 8:12], dn[:, 8:12], CL[:, bi * 4:(bi + 1) * 4], ALU.max, ["dnm", "CL"], ["dnm"])
            S.op("dve", lambda e: e.reciprocal(out=rr[:, 8:12], in_=dn[:, 8:12]), ["dnm"], ["rrm"])
            for h in range(4):
                bk = bE if h < 2 else bF
                off = (h % 2) * 129
                STT(Q[:, h, :], Q[:, h, :], dprev[:, h:h + 1], ps[bk][:, off:off + 129], ALU.mult, ALU.add,
                    [("Q", h), dkey] + pk(bk), [("Q", h)])
            yield
            for h in range(4):
                bk = bC if h < 2 else bD
                off = (h % 2) * 129
                STT(mlo[mi][:, h * 128:(h + 1) * 128], ps[bk][:, off:off + 128], rr[:, 8 + h:9 + h],
                    sgo[bi][:, h * 128:(h + 1) * 128], ALU.mult, ALU.mult,
                    pk(bk) + ["rrm", ("sgo", bi)], [("mlo", mi, h)])
            yield
            bt = newbank()
            for c in range(4):
                TR(psb[bt][:, c * 128:(c + 1) * 128], mlo[mi][:, c * 128:(c + 1) * 128], [("mlo", mi, c)],
                   pk(bt, 0, 256), c == 3)
            ACT(mixT[:, 4:8, blk], psb[bt][:, 0:512].rearrange("p (c n) -> p c n", c=4), AF.Copy,
                pk(bt, 0, 256), [("mixT", c) for c in range(4, 8)])
            yield

        def run_interleaved(gens):
            gens = list(gens)
            while gens:
                for g in list(gens):
                    try:
                        next(g)
                    except StopIteration:
                        gens.remove(g)

        def mixer_back(t):
            xb = t % 2
            S.mark(f"m{t}.blocks")
            for bi in range(NB):
                if os.environ.get('NO_IL2'):
                    for _ in swa_block(t, bi):
                        pass
                    for _ in ml_block(t, bi):
                        pass
                else:
                    run_interleaved([swa_block(t, bi), ml_block(t, bi)])
            S.mark(f"m{t}.wout")
            fs = FusedStats(xb)
            for s2 in range(2):
                wv, wkey = W.get("wout", s2 * 512, 8, 512)
                for m in range(4):
                    mm = s2 * 4 + m
                    b = newbank()
                    proj_group(b, T, lambda k: wv[:, k, m * 128:(m + 1) * 128], lambda k: mixT[:, k, :], 8,
                               lambda k: [wkey, ("mixT", k)])
                    TT(xT[xb][:, mm, :], xT[xb][:, mm, :], ps[b][:], ALU.add, pk(b) + [xk(xb, mm)], [xk(xb, mm)])
                    fs.add(mm)
                    fs.flush(3)
            fs.flush(0)

        def xattn(t):
            xb = t % 2
            if os.environ.get('NO_FUSED'):
                norm_stats(lambda _: xT[xb][:], lambda c: xk(xb, c), T)
            else:
                stats_finish(T)
            norm_apply(xb, 1)
            for s2 in range(2):
                wv, wkey = W.get("wq", s2 * 512, 8, 512)
                for m in range(4):
                    mm = s2 * 4 + m
                    b = newbank()
                    proj_group(b, T, lambda k: wv[:, k, m * 128:(m + 1) * 128], lambda k: hT[:, k, :], 8,
                               lambda k: [wkey, ("hT", k)])
                    ACT(qkT[:, mm, :], ps[b][:], AF.Copy, pk(b), [("qkT", mm)], scale=0.0625)
            for h in range(4):
                pidx = [(h % 2) * 2 + mb for mb in range(2)]
                for mb in range(2):
                    b = newbank()
                    for dc in range(2):
                        MM(ps[b][:], KTx[:, 2 * h + dc, mb * 128:(mb + 1) * 128], qkT[:, 2 * h + dc, :], dc == 0, dc == 1,
                           ["KTx", ("qkT", 2 * h + dc)], pk(b), dc == 1)
                    ACT(pT[pidx[mb]][:], ps[b][:], AF.Exp, pk(b), [("pT", pidx[mb])])
                bs = newbank()
                for mb in range(2):
                    MM(ps[bs][:], ones[:], pT[pidx[mb]][:], mb == 0, mb == 1, ["ones", ("pT", pidx[mb])], pk(bs), mb == 1)
                ri = h % 2
                ACT(lnv[:], ps[bs][:], AF.Ln, pk(bs), ["lnv"])
                ACT(rs[ri][:], lnv[:], AF.Exp, ["lnv"], [("rs", ri)], scale=-1.0)
                for ec in range(2):
                    b = newbank()
                    for mb in range(2):
                        MM(ps[b][:], Vx[mb][:, (2 * h + ec) * 128:(2 * h + ec + 1) * 128], pT[pidx[mb]][:], mb == 0, mb == 1,
                           [("Vx", mb), ("pT", pidx[mb])], pk(b), mb == 1)
                    TT(mixT[:, 2 * h + ec, :], ps[b][:], rs[ri][:], ALU.mult, pk(b) + [("rs", ri)], [("mixT", 2 * h + ec)])
            fs = FusedStats(xb)
            for s2 in range(2):
                wv, wkey = W.get("wo", s2 * 512, 8, 512)
                for m in range(4):
                    mm = s2 * 4 + m
                    b = newbank()
                    proj_group(b, T, lambda k: wv[:, k, m * 128:(m + 1) * 128], lambda k: mixT[:, k, :], 8,
                               lambda k: [wkey, ("mixT", k)])
                    TT(xT[xb][:, mm, :], xT[xb][:, mm, :], ps[b][:], ALU.add, pk(b) + [xk(xb, mm)], [xk(xb, mm)])
                    fs.add(mm)
                    fs.flush(3)
            fs.flush(0)

        def ffn(t):
            xb = t % 2
            if os.environ.get('NO_FUSED'):
                norm_stats(lambda _: xT[xb][:], lambda c: xk(xb, c), T)
            else:
                stats_finish(T)
            norm_apply(xb, 3)
            early = (t + 1 < NT) and ((t + 1) % ntps != 0) and bool(os.environ.get('USE_EARLY'))
            if early:
                nxb = (t + 1) % 2
                stats_full(xT[nxb][:], [xk(nxb, c) for c in range(8)], T)
                stats_finish(T, which=1)
            for s11 in range(11):
                wv, wkey = W.get("wup", s11 * 512, 8, 512)
                info = []
                for jj in range(2):
                    jx = s11 * 2 + jj
                    bgk = newbank()
                    proj_group(bgk, T, lambda k: wv[:, k, jj * 128:(jj + 1) * 128], lambda k: hT[:, k, :], 8,
                               lambda k: [wkey, ("hT", k)])
                    buk = newbank()
                    proj_group(buk, T, lambda k: wv[:, k, 256 + jj * 128:256 + (jj + 1) * 128], lambda k: hT[:, k, :], 8,
                               lambda k: [wkey, ("hT", k)])
                    gi = jx % 3
                    info.append((jx, bgk, buk, gbuf[gi], ("gbuf", gi), ct[jx % 2], ("ct", jx % 2), jx % 2))
                for (jx, bgk, buk, gb, gk, tt, tk, si) in info:
                    ACT(gb[:, 4:T + 4], ps[bgk][:], AF.Copy, pk(bgk), [gk])
                for (jx, bgk, buk, gb, gk, tt, tk, si) in info:
                    CP(gb[:, 0:4], ghalo[:, jx, :], [("ghalo", jx)], [gk])
                for (jx, bgk, buk, gb, gk, tt, tk, si) in info:
                    TS(tt[:], gb[:, 2:T + 2], cfw[:, jx, 0:1], None, ALU.mult, None, [gk, "cfw"], [tk])
                for (jx, bgk, buk, gb, gk, tt, tk, si) in info:
                    STT(tt[:], gb[:, 3:T + 3], cfw[:, jx, 1:2], tt[:], ALU.mult, ALU.add, [gk, "cfw", tk], [tk])
                for (jx, bgk, buk, gb, gk, tt, tk, si) in info:
                    STT(tt[:], gb[:, 4:T + 4], cfw[:, jx, 2:3], tt[:], ALU.mult, ALU.add, [gk, "cfw", tk], [tk])
                for (jx, bgk, buk, gb, gk, tt, tk, si) in info:
                    CP(ghalo[:, jx, :], gb[:, T:T + 4], [gk], [("ghalo", jx)])
                for (jx, bgk, buk, gb, gk, tt, tk, si) in info:
                    ACT(sgt[si][:], tt[:], AF.Silu, [tk, "cfb"], [("sgt", si)], bias=cfb[:, jx:jx + 1])
                for (jx, bgk, buk, gb, gk, tt, tk, si) in info:
                    TT(aT[:, jx, :], sgt[si][:], ps[buk][:], ALU.mult, [("sgt", si)] + pk(buk), [("aT", jx)])
            fg = None
            if early:
                norm_apply((t + 1) % 2, 0, which=1)
                fg = mixer_front(t + 1)
            fs = FusedStats(xb)
            for m in range(8):
                wv, wkey = W.get("wdn", m * 128, NJ, 128)
                b = newbank()
                proj_group(b, T, lambda k: wv[:, k, :], lambda k: aT[:, k, :], NJ, lambda k: [wkey, ("aT", k)])
                TT(xT[xb][:, m, :], xT[xb][:, m, :], ps[b][:], ALU.add, pk(b) + [xk(xb, m)], [xk(xb, m)])
                fs.add(m)
                fs.flush(2)
                if fg is not None and not os.environ.get('NO_IL1'):
                    next(fg, None)
            fs.flush(0)
            return fg

        def final(t):
            xb = t % 2
            if os.environ.get('NO_FUSED'):
                norm_stats(lambda _: xT[xb][:], lambda c: xk(xb, c), T)
            else:
                stats_finish(T)
            for c in range(8):
                STT(xT[xb][:, c, :], xT[xb][:, c, :], gains[:, 4, c:c + 1], rstd[:], ALU.mult, ALU.mult,
                    [xk(xb, c), "rstd", "gains"], [xk(xb, c)])
            S.dma("sp", f"o{xb}", outT_d[:, :, t * T:(t + 1) * T], xT[xb][:], [xk(xb, c) for c in range(8)], ())

        try:
            load_x(0)
            pending_front = None
            for t in range(NT):
                if t % ntps == 0:
                    mem_stage(t // ntps)
                if t + 1 < NT:
                    load_x(t + 1)
                S.mark(f"mixer{t}")
                if pending_front is None:
                    pending_front = mixer_front(t)
                for _ in pending_front:
                    pass
                mixer_back(t)
                S.mark(f"xattn{t}")
                xattn(t)
                S.mark(f"ffn{t}")
                pending_front = ffn(t)
                S.mark(f"final{t}")
                final(t)
        except StopBuild as _e:
            print('STOPPED AT', _e)
        for xb in range(2):
            nm = f"o{xb}"
            if nm in S.sems:
                nc.sync.wait_ge(S.sems[nm], S.cnt[nm])
        build_program.stats = (S.nops, S.nwaits, dict(S.cnt))
        build_program.marks = list(S.marks)
        build_program.wrec = list(W.rec)
    return nc


def _chunked(w):
    K, N = w.shape
    return np.ascontiguousarray(w.reshape(K // 128, 128, N).transpose(1, 0, 2))


def _consts():
    slopes = 2.0 ** (-(np.arange(8) + 1.0))
    k = np.arange(128)[:, None]
    q = np.arange(128)[None, :]
    swab = np.zeros((4, 128, 512), np.float32)
    for kv in range(2):
        for g in range(4):
            s = slopes[kv * 4 + g]
            d_prev = q + 128 - k
            prev = np.where(k > q, -s * d_prev, -30000.0)
            d_cur = q - k
            cur = np.where(k <= q, -s * d_cur, -30000.0)
            swab[kv * 2 + 0][:, g * 128:(g + 1) * 128] = prev
            swab[kv * 2 + 1][:, g * 128:(g + 1) * 128] = cur
    cm = np.zeros((5, 128, 128), np.float32)
    s_ = np.arange(128)[:, None]
    t_ = np.arange(128)[None, :]
    cm[0] = np.where(s_ <= t_, 128.0 ** -0.5, 0.0)
    cm[1] = np.where(s_ <= t_, -1.0, 0.0)
    cm[2] = -1.0
    cm[3] = np.eye(128)
    cm[4] = 1.0
    return swab, cm


def _shared_inputs(inp):
    f = lambda a: np.asarray(a, dtype=np.float32)
    w_in = f(inp["w_in"])[0]
    o0, o1, o2 = 512, 640, 768
    o3, o4, o5 = o2 + 1024, o2 + 1536, o2 + 2048
    qcols = []
    for c in range(4):
        for kv in range(2):
            h = kv * 4 + c
            qcols += list(range(h * 64, (h + 1) * 64))
    colsF = qcols + list(range(o2, o2 + 512)) + list(range(o2 + 512, o3)) + list(range(o0, o1))
    colsT = list(range(o3, o4)) + list(range(o4, o5)) + list(range(o1, o2)) + list(range(o5, o5 + 8))
    w_up = f(inp["w_up"])[0]
    upcols = []
    for s in range(11):
        for jj in range(2):
            j = 2 * s + jj
            upcols += list(range(j * 128, (j + 1) * 128))
        for jj in range(2):
            j = 2 * s + jj
            upcols += list(range(DFF + j * 128, DFF + (j + 1) * 128))
    gains = np.stack([f(inp["norm_mix_g"])[0], f(inp["norm_xattn_g"])[0], f(inp["norm_mem_g"])[0],
                      f(inp["norm_ffn_g"])[0], f(inp["norm_final_g"])], 0)
    gains = np.ascontiguousarray(gains.reshape(5, 8, 128).transpose(2, 0, 1))
    swab, cm = _consts()
    sh = {
        "w_inF": _chunked(w_in[:, colsF]),
        "w_inT": _chunked(w_in[:, colsT]),
        "w_out": _chunked(f(inp["w_out"])[0]),
        "wq": _chunked(f(inp["wq_x"])[0]),
        "wkv": _chunked(f(inp["wkv_x"])[0]),
        "wo": _chunked(f(inp["wo_x"])[0]),
        "w_up": _chunked(w_up[:, upcols]),
        "w_down": _chunked(f(inp["w_down"])[0]),
        "gains": gains,
        "cqw": np.ascontiguousarray(f(inp["conv_qk_w"])[0].reshape(4, 8, 128).transpose(2, 1, 0)),
        "cqb": np.ascontiguousarray(f(inp["conv_qk_b"])[0].reshape(8, 128).T),
        "cfw": np.ascontiguousarray(f(inp["conv_ffn_w"])[0].reshape(3, NJ, 128).transpose(2, 1, 0)),
        "cfb": np.ascontiguousarray(f(inp["conv_ffn_b"])[0].reshape(NJ, 128).T),
        "bgate": np.ascontiguousarray(f(inp["b_gate_if"])[0]),
        "sinks": np.ascontiguousarray(f(inp["attn_sinks"])[0]),
        "swab": swab,
        "cmat": cm,
    }
    return sh


def _core_inputs(x, mem, nseq, ntok_per_seq):
    xs = x[:, :ntok_per_seq, :].reshape(nseq * ntok_per_seq, D)
    xT = np.ascontiguousarray(xs.T.reshape(8, 128, -1).transpose(1, 0, 2))
    memT = np.ascontiguousarray(mem.transpose(0, 2, 1).reshape(nseq, 8, 128, MEM).transpose(0, 2, 1, 3))
    return {"xT": xT, "memT": memT}


def build_two_pass(nseq, ntps):
    build_program(nseq, ntps)
    return build_program(nseq, ntps, wplan=list(build_program.wrec))


_NC_CACHE = {}


def kernel(**inputs):
    x = np.asarray(inputs["x"], dtype=np.float32)
    mem = np.asarray(inputs["mem"], dtype=np.float32)
    nseq = BATCH // NCORES
    ntps = SEQ // T
    key = (nseq, ntps)
    if key not in _NC_CACHE:
        _NC_CACHE[key] = build_two_pass(nseq, ntps)
    nc = _NC_CACHE[key]
    sh = _shared_inputs(inputs)
    in_maps = []
    for c in range(NCORES):
        m = dict(sh)
        m.update(_core_inputs(x[c * nseq:(c + 1) * nseq], mem[c * nseq:(c + 1) * nseq], nseq, SEQ))
        in_maps.append(m)
    res = run_bass_kernel_spmd(nc, in_maps, core_ids=list(range(NCORES)))
    out = np.empty((BATCH, SEQ, D), np.float32)
    for c in range(NCORES):
        oT = np.asarray(res.results[c]["outT"], dtype=np.float32)
        o = oT.transpose(2, 1, 0).reshape(nseq, SEQ, D)
        out[c * nseq:(c + 1) * nseq] = o
    return out
```
